# Optimizing a Trainium2 kernel written in Bass

```python
import math
import jax
import jax.numpy as jnp
from jax import lax
import numpy as np

D_MODEL = 2048
BATCH = 2
SEQ = 4096
DEPTH = 2

GRID_W = 64
CTX_LEN = 256
NORM_EPS = 1e-6
N_BRANCH = 4
MIX_WIDTH = 768

S5_WIDTH = MIX_WIDTH
S5_GROUP = 16
S5_GROUPS = S5_WIDTH // S5_GROUP
S5_STATE = 64
SSD_WIDTH = MIX_WIDTH
SSD_HEAD_DIM = 64
SSD_HEADS = SSD_WIDTH // SSD_HEAD_DIM
SSD_GROUPS = 2
SSD_STATE = 64
SSD_CONV = 3
SSD_CHUNK = 128
SSD_CONV_CH = SSD_WIDTH + 2 * SSD_GROUPS * SSD_STATE
GLA_WIDTH = MIX_WIDTH
GLA_HEADS = 6
GLA_KEY_WIDTH = GLA_WIDTH // 2
GLA_DK = GLA_KEY_WIDTH // GLA_HEADS
GLA_DV = GLA_WIDTH // GLA_HEADS
GLA_GATE_RANK = 16
GLA_TAU = 16.0
GLA_CHUNK = 64
HY_WIDTH = MIX_WIDTH
HY_ORDER = 2
HY_SHORT = 3
HY_FILTER_DIM = 64
HY_BANDS = 16
HY_EMB = 2 * HY_BANDS + 1
HY_MAX_DECAY = math.log(1e-2) / 0.3
HY_MIN_DECAY = math.log(1e-2) / 1.5
MOE_GROUPS = 4
MOE_PER_GROUP = 4
MOE_EXPERTS = MOE_GROUPS * MOE_PER_GROUP
MOE_TOPK = 2
MOE_FF = 1024

IN_SIZES = (S5_WIDTH,
            SSD_WIDTH,
            SSD_CONV_CH,
            2 * SSD_HEADS,
            GLA_KEY_WIDTH, GLA_KEY_WIDTH, GLA_WIDTH, GLA_WIDTH,
            2 * GLA_GATE_RANK,
            (HY_ORDER + 1) * HY_WIDTH,
            N_BRANCH * D_MODEL)
IN_COLS = sum(IN_SIZES)

kernel_name = 'hybrid_s5_ssd_gla_hyena_hmoe_dit'

F32 = jnp.float32


def rmsnorm(x):
    xf = x.astype(F32)
    return xf * lax.rsqrt(jnp.mean(xf * xf, axis=-1, keepdims=True) + NORM_EPS)


def modulate(x, shift, scale):
    return rmsnorm(x) * (1.0 + scale) + shift


def split_cols(p, sizes):
    return jnp.split(p, np.cumsum(sizes)[:-1].tolist(), axis=-1)


def flip_seq(t, rev):
    return jnp.flip(t, axis=1) if rev else t


def dwconv(u, w, b):
    k = w.shape[0]
    y = lax.conv_general_dilated(u.astype(F32), w.astype(F32)[:, None, :], window_strides=(1,),
                                 padding=[(k // 2, k // 2)], dimension_numbers=('NWC', 'WIO', 'NWC'),
                                 feature_group_count=u.shape[-1])
    return y + b.astype(F32)


def to_colmajor(t, rows):
    b, n, ch = t.shape
    return t.reshape(b, rows, GRID_W, ch).transpose(0, 2, 1, 3).reshape(b, n, ch)


def from_colmajor(t, rows):
    b, n, ch = t.shape
    return t.reshape(b, GRID_W, rows, ch).transpose(0, 2, 1, 3).reshape(b, n, ch)


def cmul(ar, ai, br, bi):
    return ar * br - ai * bi, ar * bi + ai * br


def s5_combine(e1, e2):
    a1r, a1i, b1r, b1i = e1
    a2r, a2i, b2r, b2i = e2
    ar, ai = cmul(a2r, a2i, a1r, a1i)
    br, bi = cmul(a2r, a2i, b1r, b1i)
    return ar, ai, br + b2r, bi + b2i


def s5_scan(u, lam_re, lam_im, log_step, b_re, b_im, c_re, c_im, h0_re, h0_im, reverse):
    lam_re = lam_re.astype(F32)
    lam_im = lam_im.astype(F32)
    step = jnp.exp(log_step.astype(F32))[:, None]
    mag = jnp.exp(lam_re * step)
    ab_re = mag * jnp.cos(lam_im * step)
    ab_im = mag * jnp.sin(lam_im * step)
    den = lam_re * lam_re + lam_im * lam_im
    zr, zi = cmul(ab_re - 1.0, ab_im, lam_re, -lam_im)
    zr, zi = zr / den, zi / den
    bb_re, bb_im = cmul(zr[..., None], zi[..., None], b_re.astype(F32), b_im.astype(F32))
    bu_re = jnp.einsum('blgh,gph->blgp', u, bb_re)
    bu_im = jnp.einsum('blgh,gph->blgp', u, bb_im)
    first, final = (-1, 0) if reverse else (0, -1)
    ir, ii = cmul(ab_re, ab_im, h0_re, h0_im)
    bu_re = bu_re.at[:, first].add(ir)
    bu_im = bu_im.at[:, first].add(ii)
    elems = (jnp.broadcast_to(ab_re, bu_re.shape), jnp.broadcast_to(ab_im, bu_im.shape), bu_re, bu_im)
    _, _, h_re, h_im = lax.associative_scan(s5_combine, elems, reverse=reverse, axis=1)
    y = jnp.einsum('blgp,ghp->blgh', h_re, c_re.astype(F32)) - jnp.einsum('blgp,ghp->blgh', h_im, c_im.astype(F32))
    return y, h_re[:, final], h_im[:, final]


def s5_mixer(u_c, u_l, lam_re, lam_im, log_step, b_re, b_im, c_re, c_im, d_skip, glu_w):
    def grp(u):
        return u.astype(F32).reshape(u.shape[0], u.shape[1], S5_GROUPS, S5_GROUP)
    uc, ul = grp(u_c), grp(u_l)
    zero = jnp.zeros((uc.shape[0], S5_GROUPS, S5_STATE), F32)
    ys_c, ys_l = [], []
    for d, rev in enumerate((False, True)):
        prm = (lam_re[d], lam_im[d], log_step[d], b_re[d], b_im[d], c_re[d], c_im[d])
        yc, hr, hi = s5_scan(uc, *prm, zero, zero, rev)
        yl, _, _ = s5_scan(ul, *prm, hr, hi, rev)
        ys_c.append(yc)
        ys_l.append(yl)

    def out(y, u):
        y = y.reshape(u.shape[0], u.shape[1], S5_WIDTH) + d_skip.astype(F32) * u.astype(F32)
        g = jax.nn.gelu(y)
        return g * jax.nn.sigmoid(g @ glu_w)
    return out(ys_c[0] + ys_c[1], u_c), out(ys_l[0] + ys_l[1], u_l)


def segsum(a):
    t = a.shape[-1]
    cs = jnp.cumsum(a, axis=-1)
    diff = cs[..., :, None] - cs[..., None, :]
    return jnp.where(jnp.tril(jnp.ones((t, t), dtype=bool)), diff, -jnp.inf)


def ssd_chunked(xs, dt, a, bm, cm, init):
    b, n, h, p = xs.shape
    g, ns = bm.shape[2], bm.shape[3]
    q = SSD_CHUNK
    c = n // q
    xc = (xs * dt[..., None]).reshape(b, c, q, h, p)
    adt = jnp.moveaxis((a * dt).reshape(b, c, q, h), 3, 1)
    bc = jnp.repeat(bm.reshape(b, c, q, g, ns), h // g, axis=3)
    cc = jnp.repeat(cm.reshape(b, c, q, g, ns), h // g, axis=3)
    a_cs = jnp.cumsum(adt, axis=-1)
    lmat = jnp.exp(segsum(adt))
    y_diag = jnp.einsum('bclhn,bcshn,bhcls,bcshp->bclhp', cc, bc, lmat, xc)
    decay_states = jnp.exp(a_cs[..., -1:] - a_cs)
    states = jnp.einsum('bclhn,bhcl,bclhp->bchpn', bc, decay_states, xc)
    states = jnp.concatenate([init[:, None], states], axis=1)
    chunk_decay = jnp.exp(segsum(jnp.pad(a_cs[..., -1], ((0, 0), (0, 0), (1, 0)))))
    states = jnp.einsum('bhzc,bchpn->bzhpn', chunk_decay, states)
    y_off = jnp.einsum('bclhn,bchpn,bhcl->bclhp', cc, states[:, :-1], jnp.exp(a_cs))
    return (y_diag + y_off).reshape(b, n, h, p), states[:, -1]


def ssd_mixer(z_c, xbc_c, dt_c, z_l, xbc_l, dt_l, conv_w, conv_b, a_log, dt_bias, d_skip, norm_w, rows):
    z_l, xbc_l, dt_l = to_colmajor(z_l, rows), to_colmajor(xbc_l, rows), to_colmajor(dt_l, rows)

    def prep(xbc, dt_raw):
        xbc = jax.nn.silu(dwconv(xbc, conv_w, conv_b))
        xs, bm, cm = split_cols(xbc, (SSD_WIDTH, SSD_GROUPS * SSD_STATE, SSD_GROUPS * SSD_STATE))
        b, n = xs.shape[:2]
        dt = jax.nn.softplus(dt_raw.astype(F32).reshape(b, n, 2, SSD_HEADS) + dt_bias.astype(F32))
        return (xs.reshape(b, n, SSD_HEADS, SSD_HEAD_DIM), bm.reshape(b, n, SSD_GROUPS, SSD_STATE),
                cm.reshape(b, n, SSD_GROUPS, SSD_STATE), dt)
    xc, bc, cc, dtc = prep(xbc_c, dt_c)
    xl, bl, cl, dtl = prep(xbc_l, dt_l)
    dsk = d_skip.astype(F32)[:, None]
    y_c = dsk * xc
    y_l = dsk * xl
    zero = jnp.zeros((xc.shape[0], SSD_HEADS, SSD_HEAD_DIM, SSD_STATE), F32)
    for d in range(2):
        rev = d == 1
        a = -jnp.exp(a_log[d].astype(F32))
        yc, sc = ssd_chunked(flip_seq(xc, rev), flip_seq(dtc[:, :, d], rev), a, flip_seq(bc, rev), flip_seq(cc, rev), zero)
        yl, _ = ssd_chunked(flip_seq(xl, rev), flip_seq(dtl[:, :, d], rev), a, flip_seq(bl, rev), flip_seq(cl, rev), sc)
        y_c = y_c + flip_seq(yc, rev)
        y_l = y_l + flip_seq(yl, rev)

    def out(y, z):
        y = y.reshape(y.shape[0], y.shape[1], SSD_WIDTH) * jax.nn.silu(z.astype(F32))
        return rmsnorm(y) * norm_w
    return out(y_c, z_c), from_colmajor(out(y_l, z_l), rows)


def gla_chunked(q, k, v, la, init):
    b, n, h, dk = q.shape
    dv = v.shape[-1]
    qs = GLA_CHUNK
    c = n // qs
    q, k, la = (t.reshape(b, c, qs, h, dk) for t in (q, k, la))
    v = v.reshape(b, c, qs, h, dv)
    bcum = jnp.cumsum(la, axis=2)
    b_last = bcum[:, :, -1]
    q_dec = q * jnp.exp(bcum)
    k_inv = k * jnp.exp(-bcum)
    scores = jnp.einsum('bcthd,bcshd->bchts', q_dec, k_inv)
    scores = jnp.where(jnp.tril(jnp.ones((qs, qs), dtype=bool)), scores, 0.0)
    o_intra = jnp.einsum('bchts,bcshv->bcthv', scores, v)
    k_end = k * jnp.exp(b_last[:, :, None] - bcum)
    d_state = jnp.einsum('bcshd,bcshv->bchdv', k_end, v)
    gamma = jnp.exp(b_last)

    def step(s, inp):
        g_c, ds_c = inp
        return g_c[..., None] * s + ds_c, s
    final, s_prev = lax.scan(step, init, (jnp.moveaxis(gamma, 1, 0), jnp.moveaxis(d_state, 1, 0)))
    o_inter = jnp.einsum('bcthd,cbhdv->bcthv', q_dec, s_prev)
    return (o_intra + o_inter).reshape(b, n, h, dv), final


def gla_mixer(q_c, k_c, v_c, g_c, r_c, q_l, k_l, v_l, g_l, r_l, gate_w, gate_b, norm_w):
    def heads(t, dh):
        return t.astype(F32).reshape(t.shape[0], t.shape[1], GLA_HEADS, dh)

    def log_gates(r):
        r = r.astype(F32)
        return [heads(jax.nn.log_sigmoid(r[..., d * GLA_GATE_RANK:(d + 1) * GLA_GATE_RANK] @ gate_w[d] + gate_b[d]) / GLA_TAU, GLA_DK)
                for d in range(2)]
    qc, kc, vc, lac = heads(q_c, GLA_DK) * GLA_DK ** -0.5, heads(k_c, GLA_DK), heads(v_c, GLA_DV), log_gates(r_c)
    ql, kl, vl, lal = heads(q_l, GLA_DK) * GLA_DK ** -0.5, heads(k_l, GLA_DK), heads(v_l, GLA_DV), log_gates(r_l)
    zero = jnp.zeros((qc.shape[0], GLA_HEADS, GLA_DK, GLA_DV), F32)
    os_c, os_l = [], []
    for d in range(2):
        rev = d == 1
        oc, sc = gla_chunked(flip_seq(qc, rev), flip_seq(kc, rev), flip_seq(vc, rev), flip_seq(lac[d], rev), zero)
        ol, _ = gla_chunked(flip_seq(ql, rev), flip_seq(kl, rev), flip_seq(vl, rev), flip_seq(lal[d], rev), sc)
        os_c.append(flip_seq(oc, rev))
        os_l.append(flip_seq(ol, rev))

    def out(o, g):
        o = (rmsnorm(o) * norm_w).reshape(o.shape[0], o.shape[1], GLA_WIDTH)
        return o * jax.nn.silu(g.astype(F32))
    return out(os_c[0] + os_c[1], g_c), out(os_l[0] + os_l[1], g_l)


def hyena_filters(n, w1, b1, f1, w2, b2, f2, w3):
    t = jnp.linspace(0.0, 1.0, n, dtype=F32)[:, None]
    freqs = jnp.linspace(1e-4, HY_BANDS - 1, HY_BANDS, dtype=F32)
    ang = (2.0 * math.pi / n) * jnp.arange(n, dtype=F32)[:, None] * freqs[None, :]
    emb = jnp.concatenate([t, jnp.cos(ang), -jnp.sin(ang)], axis=-1)
    h = jnp.sin(f1.astype(F32) * (emb @ w1.astype(F32) + b1.astype(F32)))
    h = jnp.sin(f2.astype(F32) * (h @ w2.astype(F32) + b2.astype(F32)))
    h = (h @ w3.astype(F32)).reshape(n, HY_ORDER, 2, HY_WIDTH)
    deltas = jnp.abs(jnp.linspace(HY_MIN_DECAY, HY_MAX_DECAY, HY_WIDTH, dtype=F32))
    return h * jnp.exp(-t[:, :, None, None] * deltas)


def bidir_fftconv(u, h_f, h_b, bias):
    n = u.shape[1]
    circ = jnp.concatenate([h_f, jnp.zeros((1, h_f.shape[1]), F32), h_b[:0:-1]], axis=0)
    spec = jnp.fft.rfft(u, n=2 * n, axis=1) * jnp.fft.rfft(circ, axis=0)
    return jnp.fft.irfft(spec, n=2 * n, axis=1)[:, :n] + bias.astype(F32) * u


def hyena_stream(p, conv_w, conv_b, w1, b1, f1, w2, b2, f2, w3, bias):
    u = dwconv(p, conv_w, conv_b)
    v, x1, x2 = jnp.split(u, 3, axis=-1)
    filt = hyena_filters(p.shape[1], w1, b1, f1, w2, b2, f2, w3)
    y = v
    for o, gate in enumerate((x1, x2)):
        y = gate * bidir_fftconv(y, filt[:, o, 0], filt[:, o, 1], bias[o])
    return y


def merge_branches(ya, yb, yc, yd, gate_logits, w_branch, w_out):
    b, n, _ = gate_logits.shape
    gates = jax.nn.sigmoid(gate_logits.astype(F32)).reshape(b, n, N_BRANCH, D_MODEL)
    merged = gates[:, :, 0] * (ya @ w_branch[0])
    merged = merged + gates[:, :, 1] * (yb @ w_branch[1])
    merged = merged + gates[:, :, 2] * (yc @ w_branch[2])
    merged = merged + gates[:, :, 3] * (yd @ w_branch[3])
    return merged @ w_out


def hier_moe(h, group_w, group_b, expert_w, expert_b, w_gate, w_up, w_down):
    t = h.shape[0]
    glog = (h @ group_w + group_b).astype(F32)
    g_idx = jnp.argmax(glog, axis=-1)
    p_g = jnp.take_along_axis(jax.nn.softmax(glog, axis=-1), g_idx[:, None], axis=-1)
    elog = (h @ expert_w + expert_b).astype(F32).reshape(t, MOE_GROUPS, MOE_PER_GROUP)
    elog = jnp.take_along_axis(elog, g_idx[:, None, None], axis=1)[:, 0]
    top_v, top_i = lax.top_k(elog, MOE_TOPK)
    top_w = jax.nn.softmax(top_v, axis=-1) * p_g
    within = jnp.sum(jax.nn.one_hot(top_i, MOE_PER_GROUP, dtype=F32) * top_w[..., None], axis=1)
    combine = (jax.nn.one_hot(g_idx, MOE_GROUPS, dtype=F32)[:, :, None] * within[:, None, :]).reshape(t, MOE_EXPERTS)
    out = jnp.zeros((t, D_MODEL), F32)
    for e in range(MOE_EXPERTS):
        act = jax.nn.silu(h @ w_gate[e]) * (h @ w_up[e])
        out = out + combine[:, e:e + 1] * (act @ w_down[e])
    return out


def setup_inputs(seed: int = 0) -> dict:
    key = jax.random.key(seed)
    ks = iter(jax.random.split(key, 64))

    def nrm(shape, scale):
        return jax.random.normal(next(ks), shape, F32) * scale

    def uni(shape, lo, hi):
        return jax.random.uniform(next(ks), shape, F32, lo, hi)
    g5, p5, h5 = S5_GROUPS, S5_STATE, S5_GROUP
    dt0 = jnp.exp(uni((DEPTH, 2, SSD_HEADS), math.log(1e-3), math.log(1e-1)))
    return {
        'x': nrm((BATCH, SEQ, D_MODEL), 1.0),
        'c': nrm((BATCH, D_MODEL), 1.0),
        'ctx': nrm((BATCH, CTX_LEN, D_MODEL), 1.0),
        'c_ctx': nrm((D_MODEL,), 1.0),
        'ada_w': nrm((DEPTH, D_MODEL, 6 * D_MODEL), 0.5 * D_MODEL ** -0.5),
        'ada_b': nrm((DEPTH, 6 * D_MODEL), 0.02),
        'w_in': nrm((DEPTH, D_MODEL, IN_COLS), D_MODEL ** -0.5),
        's5_lambda_re': -0.5 + nrm((DEPTH, 2, g5, p5), 0.01),
        's5_lambda_im': math.pi * jnp.arange(p5, dtype=F32) + nrm((DEPTH, 2, g5, p5), 0.01),
        's5_log_step': uni((DEPTH, 2, g5), math.log(1e-3), math.log(1e-1)),
        's5_b_re': nrm((DEPTH, 2, g5, p5, h5), (2 * h5) ** -0.5),
        's5_b_im': nrm((DEPTH, 2, g5, p5, h5), (2 * h5) ** -0.5),
        's5_c_re': nrm((DEPTH, 2, g5, h5, p5), (2 * p5) ** -0.5 * 4.0),
        's5_c_im': nrm((DEPTH, 2, g5, h5, p5), (2 * p5) ** -0.5 * 4.0),
        's5_d': nrm((DEPTH, S5_WIDTH), 1.0),
        's5_glu_w': nrm((DEPTH, S5_WIDTH, S5_WIDTH), S5_WIDTH ** -0.5),
        'ssd_conv_w': nrm((DEPTH, SSD_CONV, SSD_CONV_CH), SSD_CONV ** -0.5),
        'ssd_conv_b': nrm((DEPTH, SSD_CONV_CH), 0.02),
        'ssd_a_log': jnp.log(uni((DEPTH, 2, SSD_HEADS), 1.0, 16.0)),
        'ssd_dt_bias': dt0 + jnp.log(-jnp.expm1(-dt0)),
        'ssd_d': 1.0 + nrm((DEPTH, SSD_HEADS), 0.1),
        'ssd_norm_w': 1.0 + nrm((DEPTH, SSD_WIDTH), 0.1),
        'gla_gate_w': nrm((DEPTH, 2, GLA_GATE_RANK, GLA_KEY_WIDTH), GLA_GATE_RANK ** -0.5),
        'gla_gate_b': nrm((DEPTH, 2, GLA_KEY_WIDTH), 0.1),
        'gla_norm_w': 1.0 + nrm((DEPTH, GLA_DV), 0.1),
        'hy_conv_w': nrm((DEPTH, HY_SHORT, (HY_ORDER + 1) * HY_WIDTH), HY_SHORT ** -0.5),
        'hy_conv_b': nrm((DEPTH, (HY_ORDER + 1) * HY_WIDTH), 0.02),
        'hy_w1': nrm((DEPTH, HY_EMB, HY_FILTER_DIM), HY_EMB ** -0.5),
        'hy_b1': nrm((DEPTH, HY_FILTER_DIM), 0.1),
        'hy_freq1': 1.0 + nrm((DEPTH, HY_FILTER_DIM), 0.1),
        'hy_w2': nrm((DEPTH, HY_FILTER_DIM, HY_FILTER_DIM), HY_FILTER_DIM ** -0.5),
        'hy_b2': nrm((DEPTH, HY_FILTER_DIM), 0.1),
        'hy_freq2': 1.0 + nrm((DEPTH, HY_FILTER_DIM), 0.1),
        'hy_w3': nrm((DEPTH, HY_FILTER_DIM, HY_ORDER * 2 * HY_WIDTH), 0.05 * HY_FILTER_DIM ** -0.5),
        'hy_bias': nrm((DEPTH, HY_ORDER, HY_WIDTH), 0.5),
        'w_branch': nrm((DEPTH, N_BRANCH, MIX_WIDTH, D_MODEL), MIX_WIDTH ** -0.5),
        'w_out': nrm((DEPTH, D_MODEL, D_MODEL), D_MODEL ** -0.5),
        'moe_group_w': nrm((DEPTH, D_MODEL, MOE_GROUPS), D_MODEL ** -0.5),
        'moe_group_b': nrm((DEPTH, MOE_GROUPS), 0.01),
        'moe_expert_w': nrm((DEPTH, D_MODEL, MOE_EXPERTS), D_MODEL ** -0.5),
        'moe_expert_b': nrm((DEPTH, MOE_EXPERTS), 0.01),
        'moe_w_gate': nrm((DEPTH, MOE_EXPERTS, D_MODEL, MOE_FF), D_MODEL ** -0.5),
        'moe_w_up': nrm((DEPTH, MOE_EXPERTS, D_MODEL, MOE_FF), D_MODEL ** -0.5),
        'moe_w_down': nrm((DEPTH, MOE_EXPERTS, MOE_FF, D_MODEL), MOE_FF ** -0.5),
        'final_norm_w': 1.0 + nrm((D_MODEL,), 0.1),
    }


def reference(x, c, ctx, c_ctx, ada_w, ada_b, w_in, s5_lambda_re, s5_lambda_im, s5_log_step, s5_b_re, s5_b_im,
              s5_c_re, s5_c_im, s5_d, s5_glu_w, ssd_conv_w, ssd_conv_b, ssd_a_log, ssd_dt_bias, ssd_d, ssd_norm_w,
              gla_gate_w, gla_gate_b, gla_norm_w, hy_conv_w, hy_conv_b, hy_w1, hy_b1, hy_freq1, hy_w2, hy_b2,
              hy_freq2, hy_w3, hy_bias, w_branch, w_out, moe_group_w, moe_group_b, moe_expert_w, moe_expert_b,
              moe_w_gate, moe_w_up, moe_w_down, final_norm_w):
    n_ctx = ctx.shape[1]
    rows = x.shape[1] // GRID_W
    x_lat = x
    x_ctx = ctx
    for li in range(DEPTH):
        last = li == DEPTH - 1
        mod = jax.nn.silu(c) @ ada_w[li] + ada_b[li]
        mod_c = jax.nn.silu(c_ctx) @ ada_w[li] + ada_b[li]
        sh1, sc1, g1, sh2, sc2, g2 = jnp.split(mod[:, None, :], 6, axis=-1)
        csh1, csc1, cg1, csh2, csc2, cg2 = jnp.split(mod_c, 6)
        h = jnp.concatenate([modulate(x_ctx, csh1, csc1), modulate(x_lat, sh1, sc1)], axis=1)
        p = h @ w_in[li]
        ac, zc, xbcc, dtc, qc, kc, vc, gc, rc, hc, gtc = split_cols(p[:, :n_ctx], IN_SIZES)
        al, zl, xbcl, dtl, ql, kl, vl, gl, rl, hl, gtl = split_cols(p[:, n_ctx:], IN_SIZES)
        ya_c, ya_l = s5_mixer(ac, al, s5_lambda_re[li], s5_lambda_im[li], s5_log_step[li], s5_b_re[li], s5_b_im[li],
                              s5_c_re[li], s5_c_im[li], s5_d[li], s5_glu_w[li])
        yb_c, yb_l = ssd_mixer(zc, xbcc, dtc, zl, xbcl, dtl, ssd_conv_w[li], ssd_conv_b[li], ssd_a_log[li],
                               ssd_dt_bias[li], ssd_d[li], ssd_norm_w[li], rows)
        yc_c, yc_l = gla_mixer(qc, kc, vc, gc, rc, ql, kl, vl, gl, rl, gla_gate_w[li], gla_gate_b[li], gla_norm_w[li])
        hy_args = (hy_conv_w[li], hy_conv_b[li], hy_w1[li], hy_b1[li], hy_freq1[li], hy_w2[li], hy_b2[li],
                   hy_freq2[li], hy_w3[li], hy_bias[li])
        yd_l = hyena_stream(hl, *hy_args)
        x_lat = x_lat + g1 * merge_branches(ya_l, yb_l, yc_l, yd_l, gtl, w_branch[li], w_out[li])
        if not last:
            yd_c = hyena_stream(hc, *hy_args)
            x_ctx = x_ctx + cg1 * merge_branches(ya_c, yb_c, yc_c, yd_c, gtc, w_branch[li], w_out[li])
            h2 = jnp.concatenate([modulate(x_ctx, csh2, csc2), modulate(x_lat, sh2, sc2)], axis=1)
        else:
            h2 = modulate(x_lat, sh2, sc2)
        y2 = hier_moe(h2.reshape(-1, D_MODEL), moe_group_w[li], moe_group_b[li], moe_expert_w[li], moe_expert_b[li],
                      moe_w_gate[li], moe_w_up[li], moe_w_down[li]).reshape(h2.shape)
        if not last:
            x_ctx = x_ctx + cg2 * y2[:, :n_ctx]
            x_lat = x_lat + g2 * y2[:, n_ctx:]
        else:
            x_lat = x_lat + g2 * y2
    return rmsnorm(x_lat) * final_norm_w
```

```python
import math
from contextlib import ExitStack
import numpy as np
import ml_dtypes
import concourse.bass as bass
import concourse.mybir as mybir
from concourse.bass_utils import run_bass_kernel_spmd

F32 = mybir.dt.float32
BF16 = mybir.dt.bfloat16
I32 = mybir.dt.int32
AF = mybir.ActivationFunctionType
ALU = mybir.AluOpType
AX = mybir.AxisListType

D = 2048
B = 2
S = 4096
DEPTH = 2
NCTX = 256
T = NCTX + S
EPS = 1e-6
NCORES = 8


class _Op:
    pass


class Tile:
    def __init__(self, ap, name):
        self.ap = ap
        self.name = name

    def __getitem__(self, idx):
        return self.ap[idx]


class _Rec:
    def __init__(self):
        self.call = None

    def __getattr__(self, name):
        def f(*a, **kw):
            self.call = (name, a, kw)
            return self
        return f


class KB:
    COMPUTE = ("pe", "dve", "act", "pool")

    def __init__(self, nc, n_dma_sems=20):
        self.nc = nc
        self.es = ExitStack()
        self.eng = {"pe": nc.tensor, "dve": nc.vector, "act": nc.scalar, "pool": nc.gpsimd, "sp": nc.sync}
        self.ops = []
        self.lastw = {}
        self.reads = {}
        self.n_dma_sems = n_dma_sems
        self._uid = 0
        self.psum_banks = None
        self.bar_from = 0

    ARENA_WORDS = 52500

    def _arena(self):
        if getattr(self, "arena", None) is None:
            self.arena = self.es.enter_context(self.nc.sbuf_tensor("arena", [128, self.ARENA_WORDS], F32))
            self.top = 0
            self.psum = [self.es.enter_context(self.nc.psum_tensor(f"psb{i}", [128, 512], F32)) for i in range(8)]
            self.psn = 0
        return self.arena

    def sb(self, shape, dtype=F32, name=None):
        ar = self._arena()
        self._uid += 1
        name = f"{name or 't'}_{self._uid}"
        P = shape[0]
        n = int(np.prod(shape[1:]))
        esz = 2 if dtype == BF16 else 4
        words = (n * esz + 3) // 4
        assert self.top + words <= self.ARENA_WORDS, f"arena overflow allocating {name} {shape}: top={self.top}"
        ap = ar[0:P, self.top:self.top + words]
        self.top += words
        if dtype != F32:
            ap = ap.bitcast(dtype)
        if esz == 2 and (n % 2):
            ap = ap[:, 0:n]
        if len(shape) == 3:
            ap = ap.rearrange("p (a b) -> p a b", a=shape[1])
        elif len(shape) == 4:
            ap = ap.rearrange("p (a b c) -> p a b c", a=shape[1], b=shape[2])
        return Tile(ap, name)

    def ps(self, shape=(128, 512), dtype=F32, name=None):
        self._arena()
        t = self.psum[self.psn % 8]
        self.psn += 1
        assert self.psn <= 8, "only 8 PSUM banks"
        return t

    def mark(self):
        self._arena()
        return (self.top, self.psn)

    def release(self, mark):
        self.barrier()
        self.top, self.psn = mark

    def barrier(self):
        last = {}
        dmas = []
        for o in self.ops[self.bar_from:]:
            if o.isdma:
                dmas.append(o)
            else:
                last[o.eng] = o
        deps = list(last.values()) + dmas
        for e in ("pe", "dve", "act", "pool", "sp"):
            o = self.op(e, lambda en: en.nop())
            o.deps = list(deps)
        self.bar_from = len(self.ops)
        self.lastw = {}
        self.reads = {}

    def dram(self, name, shape, dtype=F32, kind="Internal"):
        return self.nc.dram_tensor(name, list(shape), dtype, kind=kind)

    @staticmethod
    def _key(t):
        return t if isinstance(t, str) else t.name

    def op(self, eng, fn, r=(), w=()):
        o = _Op()
        o.eng = eng
        rec = _Rec()
        fn(rec)
        name_, a_, kw_ = rec.call
        o.fn = lambda e: getattr(e, name_)(*a_, **kw_)
        o.needed = False
        o.sig = None
        o.isdma = False
        o.seq = len(self.ops)
        deps = []
        for t in r:
            k = self._key(t)
            lw = self.lastw.get(k)
            if lw is not None:
                deps.append(lw)
        for t in w:
            k = self._key(t)
            lw = self.lastw.get(k)
            if lw is not None:
                deps.append(lw)
            deps.extend(self.reads.get(k, ()))
        dd = []
        seen = set()
        for d in deps:
            if d.seq in seen:
                continue
            seen.add(d.seq)
            if eng == "pe" and d.eng == "pe" and not d.isdma:
                continue
            dd.append(d)
        o.deps = dd
        for t in w:
            k = self._key(t)
            self.lastw[k] = o
            self.reads[k] = []
        for t in r:
            k = self._key(t)
            self.reads.setdefault(k, []).append(o)
        self.ops.append(o)
        return o

    def dma(self, out, in_, r=(), w=(), q="sp", **kw):
        o = self.op(q, lambda e: e.dma_start(out=out, in_=in_, **kw), r=r, w=w)
        o.isdma = True
        return o

    def emit(self, final_wait=()):
        nc = self.nc
        for o in self.ops:
            for d in o.deps:
                d.needed = True
        sems = {e: self.es.enter_context(nc.semaphore(f"s_{e}")) for e in self.COMPUTE}
        sigcnt = {e: 0 for e in self.COMPUTE}
        queues = sorted({o.eng for o in self.ops if o.isdma})
        dsems = {q: [self.es.enter_context(nc.semaphore(f"d_{q}{i}")) for i in range(self.n_dma_sems)] for q in queues}
        dcnt = {q: [0] * self.n_dma_sems for q in queues}
        dnext = {q: 0 for q in queues}
        waited = {}
        all_dma = []

        def wait(engname, sem, key, val):
            if waited.get((engname, key), 0) >= val:
                return
            self.eng[engname].wait_ge(sem, val)
            waited[(engname, key)] = val

        for o in self.ops:
            e = self.eng[o.eng]
            for d in o.deps:
                if d.isdma:
                    wait(o.eng, d.dsem, ("d", d.eng, d.dslot), d.dval)
                else:
                    wait(o.eng, sems[d.eng], ("c", d.eng), d.sig)
            if o.isdma:
                q = o.eng
                slot = dnext[q]
                dnext[q] = (slot + 1) % self.n_dma_sems
                sem = dsems[q][slot]
                if dcnt[q][slot] > 0:
                    wait(o.eng, sem, ("d", q, slot), dcnt[q][slot])
                dcnt[q][slot] += 16
                o.dsem = sem
                o.dslot = slot
                o.dval = dcnt[q][slot]
                o.fn(e).then_inc(sem, 16)
                all_dma.append(o)
            else:
                ins = o.fn(e)
                if o.needed:
                    sigcnt[o.eng] += 1
                    o.sig = sigcnt[o.eng]
                    ins.then_inc(sems[o.eng], 1)
        for q in queues:
            for slot in range(self.n_dma_sems):
                if dcnt[q][slot] > 0:
                    wait("sp", dsems[q][slot], ("d", q, slot), dcnt[q][slot])


def _run(nc, in_maps):
    res = run_bass_kernel_spmd(nc, in_maps, core_ids=list(range(NCORES)))
    return res.results


MODC = 6 * D // NCORES


def build_mods():
    nc = bass.Bass("TRN2", target_bir_lowering=False)
    k = KB(nc)
    cinT = nc.dram_tensor("cinT", [D, 3], F32, kind="ExternalInput").ap()
    aw = nc.dram_tensor("aw", [DEPTH, D, MODC], F32, kind="ExternalInput").ap()
    ab = nc.dram_tensor("ab", [DEPTH, 1, MODC], F32, kind="ExternalInput").ap()
    out = nc.dram_tensor("mod", [DEPTH, 3, MODC], F32, kind="ExternalOutput").ap()
    cs = k.sb([128, 16, 3], F32, "cs")
    ones = k.sb([1, 4], F32, "ones")
    wt = [k.sb([128, 16, 512], F32, f"wt{i}") for i in range(2)]
    bt = k.sb([1, DEPTH, MODC], F32, "bt")
    ot = [k.sb([3, 512], F32, f"ot{i}") for i in range(2)]
    pst = [k.ps() for _ in range(2)]
    k.dma(cs[:], cinT.rearrange("(k p) r -> p k r", p=128), w=[cs])
    k.dma(bt[:], ab.rearrange("l o c -> o l c"), w=[bt])
    k.op("dve", lambda e: e.memset(ones[:], 1.0), w=[ones])
    k.op("act", lambda e: e.activation(out=cs[:], in_=cs[:], func=AF.Silu), r=[cs], w=[cs])
    it = 0
    for li in range(DEPTH):
        for cc in range(MODC // 512):
            w_ = wt[it % 2]
            p_ = pst[it % 2]
            o_ = ot[it % 2]
            it += 1
            k.dma(w_[:], aw[li, :, cc * 512:(cc + 1) * 512].rearrange("(k p) c -> p k c", p=128), w=[w_])
            for kk in range(16):
                k.op("pe", lambda e, w_=w_, p_=p_, kk=kk: e.matmul(p_[0:3, :], lhsT=cs[:, kk, :], rhs=w_[:, kk, :],
                                                                   start=(kk == 0), stop=False), r=[cs, w_], w=[p_])
            k.op("pe", lambda e, p_=p_, li=li, cc=cc: e.matmul(p_[0:3, :], lhsT=ones[0:1, 0:3],
                                                              rhs=bt[0:1, li, cc * 512:(cc + 1) * 512],
                                                              start=False, stop=True), r=[ones, bt], w=[p_])
            k.op("dve", lambda e, p_=p_, o_=o_: e.tensor_copy(out=o_[:], in_=p_[0:3, :]), r=[p_], w=[o_])
            k.dma(out[li, :, cc * 512:(cc + 1) * 512], o_[:], r=[o_], q="pool")
    k.emit()
    return nc


def run_mods(c, c_ctx, ada_w, ada_b):
    cinT = np.ascontiguousarray(np.concatenate([c, c_ctx[None]], 0).T)
    nc = build_mods()
    maps = []
    for i in range(NCORES):
        sl = slice(i * MODC, (i + 1) * MODC)
        maps.append({"cinT": cinT, "aw": np.ascontiguousarray(ada_w[:, :, sl]),
                     "ab": np.ascontiguousarray(ada_b[:, None, sl])})
    res = _run(nc, maps)
    return np.concatenate([r["mod"] for r in res], axis=2)


IN_SIZES = (768, 768, 1024, 24, 384, 384, 768, 768, 32, 2304, 4 * D)
IN_OFF = np.concatenate([[0], np.cumsum(IN_SIZES)]).astype(int)
(O_S5, O_Z, O_XBC, O_DT, O_Q, O_K, O_V, O_G, O_R, O_HY, O_GT) = [int(v) for v in IN_OFF[:11]]


def core_cols(j):
    cols = []
    seg = {}

    def add(name, lst):
        seg[name] = (len(cols), len(lst))
        cols.extend(lst)

    def pad():
        while len(cols) % 128:
            cols.append(0)
    add("s5u", list(range(O_S5 + 192 * j, O_S5 + 192 * (j + 1))))
    heads = [j, j + 4 if j < 2 else j]
    for hi, h in enumerate(heads):
        add(f"q{hi}", list(range(O_Q + 64 * h, O_Q + 64 * (h + 1))))
        add(f"k{hi}", list(range(O_K + 64 * h, O_K + 64 * (h + 1))))
        add(f"v{hi}", list(range(O_V + 128 * h, O_V + 128 * (h + 1))))
        add(f"g{hi}", list(range(O_G + 128 * h, O_G + 128 * (h + 1))))
    add("r", list(range(O_R, O_R + 32)))
    for i, nm in enumerate(("hv", "hx1", "hx2")):
        add(nm, list(range(O_HY + 768 * i + 192 * j, O_HY + 768 * i + 192 * (j + 1))))
    pad()
    seg["ssd_start"] = (len(cols), 0)
    add("z", list(range(O_Z + 192 * j, O_Z + 192 * (j + 1))))
    add("x", list(range(O_XBC + 192 * j, O_XBC + 192 * (j + 1))))
    g = j // 2
    add("Bm", list(range(O_XBC + 768 + 64 * g, O_XBC + 768 + 64 * (g + 1))))
    add("Cm", list(range(O_XBC + 768 + 128 + 64 * g, O_XBC + 768 + 128 + 64 * (g + 1))))
    add("dt", [O_DT + 3 * j + i for i in range(3)] + [O_DT + 12 + 3 * j + i for i in range(3)])
    pad()
    seg["mix_end"] = (len(cols), 0)
    add("gate", list(range(O_GT + D * j, O_GT + D * (j + 1))))
    return cols, seg


NMIX = core_cols(0)[1]["mix_end"][0]
NA = NMIX + D
SEG = core_cols(0)[1]
NSSD0 = SEG["ssd_start"][0]

TCH = 256
NTCH = T // TCH


def emit_inproj(k, nc, xT, xTc, modv, wA, pT, gates):
    ones = k.sb([128, 128], F32, "ones")
    k.op("dve", lambda e: e.memset(ones[:], 1.0), w=[ones])
    mv = k.sb([128, 16, 4], F32, "mv")
    k.dma(mv[:], modv.rearrange("(k p) r -> p k r", p=128), w=[mv])
    k.op("dve", lambda e: e.tensor_scalar(out=mv[:, :, 1], in0=mv[:, :, 1], scalar1=1.0, scalar2=None, op0=ALU.add),
         r=[mv], w=[mv])
    k.op("dve", lambda e: e.tensor_scalar(out=mv[:, :, 3], in0=mv[:, :, 3], scalar1=1.0, scalar2=None, op0=ALU.add),
         r=[mv], w=[mv])
    epsb = k.sb([128, 1], F32, "epsb")
    k.op("dve", lambda e: e.memset(epsb[:], EPS), w=[epsb])
    xt = [k.sb([128, 16, TCH], F32, f"xt{i}") for i in range(2)]
    sq = [k.sb([128, TCH], F32, f"sq{i}") for i in range(2)]
    rstd = k.sb([128, TCH], F32, "rstd")
    tmp = [k.sb([128, TCH], F32, f"tmp{i}") for i in range(2)]
    NP0 = 9 * TCH
    hT = k.sb([128, 16, NP0], BF16, "hT")
    wf = [k.sb([128, 16, 128], F32, f"wf{i}") for i in range(2)]
    wb = [k.sb([128, 16, 128], BF16, f"wb{i}") for i in range(2)]
    ost = [k.sb([128, 512], F32, f"ost{i}") for i in range(3)]
    ps_s = k.ps()
    ps_o = [k.ps() for _ in range(3)]
    it_o = 0
    ssd_ch = list(range(NSSD0 // 128, NMIX // 128))
    for (xT, part) in ((xT, 0), (xT, 1), (xTc, 0), (xTc, 1)):
        colchunks = ssd_ch if xT is xTc else [c for c in range(NA // 128) if c not in ssd_ch]
        ch0, nch = (0, 9) if part == 0 else (9, 8)
        for ci in range(nch):
            ch = ch0 + ci
            x_ = xt[ch % 2]
            k.dma(x_[:], xT[:, ch * TCH:(ch + 1) * TCH].rearrange("(k p) t -> p k t", p=128), w=[x_])
            for kk in range(16):
                s_ = sq[kk % 2]
                k.op("act", lambda e, s_=s_, x_=x_, kk=kk: e.activation(out=s_[:], in_=x_[:, kk, :], func=AF.Square),
                     r=[x_], w=[s_])
                k.op("pe", lambda e, s_=s_, kk=kk: e.matmul(ps_s[:, 0:TCH], lhsT=ones[:], rhs=s_[:],
                                                           start=(kk == 0), stop=(kk == 15)), r=[ones, s_], w=[ps_s])
            k.op("act", lambda e: e.activation(out=rstd[:], in_=ps_s[:, 0:TCH], func=AF.Sqrt, bias=epsb[:], scale=1.0 / D),
                 r=[ps_s, epsb], w=[rstd])
            k.op("dve", lambda e: e.reciprocal(out=rstd[:], in_=rstd[:]), r=[rstd], w=[rstd])
            mo = 2 if ch == 0 else 0
            for kk in range(16):
                t_ = tmp[kk % 2]
                k.op("dve", lambda e, t_=t_, x_=x_, kk=kk: e.tensor_tensor(out=t_[:], in0=x_[:, kk, :], in1=rstd[:], op=ALU.mult),
                     r=[x_, rstd], w=[t_])
                k.op("dve", lambda e, t_=t_, kk=kk, ci=ci, mo=mo: e.tensor_scalar(
                    out=hT[:, kk, ci * TCH:(ci + 1) * TCH], in0=t_[:], scalar1=mv[:, kk, mo + 1:mo + 2],
                    scalar2=mv[:, kk, mo:mo + 1], op0=ALU.mult, op1=ALU.add), r=[t_, mv], w=[hT])
        ntok = nch * TCH
        tok0 = ch0 * TCH
        for cc in colchunks:
            wf_ = wf[cc % 2]
            wb_ = wb[cc % 2]
            k.dma(wf_[:], wA[:, cc * 128:(cc + 1) * 128].rearrange("(k p) c -> p k c", p=128), w=[wf_])
            k.op("pool", lambda e, wf_=wf_, wb_=wb_: e.tensor_copy(out=wb_[:], in_=wf_[:]), r=[wf_], w=[wb_])
            for t0 in range(0, ntok, 512):
                tn = min(512, ntok - t0)
                p_ = ps_o[it_o % 3]
                o_ = ost[it_o % 3]
                it_o += 1
                for kk in range(16):
                    k.op("pe", lambda e, p_=p_, wb_=wb_, kk=kk, t0=t0, tn=tn: e.matmul(
                        p_[:, 0:tn], lhsT=wb_[:, kk, :], rhs=hT[:, kk, t0:t0 + tn], start=(kk == 0), stop=(kk == 15)),
                        r=[wb_, hT], w=[p_])
                if cc * 128 < NMIX:
                    k.op("act", lambda e, p_=p_, o_=o_, tn=tn: e.activation(out=o_[:, 0:tn], in_=p_[:, 0:tn], func=AF.Copy),
                         r=[p_], w=[o_])
                    k.dma(pT[cc * 128:(cc + 1) * 128, tok0 + t0:tok0 + t0 + tn], o_[:, 0:tn], r=[o_], w=[pT.tensor], q="pool")
                else:
                    k.op("act", lambda e, p_=p_, o_=o_, tn=tn: e.activation(out=o_[:, 0:tn], in_=p_[:, 0:tn], func=AF.Sigmoid),
                         r=[p_], w=[o_])
                    g0 = cc * 128 - NMIX
                    k.dma(gates[g0:g0 + 128, tok0 + t0:tok0 + t0 + tn], o_[:, 0:tn], r=[o_], q="pool")


def build_am(parts=("A",)):
    nc = bass.Bass("TRN2", target_bir_lowering=False)
    k = KB(nc)
    xT = nc.dram_tensor("xT", [D, T], F32, kind="ExternalInput").ap()
    modv = nc.dram_tensor("modv", [D, 4], F32, kind="ExternalInput").ap()
    wA = nc.dram_tensor("wA", [D, NA], F32, kind="ExternalInput").ap()
    pT = nc.dram_tensor("pT", [NMIX, T], F32, kind="ExternalOutput" if "dumpP" in parts else "Internal").ap()
    gates = nc.dram_tensor("gates", [D, T], F32, kind="ExternalOutput").ap()
    if "A" in parts:
        emit_inproj(k, nc, xT, xTc, modv, wA, pT, gates)
    k.emit()
    return nc


TWO_PI = 2.0 * math.pi


def chunks(n, c=512):
    return [(t0, min(c, n - t0)) for t0 in range(0, n, c)]


def emit_sin_turns(k, out, x, shape, name):
    xi = k.sb(shape, I32, name + "_i")
    xf = k.sb(shape, F32, name + "_f")
    k.op("dve", lambda e: e.tensor_copy(out=xi[:], in_=x[:]), r=[x], w=[xi])
    k.op("dve", lambda e: e.tensor_copy(out=xf[:], in_=xi[:]), r=[xi], w=[xf])
    k.op("dve", lambda e: e.tensor_tensor(out=xf[:], in0=x[:], in1=xf[:], op=ALU.subtract), r=[x, xf], w=[xf])
    k.op("dve", lambda e: e.tensor_scalar(out=xf[:], in0=xf[:], scalar1=0.4999999, scalar2=-0.4999999, op0=ALU.min, op1=ALU.max),
         r=[xf], w=[xf])
    k.op("act", lambda e: e.activation(out=out[:], in_=xf[:], func=AF.Sin, scale=TWO_PI), r=[xf], w=[out])


def emit_s5_abar(k, lamre, lamim, ls, shape, name):
    step = k.sb(shape, F32, name + "_step")
    rho = k.sb(shape, F32, name + "_rho")
    th = k.sb(shape, F32, name + "_th")
    th2 = k.sb(shape, F32, name + "_th2")
    sn = k.sb(shape, F32, name + "_sn")
    cs = k.sb(shape, F32, name + "_cs")
    k.op("act", lambda e: e.activation(out=step[:], in_=ls[:], func=AF.Exp), r=[ls], w=[step])
    k.op("dve", lambda e: e.tensor_tensor(out=rho[:], in0=lamre[:], in1=step[:], op=ALU.mult), r=[lamre, step], w=[rho])
    k.op("act", lambda e: e.activation(out=rho[:], in_=rho[:], func=AF.Exp), r=[rho], w=[rho])
    k.op("dve", lambda e: e.tensor_tensor(out=th[:], in0=lamim[:], in1=step[:], op=ALU.mult), r=[lamim, step], w=[th])
    k.op("dve", lambda e: e.tensor_scalar(out=th[:], in0=th[:], scalar1=1.0 / TWO_PI, scalar2=None, op0=ALU.mult), r=[th], w=[th])
    k.op("dve", lambda e: e.tensor_scalar(out=th2[:], in0=th[:], scalar1=0.25, scalar2=None, op0=ALU.add), r=[th], w=[th2])
    emit_sin_turns(k, sn, th, shape, name + "_s")
    emit_sin_turns(k, cs, th2, shape, name + "_c")
    k.op("dve", lambda e: e.tensor_tensor(out=sn[:], in0=sn[:], in1=rho[:], op=ALU.mult), r=[sn, rho], w=[sn])
    k.op("dve", lambda e: e.tensor_tensor(out=cs[:], in0=cs[:], in1=rho[:], op=ALU.mult), r=[cs, rho], w=[cs])
    return cs, sn, step


NLEV = 13


def emit_s5(k, nc, pT, ya, cst, prm):
    ident, swp = cst["ident"], cst["swap"]
    AR = k.sb([128, NLEV, 24], F32, "AR")
    AI = k.sb([128, NLEV, 24], F32, "AI")
    bbT = k.sb([16, 24, 128], F32, "bbT")
    cT = k.sb([128, 24, 16], F32, "cT")
    dsk = k.sb([16, 12], F32, "dsk")
    mk_tmp = k.mark()
    l128 = k.sb([128, 2, 24], F32, "l128")
    ls128 = k.sb([128, 24], F32, "ls128")
    sg128 = k.sb([128, 1], F32, "sg128")
    k.dma(l128[:], prm["s5_lam128"], w=[l128])
    k.dma(ls128[:], prm["s5_ls128"], w=[ls128])
    k.dma(sg128[:], prm["sign128"], w=[sg128])
    lre = k.sb([128, 24], F32, "lre")
    lim = k.sb([128, 24], F32, "lim")
    k.op("dve", lambda e: e.tensor_copy(out=lre[:], in_=l128[:, 0, :]), r=[l128], w=[lre])
    k.op("dve", lambda e: e.tensor_copy(out=lim[:], in_=l128[:, 1, :]), r=[l128], w=[lim])
    ar0, ai0, _ = emit_s5_abar(k, lre, lim, ls128, [128, 24], "c128")
    k.op("dve", lambda e: e.tensor_copy(out=AR[:, 0, :], in_=ar0[:]), r=[ar0], w=[AR])
    k.op("dve", lambda e: e.tensor_scalar(out=AI[:, 0, :], in0=ai0[:], scalar1=sg128[:, 0:1], scalar2=None, op0=ALU.mult),
         r=[ai0, sg128], w=[AI])
    t1 = k.sb([128, 24], F32, "sqt1")
    t2 = k.sb([128, 24], F32, "sqt2")
    for lv in range(1, NLEV):
        k.op("dve", lambda e, lv=lv: e.tensor_tensor(out=t1[:], in0=AR[:, lv - 1, :], in1=AR[:, lv - 1, :], op=ALU.mult), r=[AR], w=[t1])
        k.op("dve", lambda e, lv=lv: e.tensor_tensor(out=t2[:], in0=AI[:, lv - 1, :], in1=AI[:, lv - 1, :], op=ALU.mult), r=[AI], w=[t2])
        k.op("dve", lambda e, lv=lv: e.tensor_tensor(out=AR[:, lv, :], in0=t1[:], in1=t2[:], op=ALU.subtract), r=[t1, t2], w=[AR])
        k.op("dve", lambda e, lv=lv: e.tensor_tensor(out=t1[:], in0=AR[:, lv - 1, :], in1=AI[:, lv - 1, :], op=ALU.mult), r=[AR, AI], w=[t1])
        k.op("dve", lambda e, lv=lv: e.tensor_scalar(out=AI[:, lv, :], in0=t1[:], scalar1=2.0, scalar2=None, op0=ALU.mult), r=[t1], w=[AI])
    l16 = k.sb([16, 2, 24 * 64], F32, "l16")
    ls16 = k.sb([16, 24 * 64], F32, "ls16")
    b16 = k.sb([16, 2, 24 * 64], F32, "b16")
    k.dma(l16[:], prm["s5_lam16"], w=[l16])
    k.dma(ls16[:], prm["s5_ls16"], w=[ls16])
    k.dma(b16[:], prm["s5_b16"], w=[b16])
    SH = [16, 24 * 64]
    lre16 = k.sb(SH, F32, "lre16")
    lim16 = k.sb(SH, F32, "lim16")
    k.op("dve", lambda e: e.tensor_copy(out=lre16[:], in_=l16[:, 0, :]), r=[l16], w=[lre16])
    k.op("dve", lambda e: e.tensor_copy(out=lim16[:], in_=l16[:, 1, :]), r=[l16], w=[lim16])
    ar, ai, _ = emit_s5_abar(k, lre16, lim16, ls16, SH, "c16")
    den = k.sb(SH, F32, "den")
    q1 = k.sb(SH, F32, "q1")
    zr = k.sb(SH, F32, "zr")
    zi = k.sb(SH, F32, "zi")
    k.op("dve", lambda e: e.tensor_tensor(out=den[:], in0=lre16[:], in1=lre16[:], op=ALU.mult), r=[lre16], w=[den])
    k.op("dve", lambda e: e.tensor_tensor(out=q1[:], in0=lim16[:], in1=lim16[:], op=ALU.mult), r=[lim16], w=[q1])
    k.op("dve", lambda e: e.tensor_tensor(out=den[:], in0=den[:], in1=q1[:], op=ALU.add), r=[den, q1], w=[den])
    k.op("dve", lambda e: e.reciprocal(out=den[:], in_=den[:]), r=[den], w=[den])
    k.op("dve", lambda e: e.tensor_scalar(out=ar[:], in0=ar[:], scalar1=-1.0, scalar2=None, op0=ALU.add), r=[ar], w=[ar])
    k.op("dve", lambda e: e.tensor_tensor(out=zr[:], in0=ar[:], in1=lre16[:], op=ALU.mult), r=[ar, lre16], w=[zr])
    k.op("dve", lambda e: e.tensor_tensor(out=q1[:], in0=ai[:], in1=lim16[:], op=ALU.mult), r=[ai, lim16], w=[q1])
    k.op("dve", lambda e: e.tensor_tensor(out=zr[:], in0=zr[:], in1=q1[:], op=ALU.add), r=[zr, q1], w=[zr])
    k.op("dve", lambda e: e.tensor_tensor(out=zr[:], in0=zr[:], in1=den[:], op=ALU.mult), r=[zr, den], w=[zr])
    k.op("dve", lambda e: e.tensor_tensor(out=zi[:], in0=ai[:], in1=lre16[:], op=ALU.mult), r=[ai, lre16], w=[zi])
    k.op("dve", lambda e: e.tensor_tensor(out=q1[:], in0=ar[:], in1=lim16[:], op=ALU.mult), r=[ar, lim16], w=[q1])
    k.op("dve", lambda e: e.tensor_tensor(out=zi[:], in0=zi[:], in1=q1[:], op=ALU.subtract), r=[zi, q1], w=[zi])
    k.op("dve", lambda e: e.tensor_tensor(out=zi[:], in0=zi[:], in1=den[:], op=ALU.mult), r=[zi, den], w=[zi])
    bre = b16[:, 0, :].rearrange("h (g p) -> h g p", p=64)
    bim = b16[:, 1, :].rearrange("h (g p) -> h g p", p=64)
    zr3 = zr[:].rearrange("h (g p) -> h g p", p=64)
    zi3 = zi[:].rearrange("h (g p) -> h g p", p=64)
    q3 = k.sb([16, 24, 64], F32, "q3")
    k.op("dve", lambda e: e.tensor_tensor(out=bbT[:, :, 0:64], in0=zr3, in1=bre, op=ALU.mult), r=[zr, b16], w=[bbT])
    k.op("dve", lambda e: e.tensor_tensor(out=q3[:], in0=zi3, in1=bim, op=ALU.mult), r=[zi, b16], w=[q3])
    k.op("dve", lambda e: e.tensor_tensor(out=bbT[:, :, 0:64], in0=bbT[:, :, 0:64], in1=q3[:], op=ALU.subtract), r=[bbT, q3], w=[bbT])
    k.op("dve", lambda e: e.tensor_tensor(out=bbT[:, :, 64:128], in0=zr3, in1=bim, op=ALU.mult), r=[zr, b16], w=[bbT])
    k.op("dve", lambda e: e.tensor_tensor(out=q3[:], in0=zi3, in1=bre, op=ALU.mult), r=[zi, b16], w=[q3])
    k.op("dve", lambda e: e.tensor_tensor(out=bbT[:, :, 64:128], in0=bbT[:, :, 64:128], in1=q3[:], op=ALU.add), r=[bbT, q3], w=[bbT])
    k.dma(cT[:], prm["s5_c128"], w=[cT])
    k.op("dve", lambda e: e.tensor_scalar(out=cT[64:128], in0=cT[64:128], scalar1=-1.0, scalar2=None, op0=ALU.mult), r=[cT], w=[cT])
    k.dma(dsk[:], prm["s5_d"], w=[dsk])
    k.release(mk_tmp)

    u3 = [k.sb([16, T + NCTX], F32, f"u3_{i}") for i in range(2)]
    H = [[k.sb([128, T], F32, f"H{d}{i}") for i in range(2)] for d in range(2)]
    MT = [k.sb([128, 128], F32, f"MT{i}") for i in range(4)]
    yt = [k.sb([16, T], F32, f"yt{i}") for i in range(2)]
    psr = [k.ps() for _ in range(4)]
    psy = [k.ps() for _ in range(2)]
    nps = 0
    nmt = 0
    r0 = SEG["s5u"][0]
    for gl in range(12):
        u_ = u3[gl % 2]
        y_ = yt[gl % 2]
        k.dma(u_[:, 0:T], pT[r0 + 16 * gl:r0 + 16 * (gl + 1), :], r=[pT.tensor], w=[u_])
        k.dma(u_[:, T:T + NCTX], pT[r0 + 16 * gl:r0 + 16 * (gl + 1), 0:NCTX], r=[pT.tensor], w=[u_])
        cur = [0, 0]
        for d in range(2):
            dg = d * 12 + gl
            c0 = 0 if d == 0 else NCTX
            for (t0, n) in chunks(T):
                p_ = psr[nps % 4]
                nps += 1
                k.op("pe", lambda e, p_=p_, dg=dg, u_=u_, c0=c0, t0=t0, n=n: e.matmul(
                    p_[:, 0:n], lhsT=bbT[:, dg, :], rhs=u_[:, c0 + t0:c0 + t0 + n], start=True, stop=True), r=[bbT, u_], w=[p_])
                k.op("act", lambda e, p_=p_, d=d, t0=t0, n=n: e.activation(out=H[d][0][:, t0:t0 + n], in_=p_[:, 0:n], func=AF.Copy),
                     r=[p_], w=[H[d][0]])
        for lv in range(NLEV):
            sh = 1 << lv
            for d in range(2):
                dg = d * 12 + gl
                src = H[d][cur[d]]
                dst = H[d][1 - cur[d]]
                cur[d] = 1 - cur[d]
                m_ = MT[nmt % 4]
                nmt += 1
                k.op("dve", lambda e, m_=m_, lv=lv, dg=dg: e.tensor_scalar(out=m_[:], in0=ident[:], scalar1=AR[:, lv, dg:dg + 1],
                                                                         scalar2=None, op0=ALU.mult), r=[ident, AR], w=[m_])
                k.op("dve", lambda e, m_=m_, lv=lv, dg=dg: e.scalar_tensor_tensor(out=m_[:], in0=swp[:], scalar=AI[:, lv, dg:dg + 1],
                                                                                in1=m_[:], op0=ALU.mult, op1=ALU.add),
                     r=[swp, AI, m_], w=[m_])
                if d == 0:
                    k.op("pool", lambda e, src=src, dst=dst, sh=sh: e.tensor_copy(out=dst[:, 0:sh], in_=src[:, 0:sh]), r=[src], w=[dst])
                    lo = sh
                else:
                    k.op("pool", lambda e, src=src, dst=dst, sh=sh: e.tensor_copy(out=dst[:, T - sh:T], in_=src[:, T - sh:T]), r=[src], w=[dst])
                    lo = 0
                for (t0, n) in chunks(T - sh):
                    ta = lo + t0
                    tb = ta - sh if d == 0 else ta + sh
                    p_ = psr[nps % 4]
                    nps += 1
                    k.op("pe", lambda e, p_=p_, m_=m_, src=src, tb=tb, n=n: e.matmul(p_[:, 0:n], lhsT=m_[:], rhs=src[:, tb:tb + n],
                                                                                  start=True, stop=True), r=[m_, src], w=[p_])
                    k.op("dve", lambda e, p_=p_, src=src, dst=dst, ta=ta, n=n: e.tensor_tensor(
                        out=dst[:, ta:ta + n], in0=src[:, ta:ta + n], in1=p_[:, 0:n], op=ALU.add), r=[src, p_], w=[dst])
        Hf = H[0][cur[0]]
        Hb = H[1][cur[1]]
        for (t0, n) in [(0, NCTX)] + [(NCTX + a, b_) for (a, b_) in chunks(S)]:
            tb = (S + t0) if t0 < NCTX else (t0 - NCTX)
            p_ = psy[(t0 // 256) % 2]
            k.op("pe", lambda e, p_=p_, gl=gl, t0=t0, n=n: e.matmul(p_[0:16, 0:n], lhsT=cT[:, gl, :], rhs=Hf[:, t0:t0 + n],
                                                                   start=True, stop=False), r=[cT, Hf], w=[p_])
            k.op("pe", lambda e, p_=p_, gl=gl, tb=tb, n=n: e.matmul(p_[0:16, 0:n], lhsT=cT[:, 12 + gl, :], rhs=Hb[:, tb:tb + n],
                                                                   start=False, stop=True), r=[cT, Hb], w=[p_])
            k.op("dve", lambda e, p_=p_, u_=u_, y_=y_, gl=gl, t0=t0, n=n: e.scalar_tensor_tensor(
                out=y_[:, t0:t0 + n], in0=u_[:, t0:t0 + n], scalar=dsk[:, gl:gl + 1], in1=p_[0:16, 0:n], op0=ALU.mult, op1=ALU.add),
                r=[u_, dsk, p_], w=[y_])
        k.dma(ya[16 * gl:16 * (gl + 1), :], y_[:], r=[y_], q="pool")


def s5_host_params(inp, li, j):
    gs = slice(12 * j, 12 * (j + 1))
    lre = inp["s5_lambda_re"][li][:, gs]
    lim = inp["s5_lambda_im"][li][:, gs]
    ls = inp["s5_log_step"][li][:, gs]
    col = lambda a: np.ascontiguousarray(a.reshape(24, 64).T)
    lam128 = np.stack([np.concatenate([col(lre), col(lre)], 0), np.concatenate([col(lim), col(lim)], 0)], 1)
    ls128 = np.broadcast_to(ls.reshape(1, 24), (128, 24))
    lam16 = np.broadcast_to(np.stack([lre.reshape(24 * 64), lim.reshape(24 * 64)], 0)[None], (16, 2, 24 * 64))
    ls16 = np.broadcast_to(np.repeat(ls.reshape(24), 64)[None], (16, 24 * 64))
    bre = inp["s5_b_re"][li][:, gs]
    bim = inp["s5_b_im"][li][:, gs]
    b16 = np.stack([bre.reshape(24 * 64, 16).T, bim.reshape(24 * 64, 16).T], 1)
    cre = inp["s5_c_re"][li][:, gs]
    cim = inp["s5_c_im"][li][:, gs]
    c128 = np.concatenate([cre.reshape(24, 16, 64).transpose(2, 0, 1), cim.reshape(24, 16, 64).transpose(2, 0, 1)], 0)
    dd = inp["s5_d"][li][192 * j:192 * (j + 1)].reshape(12, 16).T
    f = lambda a: np.ascontiguousarray(a, dtype=np.float32)
    return {"s5_lam128": f(lam128), "s5_ls128": f(ls128), "s5_lam16": f(lam16), "s5_ls16": f(ls16), "s5_b16": f(b16),
            "s5_c128": f(c128), "s5_d": f(dd)}


def host_consts():
    ident = np.eye(128, dtype=np.float32)
    swap = np.zeros((128, 128), np.float32)
    for i in range(64):
        swap[i, i + 64] = 1.0
        swap[i + 64, i] = 1.0
    sign = np.ones((128, 1), np.float32)
    sign[64:] = -1.0
    return {"c_ident": ident, "c_swap": swap, "sign128": sign}


S5_SHAPES = {"s5_lam128": [128, 2, 24], "s5_ls128": [128, 24], "s5_lam16": [16, 2, 1536], "s5_ls16": [16, 1536],
             "s5_b16": [16, 2, 1536], "s5_c128": [128, 24, 16], "s5_d": [16, 12], "sign128": [128, 1]}


def build_am(parts=("A",)):
    nc = bass.Bass("TRN2", target_bir_lowering=False)
    k = KB(nc)
    xT = nc.dram_tensor("xT", [D, T], F32, kind="ExternalInput").ap()
    xTc = nc.dram_tensor("xTc", [D, T], F32, kind="ExternalInput").ap()
    modv = nc.dram_tensor("modv", [D, 4], F32, kind="ExternalInput").ap()
    wA = nc.dram_tensor("wA", [D, NA], F32, kind="ExternalInput").ap()
    pT = nc.dram_tensor("pT", [NMIX, T], F32, kind="ExternalOutput" if "dumpP" in parts else "Internal").ap()
    gates = nc.dram_tensor("gates", [D, T], F32, kind="ExternalOutput").ap()
    prm = {}
    cst = {}
    ci = nc.dram_tensor("c_ident", [128, 128], F32, kind="ExternalInput").ap()
    cs_ = nc.dram_tensor("c_swap", [128, 128], F32, kind="ExternalInput").ap()
    cst["ident"] = k.sb([128, 128], F32, "ident")
    cst["swap"] = k.sb([128, 128], F32, "swap")
    k.dma(cst["ident"][:], ci, w=[cst["ident"]])
    k.dma(cst["swap"][:], cs_, w=[cst["swap"]])
    mk = k.mark()
    if "A" in parts:
        emit_inproj(k, nc, xT, xTc, modv, wA, pT, gates)
        k.release(mk)
    if "S5" in parts:
        for nm, shp in S5_SHAPES.items():
            prm[nm] = nc.dram_tensor(nm, shp, F32, kind="ExternalInput").ap()
        ya = nc.dram_tensor("ya", [192, T], F32, kind="ExternalOutput").ap()
        emit_s5(k, nc, pT, ya, cst, prm)
        k.release(mk)
    k.emit()
    return nc


SEGS_F = [(0, 256, False), (256, 1792, False), (1792, 3328, False), (3328, 4352, False)]
SEGS_B = [(0, 256, True), (3328, 4352, True), (1792, 3328, True), (256, 1792, True)]


def emit_outer_scan(k, cst, bufs, Xs, PX, Arep, Crep, dec_of_pt, d, yacc, first):
    selx, hm = cst["selx"], cst["hm"]
    U, Hs, carry, psx, psy = bufs
    npt = PX // 2
    it = 0
    for si, (a, b_, rev) in enumerate(SEGS_B if d == 1 else SEGS_F):
        L = b_ - a
        cks = chunks(L)
        for pt in range(npt):
            u_ = U[it % 2]
            h_ = Hs[it % 2]
            v_ = h_
            it += 1
            p0 = 0 if pt < 32 else 64
            for ci, (t0, n) in enumerate(cks):
                p_ = psx[(it + ci) % 2]
                k.op("pe", lambda e, p_=p_, pt=pt, p0=p0, t0=t0, n=n: e.matmul(
                    p_[:, 0:n], lhsT=selx[p0:p0 + 64, pt % 32, :], rhs=Xs[p0:p0 + 64, a + t0:a + t0 + n], start=True, stop=True),
                    r=[selx, Xs], w=[p_])
                k.op("dve", lambda e, p_=p_, u_=u_, t0=t0, n=n: e.tensor_tensor(
                    out=u_[:, t0:t0 + n], in0=p_[:, 0:n], in1=Arep[:, a + t0:a + t0 + n], op=ALU.mult), r=[p_, Arep], w=[u_])
            dec = dec_of_pt(pt)
            init = 0.0 if si == 0 else carry[:, pt:pt + 1]
            rd = [dec, u_] + ([] if si == 0 else [carry])
            if rev:
                k.op("dve", lambda e, h_=h_, u_=u_, dec=dec, init=init, L=L: e.tensor_tensor_scan(
                    out=h_[:, L - 1::-1] if False else h_[:, 0:L][:, ::-1], data0=dec[:, a:b_][:, ::-1], data1=u_[:, 0:L][:, ::-1],
                    initial=init, op0=ALU.mult, op1=ALU.add), r=rd, w=[h_])
                last = 0
            else:
                k.op("dve", lambda e, h_=h_, u_=u_, dec=dec, init=init, L=L: e.tensor_tensor_scan(
                    out=h_[:, 0:L], data0=dec[:, a:b_], data1=u_[:, 0:L], initial=init, op0=ALU.mult, op1=ALU.add), r=rd, w=[h_])
                last = L - 1
            if si < 3:
                k.op("act", lambda e, h_=h_, pt=pt, last=last: e.activation(out=carry[:, pt:pt + 1], in_=h_[:, last:last + 1], func=AF.Copy),
                     r=[h_], w=[carry])
            k.op("pool", lambda e, h_=h_, v_=v_, L=L: e.tensor_tensor(out=v_[:, 0:L], in0=h_[:, 0:L], in1=Crep[:, a:b_], op=ALU.mult),
                 r=[h_, Crep, carry], w=[v_])
            for ci, (t0, n) in enumerate(cks):
                k.op("pe", lambda e, ci=ci, pt=pt, v_=v_, t0=t0, n=n: e.matmul(
                    psy[ci][0:PX, 0:n], lhsT=hm[:, 128 - 2 * pt:128 - 2 * pt + PX], rhs=v_[:, t0:t0 + n], start=(pt == 0), stop=(pt == npt - 1)),
                    r=[hm, v_], w=[psy[ci]])
        for ci, (t0, n) in enumerate(cks):
            if first:
                k.op("act", lambda e, ci=ci, t0=t0, n=n: e.activation(out=yacc[0:PX, a + t0:a + t0 + n], in_=psy[ci][0:PX, 0:n], func=AF.Copy),
                     r=[psy[ci]], w=[yacc])
            else:
                k.op("dve", lambda e, ci=ci, t0=t0, n=n: e.tensor_tensor(out=yacc[0:PX, a + t0:a + t0 + n], in0=yacc[0:PX, a + t0:a + t0 + n],
                                                                        in1=psy[ci][0:PX, 0:n], op=ALU.add), r=[psy[ci], yacc], w=[yacc])


def emit_conv3(k, dst, src, w3, bias, P, silu, wt=None):
    for (a, b_) in ((0, NCTX), (NCTX, T)):
        k.op("dve", lambda e, a=a, b_=b_: e.tensor_scalar(out=dst[0:P, a:b_], in0=src[0:P, a:b_], scalar1=w3[0:P, 1:2], scalar2=bias[0:P, 0:1],
                                                         op0=ALU.mult, op1=ALU.add), r=[src, wt], w=[dst])
        k.op("dve", lambda e, a=a, b_=b_: e.scalar_tensor_tensor(out=dst[0:P, a + 1:b_], in0=src[0:P, a:b_ - 1], scalar=w3[0:P, 0:1],
                                                                in1=dst[0:P, a + 1:b_], op0=ALU.mult, op1=ALU.add), r=[src, wt, dst], w=[dst])
        k.op("dve", lambda e, a=a, b_=b_: e.scalar_tensor_tensor(out=dst[0:P, a:b_ - 1], in0=src[0:P, a + 1:b_], scalar=w3[0:P, 2:3],
                                                                in1=dst[0:P, a:b_ - 1], op0=ALU.mult, op1=ALU.add), r=[src, wt, dst], w=[dst])
    if silu:
        k.op("act", lambda e: e.activation(out=dst[0:P, :], in_=dst[0:P, :], func=AF.Silu), r=[dst], w=[dst])


def scan_bufs(k):
    U = [k.sb([128, 1536], F32, f"U{i}") for i in range(2)]
    Hs = [k.sb([128, 1536], F32, f"Hs{i}") for i in range(2)]
    carry = k.sb([128, 64], F32, "carry")
    psx = [k.ps() for _ in range(2)]
    psy = [k.ps() for _ in range(3)]
    return (U, Hs, carry, psx, psy)


def emit_ssd(k, nc, pT, yb, cst, prm):
    sp = k.sb([128, 2, 8], F32, "ssdp_x")
    spB = k.sb([128, 8], F32, "ssdp_B")
    spC = k.sb([128, 8], F32, "ssdp_C")
    sdt = k.sb([6, 4], F32, "ssdp_dt")
    one6 = k.sb([6, 1], F32, "one6")
    selr = k.sb([6, 2, 2, 128], F32, "selr")
    selh = k.sb([38, 6, 128], F32, "selh")
    k.dma(sp[:], prm["ssd_px"], w=[sp])
    k.dma(spB[:], prm["ssd_pB"], w=[spB])
    k.dma(spC[:], prm["ssd_pC"], w=[spC])
    k.dma(sdt[:], prm["ssd_pdt"], w=[sdt])
    k.dma(selr[:], prm["selr"], w=[selr])
    k.dma(selh[32:38], prm["selh"], w=[selh])
    k.op("dve", lambda e: e.memset(one6[:], 1.0), w=[one6])
    rz, rx, rB, rC, rdt = SEG["z"][0], SEG["x"][0], SEG["Bm"][0], SEG["Cm"][0], SEG["dt"][0]
    raw = k.sb([128, T], F32, "raw")
    xa = k.sb([128, T], F32, "xa")
    Brep = k.sb([128, T], F32, "Brep")
    Crep = k.sb([128, T], F32, "Crep")
    ya = k.sb([128, T], F32, "ssd_ya")
    decA = [k.sb([128, T], F32, f"decA{i}") for i in range(2)]
    dd = k.sb([38, T], F32, "dtdec")
    k.dma(raw[0:64, :], pT[rB:rB + 64, :], r=[pT.tensor], w=[raw])
    k.dma(raw[64:128, :], pT[rB:rB + 64, :], r=[pT.tensor], w=[raw])
    emit_conv3(k, Brep, raw, spB, spB[:, 3:4], 128, True, wt=spB)
    k.dma(raw[0:64, :], pT[rC:rC + 64, :], r=[pT.tensor], w=[raw])
    k.dma(raw[64:128, :], pT[rC:rC + 64, :], r=[pT.tensor], w=[raw])
    emit_conv3(k, Crep, raw, spC, spC[:, 3:4], 128, True, wt=spC)
    k.dma(dd[0:6, :], pT[rdt:rdt + 6, :], r=[pT.tensor], w=[dd])
    k.op("act", lambda e: e.activation(out=dd[0:6, :], in_=dd[0:6, :], func=AF.Exp, bias=sdt[:, 0:1], scale=1.0), r=[dd, sdt], w=[dd])
    k.op("act", lambda e: e.activation(out=dd[0:6, :], in_=dd[0:6, :], func=AF.Ln, bias=one6[:], scale=1.0), r=[dd, one6], w=[dd])
    k.op("act", lambda e: e.activation(out=sdt[:, 2:3], in_=sdt[:, 1:2], func=AF.Exp), r=[sdt], w=[sdt])
    k.op("dve", lambda e: e.tensor_scalar(out=sdt[:, 2:3], in0=sdt[:, 2:3], scalar1=-1.0, scalar2=None, op0=ALU.mult), r=[sdt], w=[sdt])
    k.op("act", lambda e: e.activation(out=raw[0:6, :], in_=dd[0:6, :], func=AF.Exp, scale=sdt[:, 2:3]), r=[dd, sdt], w=[raw])
    k.dma(dd[32:38, :], raw[0:6, :], r=[raw], w=[dd])
    bufs = scan_bufs(k)
    psx = bufs[3]
    Xs = raw
    for ti, PX in ((0, 128), (1, 64)):
        k.dma(raw[0:PX, :], pT[rx + 128 * ti:rx + 128 * ti + PX, :], r=[pT.tensor], w=[raw])
        emit_conv3(k, xa, raw, sp[:, ti, :], sp[:, ti, 3:4], PX, True, wt=sp)
        for d in range(2):
            for ci, (t0, n) in enumerate(chunks(T)):
                p_ = psx[ci % 2]
                k.op("pe", lambda e, p_=p_, d=d, ti=ti, t0=t0, n=n: e.matmul(p_[:, 0:n], lhsT=selr[:, d, ti, :], rhs=dd[0:6, t0:t0 + n],
                                                                           start=True, stop=True), r=[selr, dd], w=[p_])
                k.op("dve", lambda e, p_=p_, PX=PX, t0=t0, n=n: e.tensor_tensor(out=Xs[0:PX, t0:t0 + n], in0=xa[0:PX, t0:t0 + n],
                                                                              in1=p_[0:PX, 0:n], op=ALU.mult), r=[p_, xa], w=[Xs])
            for hh in range(2 if ti == 0 else 1):
                rr = 3 * d + 2 * ti + hh
                for ci, (t0, n) in enumerate(chunks(T)):
                    p_ = psx[ci % 2]
                    k.op("pe", lambda e, p_=p_, rr=rr, t0=t0, n=n: e.matmul(p_[:, 0:n], lhsT=selh[32:38, rr, :], rhs=dd[32:38, t0:t0 + n],
                                                                          start=True, stop=True), r=[selh, dd], w=[p_])
                    k.op("act", lambda e, p_=p_, hh=hh, t0=t0, n=n: e.activation(out=decA[hh][:, t0:t0 + n], in_=p_[:, 0:n], func=AF.Copy),
                         r=[p_], w=[decA[hh]])
            emit_outer_scan(k, cst, bufs, Xs, PX, Brep, Crep, (lambda pt: decA[0] if pt < 32 else decA[1]), d, ya, first=(d == 0))
        P = PX
        r_ = rz + 128 * ti
        k.op("dve", lambda e, P=P, ti=ti: e.scalar_tensor_tensor(out=ya[0:P, :], in0=xa[0:P, :], scalar=sp[0:P, ti, 4:5],
                                                                in1=ya[0:P, :], op0=ALU.mult, op1=ALU.add), r=[xa, sp, ya], w=[ya])
        k.dma(raw[0:P, :], pT[r_:r_ + P, :], r=[pT.tensor], w=[raw])
        k.op("act", lambda e, P=P: e.activation(out=raw[0:P, :], in_=raw[0:P, :], func=AF.Silu), r=[raw], w=[raw])
        k.op("dve", lambda e, P=P: e.tensor_tensor(out=ya[0:P, :], in0=ya[0:P, :], in1=raw[0:P, :], op=ALU.mult), r=[ya, raw], w=[ya])
        k.dma(yb[128 * ti:128 * ti + P, :], ya[0:P, :], r=[ya], q="pool")


def emit_gla(k, nc, pT, yc, cst, prm):
    gp = k.sb([128, 2, 2, 2], F32, "glap")
    gw = k.sb([16, 2, 2, 128], F32, "glaw")
    nw = k.sb([128, 1], F32, "glanw")
    one = k.sb([128, 1], F32, "gone")
    epsb = k.sb([128, 1], F32, "geps")
    ones = k.sb([128, 128], F32, "gones")
    k.dma(gp[:], prm["gla_p"], w=[gp])
    k.dma(gw[:], prm["gla_w"], w=[gw])
    k.dma(nw[:], prm["gla_nw"], w=[nw])
    k.op("dve", lambda e: e.memset(one[:], 1.0), w=[one])
    k.op("dve", lambda e: e.memset(epsb[:], EPS), w=[epsb])
    k.op("dve", lambda e: e.memset(ones[:], 1.0), w=[ones])
    k.op("dve", lambda e: e.tensor_scalar(out=gp[:], in0=gp[:], scalar1=-1.0, scalar2=None, op0=ALU.mult), r=[gp], w=[gp])
    rT = [k.sb([16, T], F32, f"rT{d}") for d in range(2)]
    rr = SEG["r"][0]
    for d in range(2):
        k.dma(rT[d][:], pT[rr + 16 * d:rr + 16 * (d + 1), :], r=[pT.tensor], w=[rT[d]])
    Krep = k.sb([128, T], F32, "Krep")
    Qrep = k.sb([128, T], F32, "Qrep")
    dec = [k.sb([128, T], F32, f"gdec{d}") for d in range(2)]
    vt = k.sb([128, T], F32, "gv")
    yo = k.sb([128, T], F32, "gyo")
    bufs = scan_bufs(k)
    psx = bufs[3]
    for hs in range(2):
        rq, rk, rv, rg = SEG[f"q{hs}"][0], SEG[f"k{hs}"][0], SEG[f"v{hs}"][0], SEG[f"g{hs}"][0]
        for half in range(2):
            k.dma(Krep[64 * half:64 * half + 64, :], pT[rk:rk + 64, :], r=[pT.tensor], w=[Krep])
            k.dma(Qrep[64 * half:64 * half + 64, :], pT[rq:rq + 64, :], r=[pT.tensor], w=[Qrep])
        k.op("dve", lambda e: e.tensor_scalar(out=Qrep[:], in0=Qrep[:], scalar1=0.125, scalar2=None, op0=ALU.mult), r=[Qrep], w=[Qrep])
        k.dma(vt[:], pT[rv:rv + 128, :], r=[pT.tensor], w=[vt])
        for d in range(2):
            for ci, (t0, n) in enumerate(chunks(T)):
                p_ = psx[ci % 2]
                k.op("pe", lambda e, p_=p_, hs=hs, d=d, t0=t0, n=n: e.matmul(p_[:, 0:n], lhsT=gw[:, hs, d, :], rhs=rT[d][:, t0:t0 + n],
                                                                           start=True, stop=True), r=[gw, rT[d]], w=[p_])
                k.op("act", lambda e, p_=p_, hs=hs, d=d, t0=t0, n=n: e.activation(out=dec[d][:, t0:t0 + n], in_=p_[:, 0:n], func=AF.Exp,
                                                                                bias=gp[:, hs, d, 0:1], scale=-1.0), r=[p_, gp], w=[dec[d]])
            k.op("act", lambda e, d=d: e.activation(out=dec[d][:], in_=dec[d][:], func=AF.Ln, bias=one[:], scale=1.0), r=[dec[d], one], w=[dec[d]])
            k.op("act", lambda e, d=d: e.activation(out=dec[d][:], in_=dec[d][:], func=AF.Exp, scale=-1.0 / 16.0), r=[dec[d]], w=[dec[d]])
            emit_outer_scan(k, cst, bufs, vt, 128, Krep, Qrep, (lambda pt, d=d: dec[d]), d, yo, first=(d == 0))
        k.dma(vt[:], pT[rg:rg + 128, :], r=[pT.tensor], w=[vt])
        k.op("act", lambda e: e.activation(out=vt[:], in_=vt[:], func=AF.Silu), r=[vt], w=[vt])
        for ci, (t0, n) in enumerate(chunks(T)):
            p_ = psx[ci % 2]
            k.op("act", lambda e, t0=t0, n=n: e.activation(out=Krep[:, t0:t0 + n], in_=yo[:, t0:t0 + n], func=AF.Square), r=[yo], w=[Krep])
            k.op("pe", lambda e, p_=p_, t0=t0, n=n: e.matmul(p_[:, 0:n], lhsT=ones[:], rhs=Krep[:, t0:t0 + n], start=True, stop=True),
                 r=[ones, Krep], w=[p_])
            k.op("act", lambda e, p_=p_, t0=t0, n=n: e.activation(out=Qrep[:, t0:t0 + n], in_=p_[:, 0:n], func=AF.Sqrt, bias=epsb[:], scale=1.0 / 128),
                 r=[p_, epsb], w=[Qrep])
        k.op("dve", lambda e: e.reciprocal(out=Qrep[:], in_=Qrep[:]), r=[Qrep], w=[Qrep])
        k.op("dve", lambda e: e.tensor_tensor(out=yo[:], in0=yo[:], in1=Qrep[:], op=ALU.mult), r=[yo, Qrep], w=[yo])
        k.op("dve", lambda e: e.scalar_tensor_tensor(out=yo[:], in0=yo[:], scalar=nw[:, 0:1], in1=vt[:], op0=ALU.mult, op1=ALU.mult),
             r=[yo, nw, vt], w=[yo])
        k.dma(yc[128 * hs:128 * (hs + 1), :], yo[:], r=[yo], q="pool")


def ssd_host_params(inp, li, j):
    cw = inp["ssd_conv_w"][li]
    cb = inp["ssd_conv_b"][li]
    px = np.zeros((128, 2, 8), np.float32)
    for ti in range(2):
        n = 128 if ti == 0 else 64
        c = 192 * j + 128 * ti + np.arange(n)
        px[:n, ti, 0:3] = cw[:, c].T
        px[:n, ti, 3] = cb[c]
        px[:n, ti, 4] = inp["ssd_d"][li][c // 64]
    g = j // 2
    pB = np.zeros((128, 8), np.float32)
    pC = np.zeros((128, 8), np.float32)
    for (arr, base) in ((pB, 768 + 64 * g), (pC, 768 + 128 + 64 * g)):
        c = base + (np.arange(128) % 64)
        arr[:, 0:3] = cw[:, c].T
        arr[:, 3] = cb[c]
    pdt = np.zeros((6, 4), np.float32)
    for d in range(2):
        for h in range(3):
            pdt[3 * d + h, 0] = inp["ssd_dt_bias"][li][d, 3 * j + h]
            pdt[3 * d + h, 1] = inp["ssd_a_log"][li][d, 3 * j + h]
    selr = np.zeros((6, 2, 2, 128), np.float32)
    for d in range(2):
        for m in range(128):
            selr[3 * d + m // 64, d, 0, m] = 1.0
            selr[3 * d + 2, d, 1, m] = 1.0
    selh = np.zeros((6, 6, 128), np.float32)
    for r in range(6):
        selh[r, r, :] = 1.0
    return {"ssd_px": px, "ssd_pB": pB, "ssd_pC": pC, "ssd_pdt": pdt, "selr": selr, "selh": selh}


SSD_SHAPES = {"ssd_px": [128, 2, 8], "ssd_pB": [128, 8], "ssd_pC": [128, 8], "ssd_pdt": [6, 4], "selr": [6, 2, 2, 128],
              "selh": [6, 6, 128]}


def gla_heads(j):
    return [j, j + 4 if j < 2 else j]


def gla_host_params(inp, li, j):
    gp = np.zeros((128, 2, 2, 2), np.float32)
    gw = np.zeros((16, 2, 2, 128), np.float32)
    for s_, h in enumerate(gla_heads(j)):
        for d in range(2):
            cols = 64 * h + (np.arange(128) % 64)
            gp[:, s_, d, 0] = inp["gla_gate_b"][li][d, cols]
            gw[:, s_, d, :] = inp["gla_gate_w"][li][d][:, cols]
    return {"gla_p": gp, "gla_w": gw, "gla_nw": np.ascontiguousarray(inp["gla_norm_w"][li][:, None], dtype=np.float32)}


GLA_SHAPES = {"gla_p": [128, 2, 2, 2], "gla_w": [16, 2, 2, 128], "gla_nw": [128, 1]}


def scan_consts():
    selx = np.zeros((128, 32, 128), np.float32)
    for p in range(128):
        q = p % 64
        selx[p, q // 2, 64 * (q % 2):64 * (q % 2) + 64] = 1.0
    hm = np.zeros((128, 256), np.float32)
    hm[0:64, 128] = 1.0
    hm[64:128, 129] = 1.0
    return {"c_selx": selx, "c_hm": hm}


def build_am(parts=("A",)):
    nc = bass.Bass("TRN2", target_bir_lowering=False)
    k = KB(nc)
    xT = nc.dram_tensor("xT", [D, T], F32, kind="ExternalInput").ap()
    xTc = nc.dram_tensor("xTc", [D, T], F32, kind="ExternalInput").ap()
    modv = nc.dram_tensor("modv", [D, 4], F32, kind="ExternalInput").ap()
    wA = nc.dram_tensor("wA", [D, NA], F32, kind="ExternalInput").ap()
    pT = nc.dram_tensor("pT", [NMIX, T], F32, kind="ExternalOutput" if "dumpP" in parts else "Internal").ap()
    gates = nc.dram_tensor("gates", [D, T], F32, kind="ExternalOutput").ap()
    prm = {}
    cst = {}
    for nm, shp in (("ident", [128, 128]), ("swap", [128, 128]), ("selx", [128, 32, 128]), ("hm", [128, 256])):
        ap = nc.dram_tensor("c_" + nm, shp, F32, kind="ExternalInput").ap()
        cst[nm] = k.sb(shp, F32, nm)
        k.dma(cst[nm][:], ap, w=[cst[nm]])
    mk = k.mark()
    if "A" in parts:
        emit_inproj(k, nc, xT, xTc, modv, wA, pT, gates)
        k.release(mk)
    for (tag, shapes, fn, oname, orows) in (("S5", S5_SHAPES, emit_s5, "ya", 192), ("SSD", SSD_SHAPES, emit_ssd, "yb", 192),
                                            ("GLA", GLA_SHAPES, emit_gla, "yc", 256), ("HY", HY_SHAPES, emit_hyena, "yd", 192)):
        if tag in parts:
            for nm, shp in shapes.items():
                if nm not in prm:
                    prm[nm] = nc.dram_tensor(nm, shp, F32, kind="ExternalInput").ap()
            o = nc.dram_tensor(oname, [orows, T], F32, kind="ExternalOutput").ap()
            fn(k, nc, pT, o, cst, prm)
            k.release(mk)
    k.emit()
    return nc


HY_SHAPES = {"hy_cw": [128, 2, 3, 4], "hy_bias": [128, 2, 2], "hy_w1": [33, 64], "hy_w2": [64, 64], "hy_mlp": [64, 4],
             "hy_w3": [64, 2, 2, 192], "hy_embL": [33, S], "hy_embC": [33, NCTX], "hy_decL": [128, 2, S], "hy_decC": [128, 2, NCTX]}


def hy_host_params(inp, li, j):
    cw = np.zeros((128, 2, 3, 4), np.float32)
    hb = np.zeros((128, 2, 2), np.float32)
    for ti in range(2):
        n = 128 if ti == 0 else 64
        for s_ in range(3):
            c = 768 * s_ + 192 * j + 128 * ti + np.arange(n)
            cw[:n, ti, s_, 0:3] = inp["hy_conv_w"][li][:, c].T
            cw[:n, ti, s_, 3] = inp["hy_conv_b"][li][c]
        c = 192 * j + 128 * ti + np.arange(n)
        hb[:n, ti, :] = inp["hy_bias"][li][:, c].T
    mlp = np.stack([inp["hy_b1"][li], inp["hy_freq1"][li], inp["hy_b2"][li], inp["hy_freq2"][li]], 1)
    w3 = inp["hy_w3"][li].reshape(64, 2, 2, 768)[:, :, :, 192 * j:192 * (j + 1)]
    out = {"hy_cw": cw, "hy_bias": hb, "hy_w1": inp["hy_w1"][li], "hy_w2": inp["hy_w2"][li], "hy_mlp": mlp, "hy_w3": w3}
    deltas = np.abs(np.linspace(math.log(1e-2) / 1.5, math.log(1e-2) / 0.3, 768, dtype=np.float32))[192 * j:192 * (j + 1)]
    for tag, n in (("L", S), ("C", NCTX)):
        t = np.linspace(0.0, 1.0, n, dtype=np.float32)
        freqs = np.linspace(1e-4, 15.0, 16, dtype=np.float32)
        ang = (np.float32(2.0 * math.pi / n) * np.arange(n, dtype=np.float32)[:, None]) * freqs[None, :]
        emb = np.concatenate([t[:, None], np.cos(ang), -np.sin(ang)], -1)
        out["hy_emb" + tag] = emb.T
        dec = np.exp(-t[None, :] * deltas[:, None])
        dd = np.zeros((128, 2, n), np.float32)
        dd[:, 0] = dec[0:128]
        dd[0:64, 1] = dec[128:192]
        out["hy_dec" + tag] = dd
    return {kk: np.ascontiguousarray(v, dtype=np.float32) for kk, v in out.items()}


def emit_hyena(k, nc, pT, yd, cst, prm):
    cw = k.sb([128, 2, 3, 4], F32, "hy_cw")
    hbias = k.sb([128, 2, 2], F32, "hy_bias")
    w1 = k.sb([33, 64], F32, "hy_w1")
    w2 = k.sb([64, 64], F32, "hy_w2")
    mlp = k.sb([64, 4], F32, "hy_mlp")
    w3 = k.sb([64, 2, 2, 192], F32, "hy_w3")
    for t_, nm in ((cw, "hy_cw"), (hbias, "hy_bias"), (w1, "hy_w1"), (w2, "hy_w2"), (mlp, "hy_mlp"), (w3, "hy_w3")):
        k.dma(t_[:], prm[nm], w=[t_])
    fsc = k.sb([64, 2], F32, "hy_fsc")
    k.op("dve", lambda e: e.tensor_scalar(out=fsc[:, 0:1], in0=mlp[:, 1:2], scalar1=1.0 / TWO_PI, scalar2=None, op0=ALU.mult), r=[mlp], w=[fsc])
    k.op("dve", lambda e: e.tensor_scalar(out=fsc[:, 1:2], in0=mlp[:, 3:4], scalar1=1.0 / TWO_PI, scalar2=None, op0=ALU.mult), r=[mlp], w=[fsc])
    h2 = {"L": k.sb([64, S], F32, "h2L"), "C": k.sb([64, NCTX], F32, "h2C")}
    ps = [k.ps() for _ in range(2)]
    mk = k.mark()
    emb = k.sb([33, 512], F32, "emb")
    h1 = k.sb([64, 512], F32, "h1")
    xa_ = k.sb([64, 512], F32, "hyx")
    xi = k.sb([64, 512], I32, "hyxi")
    xf = k.sb([64, 512], F32, "hyxf")

    def sin_layer(dst, src_ps, n, bcol, fcol):
        k.op("dve", lambda e: e.tensor_scalar(out=xa_[:, 0:n], in0=src_ps[0:64, 0:n], scalar1=mlp[:, bcol:bcol + 1], scalar2=fsc[:, fcol:fcol + 1],
                                              op0=ALU.add, op1=ALU.mult), r=[src_ps, mlp, fsc], w=[xa_])
        k.op("dve", lambda e: e.tensor_copy(out=xi[:, 0:n], in_=xa_[:, 0:n]), r=[xa_], w=[xi])
        k.op("dve", lambda e: e.tensor_copy(out=xf[:, 0:n], in_=xi[:, 0:n]), r=[xi], w=[xf])
        k.op("dve", lambda e: e.tensor_tensor(out=xf[:, 0:n], in0=xa_[:, 0:n], in1=xf[:, 0:n], op=ALU.subtract), r=[xa_, xf], w=[xf])
        k.op("dve", lambda e: e.tensor_scalar(out=xf[:, 0:n], in0=xf[:, 0:n], scalar1=0.4999999, scalar2=-0.4999999, op0=ALU.min, op1=ALU.max),
             r=[xf], w=[xf])
        k.op("act", lambda e: e.activation(out=dst, in_=xf[:, 0:n], func=AF.Sin, scale=TWO_PI), r=[xf], w=[h1, h2["L"], h2["C"]])

    for tag, n_tot in (("L", S), ("C", NCTX)):
        for (t0, n) in chunks(n_tot):
            k.dma(emb[:, 0:n], prm["hy_emb" + tag][:, t0:t0 + n], w=[emb])
            k.op("pe", lambda e, n=n: e.matmul(ps[0][0:64, 0:n], lhsT=w1[:], rhs=emb[:, 0:n], start=True, stop=True), r=[w1, emb], w=[ps[0]])
            sin_layer(h1[:, 0:n], ps[0], n, 0, 0)
            k.op("pe", lambda e, n=n: e.matmul(ps[1][0:64, 0:n], lhsT=w2[:], rhs=h1[:, 0:n], start=True, stop=True), r=[w2, h1], w=[ps[1]])
            sin_layer(h2[tag][:, t0:t0 + n], ps[1], n, 2, 1)
    k.release(mk)
    u = k.sb([128, T], F32, "hy_u")
    g = [k.sb([128, T], F32, f"hy_g{i}") for i in range(2)]
    acc = k.sb([128, T], F32, "hy_acc")
    raw = k.sb([128, T], F32, "hy_raw")
    hf = k.sb([128, S], F32, "hy_hf")
    hb = k.sb([128, S], F32, "hy_hb")
    hfc = k.sb([128, NCTX], F32, "hy_hfc")
    hbc = k.sb([128, NCTX], F32, "hy_hbc")
    dec = k.sb([128, 512], F32, "hy_dec")
    rows = [SEG["hv"][0], SEG["hx1"][0], SEG["hx2"][0]]
    for ti, P in ((0, 128), (1, 64)):
        for s_, dst in ((0, u), (1, g[0]), (2, g[1])):
            k.dma(raw[0:P, :], pT[rows[s_] + 128 * ti:rows[s_] + 128 * ti + P, :], r=[pT.tensor], w=[raw])
            emit_conv3(k, dst, raw, cw[:, ti, s_, :], cw[:, ti, s_, 3:4], P, False, wt=cw)
        for o in range(2):
            for tag, n_tot, F_, B_ in (("L", S, hf, hb), ("C", NCTX, hfc, hbc)):
                for dd_, dstf in ((0, F_), (1, B_)):
                    for ci, (t0, n) in enumerate(chunks(n_tot)):
                        p_ = ps[ci % 2]
                        k.dma(dec[0:P, 0:n], prm["hy_dec" + tag][0:P, ti, t0:t0 + n], w=[dec])
                        k.op("pe", lambda e, p_=p_, o=o, dd_=dd_, ti=ti, P=P, tag=tag, t0=t0, n=n: e.matmul(
                            p_[0:P, 0:n], lhsT=w3[:, o, dd_, 128 * ti:128 * ti + P], rhs=h2[tag][:, t0:t0 + n], start=True, stop=True),
                            r=[w3, h2[tag]], w=[p_])
                        k.op("dve", lambda e, p_=p_, dstf=dstf, P=P, t0=t0, n=n: e.tensor_tensor(out=dstf[0:P, t0:t0 + n], in0=p_[0:P, 0:n],
                                                                                             in1=dec[0:P, 0:n], op=ALU.mult), r=[p_, dec], w=[dstf])
            for (a, n_tot, F_, B_) in ((NCTX, S, hf, hb), (0, NCTX, hfc, hbc)):
                k.op("dve", lambda e, a=a, n_tot=n_tot, P=P, ti=ti, o=o: e.tensor_scalar(
                    out=acc[0:P, a:a + n_tot], in0=u[0:P, a:a + n_tot], scalar1=hbias[0:P, ti, o:o + 1], scalar2=None, op0=ALU.mult),
                    r=[u, hbias], w=[acc])
                for tau in range(n_tot):
                    L = n_tot - tau
                    k.op("dve", lambda e, a=a, tau=tau, L=L, P=P, F_=F_: e.scalar_tensor_tensor(
                        out=acc[0:P, a + tau:a + tau + L], in0=u[0:P, a:a + L], scalar=F_[0:P, tau:tau + 1], in1=acc[0:P, a + tau:a + tau + L],
                        op0=ALU.mult, op1=ALU.add), r=[u, F_, acc], w=[acc])
                    if tau > 0:
                        k.op("dve", lambda e, a=a, tau=tau, L=L, P=P, B_=B_: e.scalar_tensor_tensor(
                            out=acc[0:P, a:a + L], in0=u[0:P, a + tau:a + tau + L], scalar=B_[0:P, tau:tau + 1], in1=acc[0:P, a:a + L],
                            op0=ALU.mult, op1=ALU.add), r=[u, B_, acc], w=[acc])
            k.op("dve", lambda e, o=o, P=P: e.tensor_tensor(out=u[0:P, :], in0=acc[0:P, :], in1=g[o][0:P, :], op=ALU.mult), r=[acc, g[o]], w=[u])
        k.dma(yd[128 * ti:128 * ti + P, :], u[0:P, :], r=[u], q="pool")


NT = 64 + 1024
TCH_C = [(0, 64), (64, 512), (576, 512)]
MW = 768


def emit_cast_w(k, dst_bf, src_dram, kch, ncols, wf, it0=0):
    it = it0
    step = 256 if kch <= 8 else 128
    for c0 in range(0, ncols, step):
        n = min(step, ncols - c0)
        w_ = wf[it % 2]
        it += 1
        wv = w_[:, 0:kch * n].rearrange("p (k c) -> p k c", k=kch)
        k.dma(wv, src_dram[:, c0:c0 + n].rearrange("(k p) c -> p k c", p=128), w=[w_])
        k.op("pool", lambda e, wv=wv, c0=c0, n=n: e.tensor_copy(out=dst_bf[:, :, c0:c0 + n], in_=wv), r=[w_], w=[dst_bf])
    return it


def build_c():
    nc = bass.Bass("TRN2", target_bir_lowering=False)
    k = KB(nc)
    di = lambda nm, shp, dt=F32: nc.dram_tensor(nm, shp, dt, kind="ExternalInput").ap()
    xT = di("xT", [D, NT])
    yT = di("yT", [4, MW, NT])
    gT = di("gT", [4, D, NT])
    gluw = di("gluw", [MW, MW])
    wbr = di("wbr", [4, MW, D])
    wout = di("wout", [D, D])
    mvd = di("modv", [D, 8])
    snw = di("ssd_nw", [128, 6])
    rwd = di("rw", [D, 20])
    rbd = di("rb", [1, 20])
    xmidT = nc.dram_tensor("xmidT", [D, NT], F32, kind="ExternalOutput").ap()
    h2T = nc.dram_tensor("h2T", [D, NT], BF16, kind="ExternalOutput").ap()
    comb = nc.dram_tensor("comb", [NT, 16], F32, kind="ExternalOutput").ap()

    ones = k.sb([128, 128], F32, "ones")
    k.op("dve", lambda e: e.memset(ones[:], 1.0), w=[ones])
    epsb = k.sb([128, 1], F32, "epsb")
    k.op("dve", lambda e: e.memset(epsb[:], EPS), w=[epsb])
    mv = k.sb([128, 16, 8], F32, "mv")
    k.dma(mv[:], mvd.rearrange("(k p) r -> p k r", p=128), w=[mv])
    for col in (3, 5):
        k.op("dve", lambda e, col=col: e.tensor_scalar(out=mv[:, :, col], in0=mv[:, :, col], scalar1=1.0, scalar2=None, op0=ALU.add), r=[mv], w=[mv])
    nw = k.sb([128, 6, 1], F32, "snw")
    k.dma(nw[:, :, 0], snw, w=[nw])
    wf = [k.sb([128, 2048], F32, f"wf{i}") for i in range(2)]
    macc = k.sb([128, 16, NT], F32, "macc")
    ps = [k.ps() for _ in range(4)]
    psn = 0
    mk = k.mark()
    yf = k.sb([128, 6, NT], F32, "yf")
    ybf = k.sb([128, 6, NT], BF16, "ybf")
    t1 = k.sb([128, 6, NT], F32, "t1")
    wb = k.sb([128, 6, D], BF16, "wb")
    gw = k.sb([128, 6, MW], BF16, "gw")
    gt = [k.sb([128, 512], F32, f"gt{i}") for i in range(2)]
    tmp = [k.sb([128, 512], F32, f"tmp{i}") for i in range(2)]
    wit = 0
    git = 0
    for br in range(4):
        k.dma(yf[:], yT[br].rearrange("(k p) t -> p k t", p=128), w=[yf])
        if br == 0:
            k.op("dve", lambda e: e.tensor_tensor(out=t1[:], in0=yf[:], in1=yf[:], op=ALU.mult), r=[yf], w=[t1])
            k.op("dve", lambda e: e.tensor_scalar(out=t1[:], in0=t1[:], scalar1=0.044715, scalar2=1.0, op0=ALU.mult, op1=ALU.add), r=[t1], w=[t1])
            k.op("dve", lambda e: e.tensor_tensor(out=t1[:], in0=t1[:], in1=yf[:], op=ALU.mult), r=[t1, yf], w=[t1])
            k.op("act", lambda e: e.activation(out=t1[:], in_=t1[:], func=AF.Tanh, scale=math.sqrt(2.0 / math.pi)), r=[t1], w=[t1])
            k.op("dve", lambda e: e.tensor_scalar(out=t1[:], in0=t1[:], scalar1=1.0, scalar2=0.5, op0=ALU.add, op1=ALU.mult), r=[t1], w=[t1])
            k.op("dve", lambda e: e.tensor_tensor(out=yf[:], in0=t1[:], in1=yf[:], op=ALU.mult), r=[t1, yf], w=[yf])
            k.op("pool", lambda e: e.tensor_copy(out=ybf[:], in_=yf[:]), r=[yf], w=[ybf])
            wit = emit_cast_w(k, gw, gluw, 6, MW, wf, wit)
            for oc in range(6):
                for (t0, n) in TCH_C:
                    p_ = ps[psn % 4]
                    psn += 1
                    for kk in range(6):
                        k.op("pe", lambda e, p_=p_, oc=oc, kk=kk, t0=t0, n=n: e.matmul(p_[:, 0:n], lhsT=gw[:, kk, oc * 128:(oc + 1) * 128],
                                                                                     rhs=ybf[:, kk, t0:t0 + n], start=(kk == 0), stop=(kk == 5)),
                             r=[gw, ybf], w=[p_])
                    k.op("act", lambda e, p_=p_, oc=oc, t0=t0, n=n: e.activation(out=t1[:, oc, t0:t0 + n], in_=p_[:, 0:n], func=AF.Sigmoid),
                         r=[p_], w=[t1])
            k.op("dve", lambda e: e.tensor_tensor(out=yf[:], in0=yf[:], in1=t1[:], op=ALU.mult), r=[yf, t1], w=[yf])
        if br == 1:
            for (t0, n) in TCH_C:
                p_ = ps[psn % 4]
                psn += 1
                for kk in range(6):
                    k.op("act", lambda e, kk=kk, t0=t0, n=n: e.activation(out=t1[:, kk, t0:t0 + n], in_=yf[:, kk, t0:t0 + n], func=AF.Square),
                         r=[yf], w=[t1])
                    k.op("pe", lambda e, p_=p_, kk=kk, t0=t0, n=n: e.matmul(p_[:, 0:n], lhsT=ones[:], rhs=t1[:, kk, t0:t0 + n],
                                                                          start=(kk == 0), stop=(kk == 5)), r=[ones, t1], w=[p_])
                k.op("act", lambda e, p_=p_, t0=t0, n=n: e.activation(out=t1[:, 0, t0:t0 + n], in_=p_[:, 0:n], func=AF.Sqrt, bias=epsb[:], scale=1.0 / MW),
                     r=[p_, epsb], w=[t1])
            k.op("dve", lambda e: e.reciprocal(out=t1[:, 0, :], in_=t1[:, 0, :]), r=[t1], w=[t1])
            for kk in range(6):
                k.op("dve", lambda e, kk=kk: e.scalar_tensor_tensor(out=yf[:, kk, :], in0=yf[:, kk, :], scalar=nw[:, kk, 0:1], in1=t1[:, 0, :],
                                                                   op0=ALU.mult, op1=ALU.mult), r=[yf, nw, t1], w=[yf])
        k.op("pool", lambda e: e.tensor_copy(out=ybf[:], in_=yf[:]), r=[yf], w=[ybf])
        wit = emit_cast_w(k, wb, wbr[br], 6, D, wf, wit)
        for dc in range(16):
            for (t0, n) in TCH_C:
                p_ = ps[psn % 4]
                psn += 1
                g_ = gt[git % 2]
                m_ = tmp[git % 2]
                git += 1
                k.dma(g_[:, 0:n], gT[br, dc * 128:(dc + 1) * 128, t0:t0 + n], w=[g_])
                for kk in range(6):
                    k.op("pe", lambda e, p_=p_, dc=dc, kk=kk, t0=t0, n=n: e.matmul(p_[:, 0:n], lhsT=wb[:, kk, dc * 128:(dc + 1) * 128],
                                                                                 rhs=ybf[:, kk, t0:t0 + n], start=(kk == 0), stop=(kk == 5)),
                         r=[wb, ybf], w=[p_])
                if br == 0:
                    k.op("dve", lambda e, p_=p_, g_=g_, dc=dc, t0=t0, n=n: e.tensor_tensor(out=macc[:, dc, t0:t0 + n], in0=p_[:, 0:n], in1=g_[:, 0:n],
                                                                                         op=ALU.mult), r=[p_, g_], w=[macc])
                else:
                    k.op("dve", lambda e, p_=p_, g_=g_, m_=m_, n=n: e.tensor_tensor(out=m_[:, 0:n], in0=p_[:, 0:n], in1=g_[:, 0:n], op=ALU.mult),
                         r=[p_, g_], w=[m_])
                    k.op("pool", lambda e, m_=m_, dc=dc, t0=t0, n=n: e.tensor_tensor(out=macc[:, dc, t0:t0 + n], in0=macc[:, dc, t0:t0 + n],
                                                                                   in1=m_[:, 0:n], op=ALU.add), r=[m_, macc], w=[macc])
    k.release(mk)
    mk2 = k.mark()
    mbf = k.sb([128, 16, NT], BF16, "mbf")
    k.op("pool", lambda e: e.tensor_copy(out=mbf[:], in_=macc[:]), r=[macc], w=[mbf])
    xmid = macc
    wo = [k.sb([128, 16, 128], BF16, f"wo{i}") for i in range(2)]
    xin = [k.sb([128, NT], F32, f"xin{i}") for i in range(2)]
    for dc in range(16):
        w_ = wf[dc % 2]
        wo_ = wo[dc % 2]
        x_ = xin[dc % 2]
        wv = w_[:].rearrange("p (k c) -> p k c", k=16)
        k.dma(wv, wout[:, dc * 128:(dc + 1) * 128].rearrange("(k p) c -> p k c", p=128), w=[w_])
        k.op("pool", lambda e, wv=wv, wo_=wo_: e.tensor_copy(out=wo_[:], in_=wv), r=[w_], w=[wo_])
        k.dma(x_[:], xT[dc * 128:(dc + 1) * 128, :], w=[x_])
        for (t0, n) in TCH_C:
            p_ = ps[psn % 4]
            psn += 1
            for kk in range(16):
                k.op("pe", lambda e, p_=p_, wo_=wo_, kk=kk, t0=t0, n=n: e.matmul(p_[:, 0:n], lhsT=wo_[:, kk, :], rhs=mbf[:, kk, t0:t0 + n],
                                                                               start=(kk == 0), stop=(kk == 15)), r=[wo_, mbf], w=[p_])
            gcol = 1 if t0 == 0 else 0
            k.op("dve", lambda e, p_=p_, x_=x_, dc=dc, t0=t0, n=n, gcol=gcol: e.scalar_tensor_tensor(
                out=xmid[:, dc, t0:t0 + n], in0=p_[:, 0:n], scalar=mv[:, dc, gcol:gcol + 1], in1=x_[:, t0:t0 + n], op0=ALU.mult, op1=ALU.add),
                r=[p_, mv, x_, mbf], w=[xmid])
        k.dma(xmidT[dc * 128:(dc + 1) * 128, :], xmid[:, dc, :], r=[xmid], q="pool")
    k.release(mk2)
    h2 = k.sb([128, 16, NT], F32, "h2")
    rstd = k.sb([128, NT], F32, "rstd")
    sq = [k.sb([128, 512], F32, f"sq{i}") for i in range(2)]
    for (t0, n) in TCH_C:
        p_ = ps[psn % 4]
        psn += 1
        for kk in range(16):
            s_ = sq[kk % 2]
            k.op("act", lambda e, s_=s_, kk=kk, t0=t0, n=n: e.activation(out=s_[:, 0:n], in_=xmid[:, kk, t0:t0 + n], func=AF.Square), r=[xmid], w=[s_])
            k.op("pe", lambda e, p_=p_, s_=s_, kk=kk, n=n: e.matmul(p_[:, 0:n], lhsT=ones[:], rhs=s_[:, 0:n], start=(kk == 0), stop=(kk == 15)),
                 r=[ones, s_], w=[p_])
        k.op("act", lambda e, p_=p_, t0=t0, n=n: e.activation(out=rstd[:, t0:t0 + n], in_=p_[:, 0:n], func=AF.Sqrt, bias=epsb[:], scale=1.0 / D),
             r=[p_, epsb], w=[rstd])
    k.op("dve", lambda e: e.reciprocal(out=rstd[:], in_=rstd[:]), r=[rstd], w=[rstd])
    hb = [k.sb([128, NT], BF16, f"hb{i}") for i in range(2)]
    for kk in range(16):
        k.op("dve", lambda e, kk=kk: e.tensor_tensor(out=h2[:, kk, :], in0=xmid[:, kk, :], in1=rstd[:], op=ALU.mult), r=[xmid, rstd], w=[h2])
        for (t0, n) in TCH_C:
            mo = 4 if t0 == 0 else 2
            k.op("dve", lambda e, kk=kk, t0=t0, n=n, mo=mo: e.tensor_scalar(out=h2[:, kk, t0:t0 + n], in0=h2[:, kk, t0:t0 + n],
                                                                           scalar1=mv[:, kk, mo + 1:mo + 2], scalar2=mv[:, kk, mo:mo + 1],
                                                                           op0=ALU.mult, op1=ALU.add), r=[h2, mv], w=[h2])
        hb_ = hb[kk % 2]
        k.op("pool", lambda e, hb_=hb_, kk=kk: e.tensor_copy(out=hb_[:], in_=h2[:, kk, :]), r=[h2], w=[hb_])
        k.dma(h2T[kk * 128:(kk + 1) * 128, :], hb_[:], r=[hb_], q="pool")
    rw = k.sb([128, 16, 20], F32, "rw")
    rb = k.sb([1, 20], F32, "rb")
    k.dma(rw[:], rwd.rearrange("(k p) c -> p k c", p=128), w=[rw])
    k.dma(rb[:], rbd, w=[rb])
    lg = k.sb([128, 20], F32, "lg")
    sm = {nm: k.sb([128, 4], F32, "r_" + nm) for nm in ("ohg", "eg", "esel", "oh1", "msk", "oh2", "within")}
    sc = {nm: k.sb([128, 1], F32, "r_" + nm) for nm in ("gmax", "ngmax", "ssum", "pg", "m1", "nm1", "m2", "e2", "den", "w1", "w2")}
    cb = [k.sb([128, 16], F32, f"cb{i}") for i in range(2)]
    V = lambda e: e
    for ti, (t0, n) in enumerate([(0, 64)] + [(64 + 128 * i, 128) for i in range(8)]):
        p_ = ps[psn % 4]
        psn += 1
        for kk in range(16):
            k.op("pe", lambda e, p_=p_, kk=kk, t0=t0, n=n: e.matmul(p_[0:n, 0:20], lhsT=h2[:, kk, t0:t0 + n], rhs=rw[:, kk, :],
                                                                   start=(kk == 0), stop=False), r=[h2, rw], w=[p_])
        k.op("pe", lambda e, p_=p_, n=n: e.matmul(p_[0:n, 0:20], lhsT=ones[0:1, 0:n], rhs=rb[0:1, :], start=False, stop=True), r=[ones, rb], w=[p_])
        k.op("act", lambda e, p_=p_, n=n: e.activation(out=lg[0:n, :], in_=p_[0:n, 0:20], func=AF.Copy), r=[p_], w=[lg])
        P = n
        o = lambda eng, fn, r, w: k.op(eng, fn, r=r, w=w)
        o("dve", lambda e: e.reduce_max(out=sc["gmax"][0:P], in_=lg[0:P, 0:4], axis=AX.X), [lg], [sc["gmax"]])
        o("dve", lambda e: e.tensor_scalar(out=sm["ohg"][0:P], in0=lg[0:P, 0:4], scalar1=sc["gmax"][0:P, 0:1], scalar2=None, op0=ALU.is_ge),
          [lg, sc["gmax"]], [sm["ohg"]])
        o("dve", lambda e: e.tensor_scalar(out=sc["ngmax"][0:P], in0=sc["gmax"][0:P], scalar1=-1.0, scalar2=None, op0=ALU.mult), [sc["gmax"]], [sc["ngmax"]])
        o("act", lambda e: e.activation(out=sm["eg"][0:P], in_=lg[0:P, 0:4], func=AF.Exp, bias=sc["ngmax"][0:P], scale=1.0), [lg, sc["ngmax"]], [sm["eg"]])
        o("dve", lambda e: e.reduce_sum(out=sc["ssum"][0:P], in_=sm["eg"][0:P], axis=AX.X), [sm["eg"]], [sc["ssum"]])
        o("dve", lambda e: e.reciprocal(out=sc["pg"][0:P], in_=sc["ssum"][0:P]), [sc["ssum"]], [sc["pg"]])
        o("dve", lambda e: e.tensor_scalar(out=sm["esel"][0:P], in0=lg[0:P, 4:8], scalar1=sm["ohg"][0:P, 0:1], scalar2=None, op0=ALU.mult),
          [lg, sm["ohg"]], [sm["esel"]])
        for g_ in range(1, 4):
            o("dve", lambda e, g_=g_: e.scalar_tensor_tensor(out=sm["esel"][0:P], in0=lg[0:P, 4 + 4 * g_:8 + 4 * g_], scalar=sm["ohg"][0:P, g_:g_ + 1],
                                                           in1=sm["esel"][0:P], op0=ALU.mult, op1=ALU.add), [lg, sm["ohg"], sm["esel"]], [sm["esel"]])
        o("dve", lambda e: e.reduce_max(out=sc["m1"][0:P], in_=sm["esel"][0:P], axis=AX.X), [sm["esel"]], [sc["m1"]])
        o("dve", lambda e: e.tensor_scalar(out=sm["oh1"][0:P], in0=sm["esel"][0:P], scalar1=sc["m1"][0:P, 0:1], scalar2=None, op0=ALU.is_ge),
          [sm["esel"], sc["m1"]], [sm["oh1"]])
        o("dve", lambda e: e.scalar_tensor_tensor(out=sm["msk"][0:P], in0=sm["oh1"][0:P], scalar=-1.0e30, in1=sm["esel"][0:P], op0=ALU.mult, op1=ALU.add),
          [sm["oh1"], sm["esel"]], [sm["msk"]])
        o("dve", lambda e: e.reduce_max(out=sc["m2"][0:P], in_=sm["msk"][0:P], axis=AX.X), [sm["msk"]], [sc["m2"]])
        o("dve", lambda e: e.tensor_scalar(out=sm["oh2"][0:P], in0=sm["msk"][0:P], scalar1=sc["m2"][0:P, 0:1], scalar2=None, op0=ALU.is_ge),
          [sm["msk"], sc["m2"]], [sm["oh2"]])
        o("dve", lambda e: e.tensor_scalar(out=sc["nm1"][0:P], in0=sc["m1"][0:P], scalar1=-1.0, scalar2=None, op0=ALU.mult), [sc["m1"]], [sc["nm1"]])
        o("act", lambda e: e.activation(out=sc["e2"][0:P], in_=sc["m2"][0:P], func=AF.Exp, bias=sc["nm1"][0:P], scale=1.0), [sc["m2"], sc["nm1"]], [sc["e2"]])
        o("dve", lambda e: e.tensor_scalar(out=sc["den"][0:P], in0=sc["e2"][0:P], scalar1=1.0, scalar2=None, op0=ALU.add), [sc["e2"]], [sc["den"]])
        o("dve", lambda e: e.reciprocal(out=sc["den"][0:P], in_=sc["den"][0:P]), [sc["den"]], [sc["den"]])
        o("dve", lambda e: e.tensor_tensor(out=sc["w1"][0:P], in0=sc["den"][0:P], in1=sc["pg"][0:P], op=ALU.mult), [sc["den"], sc["pg"]], [sc["w1"]])
        o("dve", lambda e: e.tensor_tensor(out=sc["w2"][0:P], in0=sc["w1"][0:P], in1=sc["e2"][0:P], op=ALU.mult), [sc["w1"], sc["e2"]], [sc["w2"]])
        o("dve", lambda e: e.tensor_scalar(out=sm["within"][0:P], in0=sm["oh1"][0:P], scalar1=sc["w1"][0:P, 0:1], scalar2=None, op0=ALU.mult),
          [sm["oh1"], sc["w1"]], [sm["within"]])
        o("dve", lambda e: e.scalar_tensor_tensor(out=sm["within"][0:P], in0=sm["oh2"][0:P], scalar=sc["w2"][0:P, 0:1], in1=sm["within"][0:P],
                                                  op0=ALU.mult, op1=ALU.add), [sm["oh2"], sc["w2"], sm["within"]], [sm["within"]])
        c_ = cb[ti % 2]
        for g_ in range(4):
            o("dve", lambda e, g_=g_, c_=c_: e.tensor_scalar(out=c_[0:P, 4 * g_:4 * g_ + 4], in0=sm["within"][0:P], scalar1=sm["ohg"][0:P, g_:g_ + 1],
                                                           scalar2=None, op0=ALU.mult), [sm["within"], sm["ohg"]], [c_])
        k.dma(comb[t0:t0 + n, :], c_[0:P, :], r=[c_], q="pool")
    k.emit()
    return nc


TT = B * T
FF = 1024


def build_e():
    nc = bass.Bass("TRN2", target_bir_lowering=False)
    k = KB(nc)
    h2T = nc.dram_tensor("h2T", [D, TT], BF16, kind="ExternalInput").ap()
    cmb = nc.dram_tensor("cmb", [2, 128, TT], F32, kind="ExternalInput").ap()
    wg = nc.dram_tensor("wg", [2, D, FF], F32, kind="ExternalInput").ap()
    wu = nc.dram_tensor("wu", [2, D, FF], F32, kind="ExternalInput").ap()
    wd = nc.dram_tensor("wd", [2, FF, D], F32, kind="ExternalInput").ap()
    part = nc.dram_tensor("part", [D, TT], F32, kind="ExternalOutput").ap()
    Wg = k.sb([128, 16, FF], BF16, "Wg")
    Wu = k.sb([128, 16, FF], BF16, "Wu")
    Wd = k.sb([128, 8, D], BF16, "Wd")
    wf = [k.sb([128, 2048], F32, f"wf{i}") for i in range(2)]
    hch = [k.sb([128, 16, 512], BF16, f"hch{i}") for i in range(2)]
    abf = [k.sb([128, 8, 512], BF16, f"abf{i}") for i in range(2)]
    cch = [k.sb([128, 512], F32, f"cch{i}") for i in range(2)]
    tmp = [k.sb([128, 512], F32, f"tmp{i}") for i in range(2)]
    ost = [k.sb([128, 512], F32, f"ost{i}") for i in range(3)]
    prv = [k.sb([128, 512], F32, f"prv{i}") for i in range(3)]
    pg = [k.ps() for _ in range(2)]
    pu = [k.ps() for _ in range(2)]
    po = [k.ps() for _ in range(3)]
    wit = 0
    io = 0
    for ex in range(2):
        wit = emit_cast_w(k, Wg, wg[ex], 16, FF, wf, wit)
        wit = emit_cast_w(k, Wu, wu[ex], 16, FF, wf, wit)
        wit = emit_cast_w(k, Wd, wd[ex], 8, D, wf, wit)
        for tc in range(TT // 512):
            t0 = tc * 512
            h_ = hch[tc % 2]
            a_ = abf[tc % 2]
            c_ = cch[tc % 2]
            k.dma(h_[:], h2T[:, t0:t0 + 512].rearrange("(k p) t -> p k t", p=128), w=[h_])
            k.dma(c_[:], cmb[ex, :, t0:t0 + 512], w=[c_])
            for fc in range(8):
                g_ = pg[fc % 2]
                u_ = pu[fc % 2]
                m_ = tmp[fc % 2]
                for kk in range(16):
                    k.op("pe", lambda e, g_=g_, h_=h_, fc=fc, kk=kk: e.matmul(g_[:], lhsT=Wg[:, kk, fc * 128:(fc + 1) * 128], rhs=h_[:, kk, :],
                                                                            start=(kk == 0), stop=(kk == 15)), r=[Wg, h_], w=[g_])
                for kk in range(16):
                    k.op("pe", lambda e, u_=u_, h_=h_, fc=fc, kk=kk: e.matmul(u_[:], lhsT=Wu[:, kk, fc * 128:(fc + 1) * 128], rhs=h_[:, kk, :],
                                                                            start=(kk == 0), stop=(kk == 15)), r=[Wu, h_], w=[u_])
                k.op("act", lambda e, g_=g_, m_=m_: e.activation(out=m_[:], in_=g_[:], func=AF.Silu), r=[g_], w=[m_])
                k.op("dve", lambda e, u_=u_, m_=m_: e.tensor_tensor(out=m_[:], in0=m_[:], in1=u_[:], op=ALU.mult), r=[m_, u_], w=[m_])
                k.op("pool", lambda e, m_=m_, a_=a_, c_=c_, fc=fc: e.tensor_tensor(out=a_[:, fc, :], in0=m_[:], in1=c_[:], op=ALU.mult),
                     r=[m_, c_], w=[a_])
            for dc in range(16):
                o_ = po[io % 3]
                s_ = ost[io % 3]
                p_ = prv[io % 3]
                io += 1
                key = f"part_{dc}_{tc}"
                for fc in range(8):
                    k.op("pe", lambda e, o_=o_, a_=a_, dc=dc, fc=fc: e.matmul(o_[:], lhsT=Wd[:, fc, dc * 128:(dc + 1) * 128], rhs=a_[:, fc, :],
                                                                            start=(fc == 0), stop=(fc == 7)), r=[Wd, a_], w=[o_])
                if ex == 0:
                    k.op("act", lambda e, o_=o_, s_=s_: e.activation(out=s_[:], in_=o_[:], func=AF.Copy), r=[o_], w=[s_])
                else:
                    k.dma(p_[:], part[dc * 128:(dc + 1) * 128, t0:t0 + 512], r=[key], w=[p_])
                    k.op("dve", lambda e, o_=o_, s_=s_, p_=p_: e.tensor_tensor(out=s_[:], in0=o_[:], in1=p_[:], op=ALU.add), r=[o_, p_], w=[s_])
                k.dma(part[dc * 128:(dc + 1) * 128, t0:t0 + 512], s_[:], r=[s_], w=[key], q="pool")
    k.emit()
    return nc


def run_e(inp, li, h2T, comb):
    maps = []
    for e in range(NCORES):
        cb = np.ascontiguousarray(np.broadcast_to(comb[:, 2 * e:2 * e + 2].T[:, None, :], (2, 128, TT)))
        maps.append({"h2T": h2T, "cmb": cb, "wg": inp["moe_w_gate"][li][2 * e:2 * e + 2], "wu": inp["moe_w_up"][li][2 * e:2 * e + 2],
                     "wd": inp["moe_w_down"][li][2 * e:2 * e + 2]})
    res = _run(build_e(), maps)
    return [r["part"] for r in res]


def build_r():
    nc = bass.Bass("TRN2", target_bir_lowering=False)
    k = KB(nc)
    xmidT = nc.dram_tensor("xmidT", [D, NT], F32, kind="ExternalInput").ap()
    parts = nc.dram_tensor("parts", [NCORES, D, NT], F32, kind="ExternalInput").ap()
    gvd = nc.dram_tensor("gv", [D, 4], F32, kind="ExternalInput").ap()
    xendT = nc.dram_tensor("xendT", [D, NT], F32, kind="ExternalOutput").ap()
    outT = nc.dram_tensor("outT", [D, NT], F32, kind="ExternalOutput").ap()
    ones = k.sb([128, 128], F32, "ones")
    k.op("dve", lambda e: e.memset(ones[:], 1.0), w=[ones])
    epsb = k.sb([128, 1], F32, "epsb")
    k.op("dve", lambda e: e.memset(epsb[:], EPS), w=[epsb])
    gv = k.sb([128, 16, 4], F32, "gv")
    k.dma(gv[:], gvd.rearrange("(k p) r -> p k r", p=128), w=[gv])
    xend = k.sb([128, 16, NT], F32, "xend")
    pb = [[k.sb([128, NT], F32, f"pb{i}_{e}") for e in range(NCORES)] for i in range(2)]
    xm = [k.sb([128, NT], F32, f"xm{i}") for i in range(2)]
    ps = [k.ps() for _ in range(3)]
    for dc in range(16):
        bufs = pb[dc % 2]
        x_ = xm[dc % 2]
        k.dma(x_[:], xmidT[dc * 128:(dc + 1) * 128, :], w=[x_])
        for e_ in range(NCORES):
            k.dma(bufs[e_][:], parts[e_, dc * 128:(dc + 1) * 128, :], w=[bufs[e_]])
        for (a_, b_, eng) in ((0, 1, "dve"), (2, 3, "pool"), (4, 5, "dve"), (6, 7, "pool"), (0, 2, "dve"), (4, 6, "pool"), (0, 4, "dve")):
            k.op(eng, lambda e, a_=a_, b_=b_, bufs=bufs: e.tensor_tensor(out=bufs[a_][:], in0=bufs[a_][:], in1=bufs[b_][:], op=ALU.add),
                 r=[bufs[a_], bufs[b_]], w=[bufs[a_]])
        for (t0, n, gc) in ((0, 64, 1), (64, 1024, 0)):
            k.op("dve", lambda e, bufs=bufs, x_=x_, dc=dc, t0=t0, n=n, gc=gc: e.scalar_tensor_tensor(
                out=xend[:, dc, t0:t0 + n], in0=bufs[0][:, t0:t0 + n], scalar=gv[:, dc, gc:gc + 1], in1=x_[:, t0:t0 + n], op0=ALU.mult, op1=ALU.add),
                r=[bufs[0], gv, x_], w=[xend])
        k.dma(xendT[dc * 128:(dc + 1) * 128, :], xend[:, dc, :], r=[xend], q="pool")
    rstd = k.sb([128, NT], F32, "rstd")
    sq = [k.sb([128, 512], F32, f"sq{i}") for i in range(2)]
    for ci, (t0, n) in enumerate(TCH_C):
        p_ = ps[ci % 3]
        for kk in range(16):
            s_ = sq[kk % 2]
            k.op("act", lambda e, s_=s_, kk=kk, t0=t0, n=n: e.activation(out=s_[:, 0:n], in_=xend[:, kk, t0:t0 + n], func=AF.Square), r=[xend], w=[s_])
            k.op("pe", lambda e, p_=p_, s_=s_, kk=kk, n=n: e.matmul(p_[:, 0:n], lhsT=ones[:], rhs=s_[:, 0:n], start=(kk == 0), stop=(kk == 15)),
                 r=[ones, s_], w=[p_])
        k.op("act", lambda e, p_=p_, t0=t0, n=n: e.activation(out=rstd[:, t0:t0 + n], in_=p_[:, 0:n], func=AF.Sqrt, bias=epsb[:], scale=1.0 / D),
             r=[p_, epsb], w=[rstd])
    k.op("dve", lambda e: e.reciprocal(out=rstd[:], in_=rstd[:]), r=[rstd], w=[rstd])
    ob = pb[0]
    for kk in range(16):
        o_ = ob[kk % 4]
        k.op("dve", lambda e, o_=o_, kk=kk: e.scalar_tensor_tensor(out=o_[:], in0=xend[:, kk, :], scalar=gv[:, kk, 2:3], in1=rstd[:],
                                                                 op0=ALU.mult, op1=ALU.mult), r=[xend, gv, rstd], w=[o_])
        k.dma(outT[kk * 128:(kk + 1) * 128, :], o_[:], r=[o_], q="pool")
    k.emit()
    return nc


def run_r(inp, mods, li, xmid, parts):
    m = mods[li]
    maps = []
    for core in range(NCORES):
        b, q = core // 4, core % 4
        idx = _tok_idx(q)
        gv = np.stack([m[b, 5 * D:6 * D], m[2, 5 * D:6 * D], inp["final_norm_w"], np.zeros(D, np.float32)], 1)
        maps.append({"xmidT": np.ascontiguousarray(xmid[b, idx].T),
                     "parts": np.ascontiguousarray(np.stack([p[:, b * T + idx] for p in parts], 0)),
                     "gv": np.ascontiguousarray(gv)})
    res = _run(build_r(), maps)
    xend = np.zeros((B, T, D), np.float32)
    out = np.zeros((B, T, D), np.float32)
    for core in range(NCORES):
        b, q = core // 4, core % 4
        idx = _tok_idx(q)
        xend[b, idx] = res[core]["xendT"].T
        out[b, idx] = res[core]["outT"].T
    return xend, out


def _colmajor(a):
    return a.reshape(64, 64, -1).transpose(1, 0, 2).reshape(S, -1)


ALL_PARTS = ("A", "S5", "SSD", "GLA", "HY")


def run_mixers(inp, mods, li, x_lat, x_ctx, parts=ALL_PARTS):
    maps = []
    for core in range(NCORES):
        b, j = core // 4, core % 4
        xT = np.ascontiguousarray(np.concatenate([x_ctx[b], x_lat[b]], 0).T)
        xTc = np.ascontiguousarray(np.concatenate([x_ctx[b], _colmajor(x_lat[b])], 0).T)
        cols, _ = core_cols(j)
        m = mods[li]
        modv = np.stack([m[b, 0:D], m[b, D:2 * D], m[2, 0:D], m[2, D:2 * D]], 1)
        mp = {"xT": xT, "xTc": xTc, "modv": np.ascontiguousarray(modv), "wA": np.ascontiguousarray(inp["w_in"][li][:, cols])}
        mp.update(host_consts())
        mp.update(scan_consts())
        mp.update(s5_host_params(inp, li, j))
        mp.update(ssd_host_params(inp, li, j))
        mp.update(gla_host_params(inp, li, j))
        mp.update(hy_host_params(inp, li, j))
        maps.append(mp)
    nc = build_am(parts=parts)
    return _run(nc, maps)


def assemble_mixers(res):
    ys, gs = [], []
    for b in range(B):
        r = res[4 * b:4 * b + 4]
        ya = np.concatenate([r[j]["ya"] for j in range(4)], 0)
        yb = np.concatenate([r[j]["yb"] for j in range(4)], 0)
        lat = yb[:, NCTX:].reshape(MW, 64, 64).transpose(0, 2, 1).reshape(MW, S)
        yb = np.concatenate([yb[:, :NCTX], lat], 1)
        yc = np.concatenate([r[0]["yc"][0:128], r[1]["yc"][0:128], r[2]["yc"][0:128], r[3]["yc"][0:128],
                             r[0]["yc"][128:256], r[1]["yc"][128:256]], 0)
        yd = np.concatenate([r[j]["yd"] for j in range(4)], 0)
        ys.append(np.stack([ya, yb, yc, yd], 0))
        gs.append(np.stack([r[j]["gates"] for j in range(4)], 0))
    return ys, gs


def _tok_idx(q):
    return np.concatenate([np.arange(64 * q, 64 * q + 64), NCTX + np.arange(1024 * q, 1024 * q + 1024)])


def run_c(inp, mods, li, x_lat, x_ctx, res_am):
    ys, gs = assemble_mixers(res_am)
    m = mods[li]
    maps = []
    rw = np.ascontiguousarray(np.concatenate([inp["moe_group_w"][li], inp["moe_expert_w"][li]], 1))
    rb = np.ascontiguousarray(np.concatenate([inp["moe_group_b"][li], inp["moe_expert_b"][li]])[None, :])
    for core in range(NCORES):
        b, q = core // 4, core % 4
        idx = _tok_idx(q)
        xall = np.concatenate([x_ctx[b], x_lat[b]], 0)
        z = np.zeros(D, np.float32)
        modv = np.stack([m[b, 2 * D:3 * D], m[2, 2 * D:3 * D], m[b, 3 * D:4 * D], m[b, 4 * D:5 * D], m[2, 3 * D:4 * D], m[2, 4 * D:5 * D], z, z], 1)
        maps.append({"xT": np.ascontiguousarray(xall[idx].T), "yT": np.ascontiguousarray(ys[b][:, :, idx]),
                     "gT": np.ascontiguousarray(gs[b][:, :, idx]), "gluw": inp["s5_glu_w"][li], "wbr": inp["w_branch"][li],
                     "wout": inp["w_out"][li], "modv": np.ascontiguousarray(modv),
                     "ssd_nw": np.ascontiguousarray(inp["ssd_norm_w"][li].reshape(6, 128).T), "rw": rw, "rb": rb})
    res = _run(build_c(), maps)
    xmid = np.zeros((B, T, D), np.float32)
    h2T = np.zeros((D, B * T), ml_dtypes.bfloat16)
    comb = np.zeros((B * T, 16), np.float32)
    for core in range(NCORES):
        b, q = core // 4, core % 4
        idx = _tok_idx(q)
        xmid[b, idx] = res[core]["xmidT"].T
        h2T[:, b * T + idx] = res[core]["h2T"]
        comb[b * T + idx] = res[core]["comb"]
    return xmid, h2T, comb


def kernel(**inputs):
    inp = {k_: np.asarray(v) for k_, v in inputs.items()}
    mods = run_mods(inp["c"], inp["c_ctx"], inp["ada_w"], inp["ada_b"])
    x_lat, x_ctx = inp["x"], inp["ctx"]
    out = None
    for li in range(DEPTH):
        res = run_mixers(inp, mods, li, x_lat, x_ctx)
        xmid, h2T, comb = run_c(inp, mods, li, x_lat, x_ctx, res)
        del res
        parts = run_e(inp, li, h2T, comb)
        xend, out = run_r(inp, mods, li, xmid, parts)
        del parts
        x_ctx, x_lat = xend[:, :NCTX], xend[:, NCTX:]
    return np.ascontiguousarray(out[:, NCTX:])
```

```python
import math
from contextlib import ExitStack
import numpy as np
import ml_dtypes
import concourse.bass as bass
import concourse.mybir as mybir
from concourse.bass_utils import run_bass_kernel_spmd

F32 = mybir.dt.float32
BF16 = mybir.dt.bfloat16
I32 = mybir.dt.int32
AF = mybir.ActivationFunctionType
ALU = mybir.AluOpType
AX = mybir.AxisListType

D = 2048
B = 2
S = 4096
DEPTH = 2
NCTX = 256
T = NCTX + S
EPS = 1e-6
NCORES = 8


class _Op:
    pass


class Tile:
    def __init__(self, ap, name):
        self.ap = ap
        self.name = name

    def __getitem__(self, idx):
        return self.ap[idx]


class _Rec:
    def __init__(self):
        self.call = None

    def __getattr__(self, name):
        def f(*a, **kw):
            self.call = (name, a, kw)
            return self
        return f


class KB:
    COMPUTE = ("pe", "dve", "act", "pool")

    def __init__(self, nc, n_dma_sems=20):
        self.nc = nc
        self.es = ExitStack()
        self.eng = {"pe": nc.tensor, "dve": nc.vector, "act": nc.scalar, "pool": nc.gpsimd, "sp": nc.sync}
        self.ops = []
        self.lastw = {}
        self.reads = {}
        self.n_dma_sems = n_dma_sems
        self._uid = 0
        self.psum_banks = None
        self.bar_from = 0

    ARENA_WORDS = 52500

    def _arena(self):
        if getattr(self, "arena", None) is None:
            self.arena = self.es.enter_context(self.nc.sbuf_tensor("arena", [128, self.ARENA_WORDS], F32))
            self.top = 0
            self.psum = [self.es.enter_context(self.nc.psum_tensor(f"psb{i}", [128, 512], F32)) for i in range(8)]
            self.psn = 0
        return self.arena

    def sb(self, shape, dtype=F32, name=None):
        ar = self._arena()
        self._uid += 1
        name = f"{name or 't'}_{self._uid}"
        P = shape[0]
        n = int(np.prod(shape[1:]))
        esz = 2 if dtype == BF16 else 4
        words = (n * esz + 3) // 4
        assert self.top + words <= self.ARENA_WORDS, f"arena overflow allocating {name} {shape}: top={self.top}"
        ap = ar[0:P, self.top:self.top + words]
        self.top += words
        if dtype != F32:
            ap = ap.bitcast(dtype)
        if esz == 2 and (n % 2):
            ap = ap[:, 0:n]
        if len(shape) == 3:
            ap = ap.rearrange("p (a b) -> p a b", a=shape[1])
        elif len(shape) == 4:
            ap = ap.rearrange("p (a b c) -> p a b c", a=shape[1], b=shape[2])
        return Tile(ap, name)

    def ps(self, shape=(128, 512), dtype=F32, name=None):
        self._arena()
        t = self.psum[self.psn % 8]
        self.psn += 1
        assert self.psn <= 8, "only 8 PSUM banks"
        return t

    def mark(self):
        self._arena()
        return (self.top, self.psn)

    def release(self, mark):
        self.barrier()
        self.top, self.psn = mark

    def barrier(self):
        last = {}
        dmas = []
        for o in self.ops[self.bar_from:]:
            if o.isdma:
                dmas.append(o)
            else:
                last[o.eng] = o
        deps = list(last.values()) + dmas
        for e in ("pe", "dve", "act", "pool", "sp"):
            o = self.op(e, lambda en: en.nop())
            o.deps = list(deps)
        self.bar_from = len(self.ops)
        self.lastw = {}
        self.reads = {}

    def dram(self, name, shape, dtype=F32, kind="Internal"):
        return self.nc.dram_tensor(name, list(shape), dtype, kind=kind)

    @staticmethod
    def _key(t):
        return t if isinstance(t, str) else t.name

    def op(self, eng, fn, r=(), w=()):
        o = _Op()
        o.eng = eng
        rec = _Rec()
        fn(rec)
        name_, a_, kw_ = rec.call
        o.fn = lambda e: getattr(e, name_)(*a_, **kw_)
        o.needed = False
        o.sig = None
        o.isdma = False
        o.seq = len(self.ops)
        deps = []
        for t in r:
            k = self._key(t)
            lw = self.lastw.get(k)
            if lw is not None:
                deps.append(lw)
        for t in w:
            k = self._key(t)
            lw = self.lastw.get(k)
            if lw is not None:
                deps.append(lw)
            deps.extend(self.reads.get(k, ()))
        dd = []
        seen = set()
        for d in deps:
            if d.seq in seen:
                continue
            seen.add(d.seq)
            if eng == "pe" and d.eng == "pe" and not d.isdma:
                continue
            dd.append(d)
        o.deps = dd
        for t in w:
            k = self._key(t)
            self.lastw[k] = o
            self.reads[k] = []
        for t in r:
            k = self._key(t)
            self.reads.setdefault(k, []).append(o)
        self.ops.append(o)
        return o

    def dma(self, out, in_, r=(), w=(), q="sp", **kw):
        o = self.op(q, lambda e: e.dma_start(out=out, in_=in_, **kw), r=r, w=w)
        o.isdma = True
        return o

    def emit(self, final_wait=()):
        nc = self.nc
        for o in self.ops:
            for d in o.deps:
                d.needed = True
        sems = {e: self.es.enter_context(nc.semaphore(f"s_{e}")) for e in self.COMPUTE}
        sigcnt = {e: 0 for e in self.COMPUTE}
        queues = sorted({o.eng for o in self.ops if o.isdma})
        dsems = {q: [self.es.enter_context(nc.semaphore(f"d_{q}{i}")) for i in range(self.n_dma_sems)] for q in queues}
        dcnt = {q: [0] * self.n_dma_sems for q in queues}
        dnext = {q: 0 for q in queues}
        waited = {}
        all_dma = []

        def wait(engname, sem, key, val):
            if waited.get((engname, key), 0) >= val:
                return
            self.eng[engname].wait_ge(sem, val)
            waited[(engname, key)] = val

        for o in self.ops:
            e = self.eng[o.eng]
            for d in o.deps:
                if d.isdma:
                    wait(o.eng, d.dsem, ("d", d.eng, d.dslot), d.dval)
                else:
                    wait(o.eng, sems[d.eng], ("c", d.eng), d.sig)
            if o.isdma:
                q = o.eng
                slot = dnext[q]
                dnext[q] = (slot + 1) % self.n_dma_sems
                sem = dsems[q][slot]
                if dcnt[q][slot] > 0:
                    wait(o.eng, sem, ("d", q, slot), dcnt[q][slot])
                dcnt[q][slot] += 16
                o.dsem = sem
                o.dslot = slot
                o.dval = dcnt[q][slot]
                o.fn(e).then_inc(sem, 16)
                all_dma.append(o)
            else:
                ins = o.fn(e)
                if o.needed:
                    sigcnt[o.eng] += 1
                    o.sig = sigcnt[o.eng]
                    ins.then_inc(sems[o.eng], 1)
        for q in queues:
            for slot in range(self.n_dma_sems):
                if dcnt[q][slot] > 0:
                    wait("sp", dsems[q][slot], ("d", q, slot), dcnt[q][slot])


def _run(nc, in_maps):
    res = run_bass_kernel_spmd(nc, in_maps, core_ids=list(range(NCORES)))
    return res.results


MODC = 6 * D // NCORES


def build_mods():
    nc = bass.Bass("TRN2", target_bir_lowering=False)
    k = KB(nc)
    cinT = nc.dram_tensor("cinT", [D, 3], F32, kind="ExternalInput").ap()
    aw = nc.dram_tensor("aw", [DEPTH, D, MODC], F32, kind="ExternalInput").ap()
    ab = nc.dram_tensor("ab", [DEPTH, 1, MODC], F32, kind="ExternalInput").ap()
    out = nc.dram_tensor("mod", [DEPTH, 3, MODC], F32, kind="ExternalOutput").ap()
    cs = k.sb([128, 16, 3], F32, "cs")
    ones = k.sb([1, 4], F32, "ones")
    wt = [k.sb([128, 16, 512], F32, f"wt{i}") for i in range(2)]
    bt = k.sb([1, DEPTH, MODC], F32, "bt")
    ot = [k.sb([3, 512], F32, f"ot{i}") for i in range(2)]
    pst = [k.ps() for _ in range(2)]
    k.dma(cs[:], cinT.rearrange("(k p) r -> p k r", p=128), w=[cs])
    k.dma(bt[:], ab.rearrange("l o c -> o l c"), w=[bt])
    k.op("dve", lambda e: e.memset(ones[:], 1.0), w=[ones])
    k.op("act", lambda e: e.activation(out=cs[:], in_=cs[:], func=AF.Silu), r=[cs], w=[cs])
    it = 0
    for li in range(DEPTH):
        for cc in range(MODC // 512):
            w_ = wt[it % 2]
            p_ = pst[it % 2]
            o_ = ot[it % 2]
            it += 1
            k.dma(w_[:], aw[li, :, cc * 512:(cc + 1) * 512].rearrange("(k p) c -> p k c", p=128), w=[w_])
            for kk in range(16):
                k.op("pe", lambda e, w_=w_, p_=p_, kk=kk: e.matmul(p_[0:3, :], lhsT=cs[:, kk, :], rhs=w_[:, kk, :],
                                                                   start=(kk == 0), stop=False), r=[cs, w_], w=[p_])
            k.op("pe", lambda e, p_=p_, li=li, cc=cc: e.matmul(p_[0:3, :], lhsT=ones[0:1, 0:3],
                                                              rhs=bt[0:1, li, cc * 512:(cc + 1) * 512],
                                                              start=False, stop=True), r=[ones, bt], w=[p_])
            k.op("dve", lambda e, p_=p_, o_=o_: e.tensor_copy(out=o_[:], in_=p_[0:3, :]), r=[p_], w=[o_])
            k.dma(out[li, :, cc * 512:(cc + 1) * 512], o_[:], r=[o_], q="pool")
    k.emit()
    return nc


def run_mods(c, c_ctx, ada_w, ada_b):
    cinT = np.ascontiguousarray(np.concatenate([c, c_ctx[None]], 0).T)
    nc = build_mods()
    maps = []
    for i in range(NCORES):
        sl = slice(i * MODC, (i + 1) * MODC)
        maps.append({"cinT": cinT, "aw": np.ascontiguousarray(ada_w[:, :, sl]),
                     "ab": np.ascontiguousarray(ada_b[:, None, sl])})
    res = _run(nc, maps)
    return np.concatenate([r["mod"] for r in res], axis=2)


IN_SIZES = (768, 768, 1024, 24, 384, 384, 768, 768, 32, 2304, 4 * D)
IN_OFF = np.concatenate([[0], np.cumsum(IN_SIZES)]).astype(int)
(O_S5, O_Z, O_XBC, O_DT, O_Q, O_K, O_V, O_G, O_R, O_HY, O_GT) = [int(v) for v in IN_OFF[:11]]


def core_cols(j):
    cols = []
    seg = {}

    def add(name, lst):
        seg[name] = (len(cols), len(lst))
        cols.extend(lst)

    def pad():
        while len(cols) % 128:
            cols.append(0)
    add("s5u", list(range(O_S5 + 192 * j, O_S5 + 192 * (j + 1))))
    heads = [j, j + 4 if j < 2 else j]
    for hi, h in enumerate(heads):
        add(f"q{hi}", list(range(O_Q + 64 * h, O_Q + 64 * (h + 1))))
        add(f"k{hi}", list(range(O_K + 64 * h, O_K + 64 * (h + 1))))
        add(f"v{hi}", list(range(O_V + 128 * h, O_V + 128 * (h + 1))))
        add(f"g{hi}", list(range(O_G + 128 * h, O_G + 128 * (h + 1))))
    add("r", list(range(O_R, O_R + 32)))
    for i, nm in enumerate(("hv", "hx1", "hx2")):
        add(nm, list(range(O_HY + 768 * i + 192 * j, O_HY + 768 * i + 192 * (j + 1))))
    pad()
    seg["ssd_start"] = (len(cols), 0)
    add("z", list(range(O_Z + 192 * j, O_Z + 192 * (j + 1))))
    add("x", list(range(O_XBC + 192 * j, O_XBC + 192 * (j + 1))))
    g = j // 2
    add("Bm", list(range(O_XBC + 768 + 64 * g, O_XBC + 768 + 64 * (g + 1))))
    add("Cm", list(range(O_XBC + 768 + 128 + 64 * g, O_XBC + 768 + 128 + 64 * (g + 1))))
    add("dt", [O_DT + 3 * j + i for i in range(3)] + [O_DT + 12 + 3 * j + i for i in range(3)])
    pad()
    seg["mix_end"] = (len(cols), 0)
    add("gate", list(range(O_GT + D * j, O_GT + D * (j + 1))))
    return cols, seg


NMIX = core_cols(0)[1]["mix_end"][0]
NA = NMIX + D
SEG = core_cols(0)[1]
NSSD0 = SEG["ssd_start"][0]

TCH = 256
NTCH = T // TCH


def emit_inproj(k, nc, xT, xTc, modv, wA, pT, gates):
    ones = k.sb([128, 128], F32, "ones")
    k.op("dve", lambda e: e.memset(ones[:], 1.0), w=[ones])
    mv = k.sb([128, 16, 4], F32, "mv")
    k.dma(mv[:], modv.rearrange("(k p) r -> p k r", p=128), w=[mv])
    k.op("dve", lambda e: e.tensor_scalar(out=mv[:, :, 1], in0=mv[:, :, 1], scalar1=1.0, scalar2=None, op0=ALU.add),
         r=[mv], w=[mv])
    k.op("dve", lambda e: e.tensor_scalar(out=mv[:, :, 3], in0=mv[:, :, 3], scalar1=1.0, scalar2=None, op0=ALU.add),
         r=[mv], w=[mv])
    epsb = k.sb([128, 1], F32, "epsb")
    k.op("dve", lambda e: e.memset(epsb[:], EPS), w=[epsb])
    xt = [k.sb([128, 16, TCH], F32, f"xt{i}") for i in range(2)]
    sq = [k.sb([128, TCH], F32, f"sq{i}") for i in range(2)]
    rstd = k.sb([128, TCH], F32, "rstd")
    tmp = [k.sb([128, TCH], F32, f"tmp{i}") for i in range(2)]
    NP0 = 9 * TCH
    hT = k.sb([128, 16, NP0], BF16, "hT")
    wf = [k.sb([128, 16, 128], F32, f"wf{i}") for i in range(2)]
    wb = [k.sb([128, 16, 128], BF16, f"wb{i}") for i in range(2)]
    ost = [k.sb([128, 512], F32, f"ost{i}") for i in range(3)]
    ostb = [k.sb([128, 512], BF16, f"ostb{i}") for i in range(3)]
    ps_s = k.ps()
    ps_o = [k.ps() for _ in range(3)]
    it_o = 0
    ssd_ch = list(range(NSSD0 // 128, NMIX // 128))
    for part in (0, 1):
        colchunks = list(range(NA // 128))
        ch0, nch = (0, 9) if part == 0 else (9, 8)
        for ci in range(nch):
            ch = ch0 + ci
            x_ = xt[ch % 2]
            k.dma(x_[:], xT[:, ch * TCH:(ch + 1) * TCH].rearrange("(k p) t -> p k t", p=128), w=[x_])
            for kk in range(16):
                s_ = sq[kk % 2]
                k.op("act", lambda e, s_=s_, x_=x_, kk=kk: e.activation(out=s_[:], in_=x_[:, kk, :], func=AF.Square),
                     r=[x_], w=[s_])
                k.op("pe", lambda e, s_=s_, kk=kk: e.matmul(ps_s[:, 0:TCH], lhsT=ones[:], rhs=s_[:],
                                                           start=(kk == 0), stop=(kk == 15)), r=[ones, s_], w=[ps_s])
            k.op("act", lambda e: e.activation(out=rstd[:], in_=ps_s[:, 0:TCH], func=AF.Sqrt, bias=epsb[:], scale=1.0 / D),
                 r=[ps_s, epsb], w=[rstd])
            k.op("dve", lambda e: e.reciprocal(out=rstd[:], in_=rstd[:]), r=[rstd], w=[rstd])
            mo = 2 if ch == 0 else 0
            for kk in range(16):
                t_ = tmp[kk % 2]
                k.op("dve", lambda e, t_=t_, x_=x_, kk=kk: e.tensor_tensor(out=t_[:], in0=x_[:, kk, :], in1=rstd[:], op=ALU.mult),
                     r=[x_, rstd], w=[t_])
                k.op("dve", lambda e, t_=t_, kk=kk, ci=ci, mo=mo: e.tensor_scalar(
                    out=hT[:, kk, ci * TCH:(ci + 1) * TCH], in0=t_[:], scalar1=mv[:, kk, mo + 1:mo + 2],
                    scalar2=mv[:, kk, mo:mo + 1], op0=ALU.mult, op1=ALU.add), r=[t_, mv], w=[hT])
        ntok = nch * TCH
        tok0 = ch0 * TCH
        for cc in colchunks:
            wf_ = wf[cc % 2]
            wb_ = wb[cc % 2]
            k.dma(wf_[:], wA[:, cc * 128:(cc + 1) * 128].rearrange("(k p) c -> p k c", p=128), w=[wf_])
            k.op("pool", lambda e, wf_=wf_, wb_=wb_: e.tensor_copy(out=wb_[:], in_=wf_[:]), r=[wf_], w=[wb_])
            for t0 in range(0, ntok, 512):
                tn = min(512, ntok - t0)
                p_ = ps_o[it_o % 3]
                o_ = ost[it_o % 3]
                it_o += 1
                for kk in range(16):
                    k.op("pe", lambda e, p_=p_, wb_=wb_, kk=kk, t0=t0, tn=tn: e.matmul(
                        p_[:, 0:tn], lhsT=wb_[:, kk, :], rhs=hT[:, kk, t0:t0 + tn], start=(kk == 0), stop=(kk == 15)),
                        r=[wb_, hT], w=[p_])
                if cc * 128 < NMIX:
                    k.op("act", lambda e, p_=p_, o_=o_, tn=tn: e.activation(out=o_[:, 0:tn], in_=p_[:, 0:tn], func=AF.Copy),
                         r=[p_], w=[o_])
                    k.dma(pT[cc * 128:(cc + 1) * 128, tok0 + t0:tok0 + t0 + tn], o_[:, 0:tn], r=[o_], w=[pT.tensor], q="pool")
                else:
                    ob_ = ostb[it_o % 3]
                    k.op("act", lambda e, p_=p_, ob_=ob_, tn=tn: e.activation(out=ob_[:, 0:tn], in_=p_[:, 0:tn], func=AF.Sigmoid),
                         r=[p_], w=[ob_])
                    g0 = cc * 128 - NMIX
                    k.dma(gates[g0:g0 + 128, tok0 + t0:tok0 + t0 + tn], ob_[:, 0:tn], r=[ob_], q="pool")


def build_am(parts=("A",)):
    nc = bass.Bass("TRN2", target_bir_lowering=False)
    k = KB(nc)
    xT = nc.dram_tensor("xT", [D, T], F32, kind="ExternalInput").ap()
    modv = nc.dram_tensor("modv", [D, 4], F32, kind="ExternalInput").ap()
    wA = nc.dram_tensor("wA", [D, NA], F32, kind="ExternalInput").ap()
    pT = nc.dram_tensor("pT", [NMIX, T], F32, kind="ExternalOutput" if "dumpP" in parts else "Internal").ap()
    gates = nc.dram_tensor("gates", [D, T], BF16, kind="ExternalOutput").ap()
    if "A" in parts:
        emit_inproj(k, nc, xT, xTc, modv, wA, pT, gates)
    k.emit()
    return nc


TWO_PI = 2.0 * math.pi


def chunks(n, c=512):
    return [(t0, min(c, n - t0)) for t0 in range(0, n, c)]


def emit_sin_turns(k, out, x, shape, name):
    xi = k.sb(shape, I32, name + "_i")
    xf = k.sb(shape, F32, name + "_f")
    k.op("dve", lambda e: e.tensor_copy(out=xi[:], in_=x[:]), r=[x], w=[xi])
    k.op("dve", lambda e: e.tensor_copy(out=xf[:], in_=xi[:]), r=[xi], w=[xf])
    k.op("dve", lambda e: e.tensor_tensor(out=xf[:], in0=x[:], in1=xf[:], op=ALU.subtract), r=[x, xf], w=[xf])
    k.op("dve", lambda e: e.tensor_scalar(out=xf[:], in0=xf[:], scalar1=0.4999999, scalar2=-0.4999999, op0=ALU.min, op1=ALU.max),
         r=[xf], w=[xf])
    k.op("act", lambda e: e.activation(out=out[:], in_=xf[:], func=AF.Sin, scale=TWO_PI), r=[xf], w=[out])


def emit_s5_abar(k, lamre, lamim, ls, shape, name):
    step = k.sb(shape, F32, name + "_step")
    rho = k.sb(shape, F32, name + "_rho")
    th = k.sb(shape, F32, name + "_th")
    th2 = k.sb(shape, F32, name + "_th2")
    sn = k.sb(shape, F32, name + "_sn")
    cs = k.sb(shape, F32, name + "_cs")
    k.op("act", lambda e: e.activation(out=step[:], in_=ls[:], func=AF.Exp), r=[ls], w=[step])
    k.op("dve", lambda e: e.tensor_tensor(out=rho[:], in0=lamre[:], in1=step[:], op=ALU.mult), r=[lamre, step], w=[rho])
    k.op("act", lambda e: e.activation(out=rho[:], in_=rho[:], func=AF.Exp), r=[rho], w=[rho])
    k.op("dve", lambda e: e.tensor_tensor(out=th[:], in0=lamim[:], in1=step[:], op=ALU.mult), r=[lamim, step], w=[th])
    k.op("dve", lambda e: e.tensor_scalar(out=th[:], in0=th[:], scalar1=1.0 / TWO_PI, scalar2=None, op0=ALU.mult), r=[th], w=[th])
    k.op("dve", lambda e: e.tensor_scalar(out=th2[:], in0=th[:], scalar1=0.25, scalar2=None, op0=ALU.add), r=[th], w=[th2])
    emit_sin_turns(k, sn, th, shape, name + "_s")
    emit_sin_turns(k, cs, th2, shape, name + "_c")
    k.op("dve", lambda e: e.tensor_tensor(out=sn[:], in0=sn[:], in1=rho[:], op=ALU.mult), r=[sn, rho], w=[sn])
    k.op("dve", lambda e: e.tensor_tensor(out=cs[:], in0=cs[:], in1=rho[:], op=ALU.mult), r=[cs, rho], w=[cs])
    return cs, sn, step


NLEV = 13


def emit_s5(k, nc, pT, ya, cst, prm):
    ident, swp = cst["ident"], cst["swap"]
    AR = k.sb([128, NLEV, 24], F32, "AR")
    AI = k.sb([128, NLEV, 24], F32, "AI")
    bbT = k.sb([16, 24, 128], F32, "bbT")
    cT = k.sb([128, 24, 16], F32, "cT")
    dsk = k.sb([16, 12], F32, "dsk")
    mk_tmp = k.mark()
    l128 = k.sb([128, 2, 24], F32, "l128")
    ls128 = k.sb([128, 24], F32, "ls128")
    sg128 = k.sb([128, 1], F32, "sg128")
    k.dma(l128[:], prm["s5_lam128"], w=[l128])
    k.dma(ls128[:], prm["s5_ls128"], w=[ls128])
    k.dma(sg128[:], prm["sign128"], w=[sg128])
    lre = k.sb([128, 24], F32, "lre")
    lim = k.sb([128, 24], F32, "lim")
    k.op("dve", lambda e: e.tensor_copy(out=lre[:], in_=l128[:, 0, :]), r=[l128], w=[lre])
    k.op("dve", lambda e: e.tensor_copy(out=lim[:], in_=l128[:, 1, :]), r=[l128], w=[lim])
    ar0, ai0, _ = emit_s5_abar(k, lre, lim, ls128, [128, 24], "c128")
    k.op("dve", lambda e: e.tensor_copy(out=AR[:, 0, :], in_=ar0[:]), r=[ar0], w=[AR])
    k.op("dve", lambda e: e.tensor_scalar(out=AI[:, 0, :], in0=ai0[:], scalar1=sg128[:, 0:1], scalar2=None, op0=ALU.mult),
         r=[ai0, sg128], w=[AI])
    t1 = k.sb([128, 24], F32, "sqt1")
    t2 = k.sb([128, 24], F32, "sqt2")
    for lv in range(1, NLEV):
        k.op("dve", lambda e, lv=lv: e.tensor_tensor(out=t1[:], in0=AR[:, lv - 1, :], in1=AR[:, lv - 1, :], op=ALU.mult), r=[AR], w=[t1])
        k.op("dve", lambda e, lv=lv: e.tensor_tensor(out=t2[:], in0=AI[:, lv - 1, :], in1=AI[:, lv - 1, :], op=ALU.mult), r=[AI], w=[t2])
        k.op("dve", lambda e, lv=lv: e.tensor_tensor(out=AR[:, lv, :], in0=t1[:], in1=t2[:], op=ALU.subtract), r=[t1, t2], w=[AR])
        k.op("dve", lambda e, lv=lv: e.tensor_tensor(out=t1[:], in0=AR[:, lv - 1, :], in1=AI[:, lv - 1, :], op=ALU.mult), r=[AR, AI], w=[t1])
        k.op("dve", lambda e, lv=lv: e.tensor_scalar(out=AI[:, lv, :], in0=t1[:], scalar1=2.0, scalar2=None, op0=ALU.mult), r=[t1], w=[AI])
    l16 = k.sb([16, 2, 24 * 64], F32, "l16")
    ls16 = k.sb([16, 24 * 64], F32, "ls16")
    b16 = k.sb([16, 2, 24 * 64], F32, "b16")
    k.dma(l16[:], prm["s5_lam16"], w=[l16])
    k.dma(ls16[:], prm["s5_ls16"], w=[ls16])
    k.dma(b16[:], prm["s5_b16"], w=[b16])
    SH = [16, 24 * 64]
    lre16 = k.sb(SH, F32, "lre16")
    lim16 = k.sb(SH, F32, "lim16")
    k.op("dve", lambda e: e.tensor_copy(out=lre16[:], in_=l16[:, 0, :]), r=[l16], w=[lre16])
    k.op("dve", lambda e: e.tensor_copy(out=lim16[:], in_=l16[:, 1, :]), r=[l16], w=[lim16])
    ar, ai, _ = emit_s5_abar(k, lre16, lim16, ls16, SH, "c16")
    den = k.sb(SH, F32, "den")
    q1 = k.sb(SH, F32, "q1")
    zr = k.sb(SH, F32, "zr")
    zi = k.sb(SH, F32, "zi")
    k.op("dve", lambda e: e.tensor_tensor(out=den[:], in0=lre16[:], in1=lre16[:], op=ALU.mult), r=[lre16], w=[den])
    k.op("dve", lambda e: e.tensor_tensor(out=q1[:], in0=lim16[:], in1=lim16[:], op=ALU.mult), r=[lim16], w=[q1])
    k.op("dve", lambda e: e.tensor_tensor(out=den[:], in0=den[:], in1=q1[:], op=ALU.add), r=[den, q1], w=[den])
    k.op("dve", lambda e: e.reciprocal(out=den[:], in_=den[:]), r=[den], w=[den])
    k.op("dve", lambda e: e.tensor_scalar(out=ar[:], in0=ar[:], scalar1=-1.0, scalar2=None, op0=ALU.add), r=[ar], w=[ar])
    k.op("dve", lambda e: e.tensor_tensor(out=zr[:], in0=ar[:], in1=lre16[:], op=ALU.mult), r=[ar, lre16], w=[zr])
    k.op("dve", lambda e: e.tensor_tensor(out=q1[:], in0=ai[:], in1=lim16[:], op=ALU.mult), r=[ai, lim16], w=[q1])
    k.op("dve", lambda e: e.tensor_tensor(out=zr[:], in0=zr[:], in1=q1[:], op=ALU.add), r=[zr, q1], w=[zr])
    k.op("dve", lambda e: e.tensor_tensor(out=zr[:], in0=zr[:], in1=den[:], op=ALU.mult), r=[zr, den], w=[zr])
    k.op("dve", lambda e: e.tensor_tensor(out=zi[:], in0=ai[:], in1=lre16[:], op=ALU.mult), r=[ai, lre16], w=[zi])
    k.op("dve", lambda e: e.tensor_tensor(out=q1[:], in0=ar[:], in1=lim16[:], op=ALU.mult), r=[ar, lim16], w=[q1])
    k.op("dve", lambda e: e.tensor_tensor(out=zi[:], in0=zi[:], in1=q1[:], op=ALU.subtract), r=[zi, q1], w=[zi])
    k.op("dve", lambda e: e.tensor_tensor(out=zi[:], in0=zi[:], in1=den[:], op=ALU.mult), r=[zi, den], w=[zi])
    bre = b16[:, 0, :].rearrange("h (g p) -> h g p", p=64)
    bim = b16[:, 1, :].rearrange("h (g p) -> h g p", p=64)
    zr3 = zr[:].rearrange("h (g p) -> h g p", p=64)
    zi3 = zi[:].rearrange("h (g p) -> h g p", p=64)
    q3 = k.sb([16, 24, 64], F32, "q3")
    k.op("dve", lambda e: e.tensor_tensor(out=bbT[:, :, 0:64], in0=zr3, in1=bre, op=ALU.mult), r=[zr, b16], w=[bbT])
    k.op("dve", lambda e: e.tensor_tensor(out=q3[:], in0=zi3, in1=bim, op=ALU.mult), r=[zi, b16], w=[q3])
    k.op("dve", lambda e: e.tensor_tensor(out=bbT[:, :, 0:64], in0=bbT[:, :, 0:64], in1=q3[:], op=ALU.subtract), r=[bbT, q3], w=[bbT])
    k.op("dve", lambda e: e.tensor_tensor(out=bbT[:, :, 64:128], in0=zr3, in1=bim, op=ALU.mult), r=[zr, b16], w=[bbT])
    k.op("dve", lambda e: e.tensor_tensor(out=q3[:], in0=zi3, in1=bre, op=ALU.mult), r=[zi, b16], w=[q3])
    k.op("dve", lambda e: e.tensor_tensor(out=bbT[:, :, 64:128], in0=bbT[:, :, 64:128], in1=q3[:], op=ALU.add), r=[bbT, q3], w=[bbT])
    k.dma(cT[:], prm["s5_c128"], w=[cT])
    k.op("dve", lambda e: e.tensor_scalar(out=cT[64:128], in0=cT[64:128], scalar1=-1.0, scalar2=None, op0=ALU.mult), r=[cT], w=[cT])
    k.dma(dsk[:], prm["s5_d"], w=[dsk])
    k.release(mk_tmp)

    u3 = [k.sb([16, T + NCTX], F32, f"u3_{i}") for i in range(2)]
    H = [[k.sb([128, T], F32, f"H{d}{i}") for i in range(2)] for d in range(2)]
    MT = [k.sb([128, 128], F32, f"MT{i}") for i in range(4)]
    yt = [k.sb([16, T], F32, f"yt{i}") for i in range(2)]
    psr = [k.ps() for _ in range(4)]
    psy = [k.ps() for _ in range(2)]
    nps = 0
    nmt = 0
    r0 = SEG["s5u"][0]
    for gl in range(12):
        u_ = u3[gl % 2]
        y_ = yt[gl % 2]
        k.dma(u_[:, 0:T], pT[r0 + 16 * gl:r0 + 16 * (gl + 1), :], r=[pT.tensor], w=[u_])
        k.dma(u_[:, T:T + NCTX], pT[r0 + 16 * gl:r0 + 16 * (gl + 1), 0:NCTX], r=[pT.tensor], w=[u_])
        cur = [0, 0]
        for d in range(2):
            dg = d * 12 + gl
            c0 = 0 if d == 0 else NCTX
            for (t0, n) in chunks(T):
                p_ = psr[nps % 4]
                nps += 1
                k.op("pe", lambda e, p_=p_, dg=dg, u_=u_, c0=c0, t0=t0, n=n: e.matmul(
                    p_[:, 0:n], lhsT=bbT[:, dg, :], rhs=u_[:, c0 + t0:c0 + t0 + n], start=True, stop=True), r=[bbT, u_], w=[p_])
                k.op("act", lambda e, p_=p_, d=d, t0=t0, n=n: e.activation(out=H[d][0][:, t0:t0 + n], in_=p_[:, 0:n], func=AF.Copy),
                     r=[p_], w=[H[d][0]])
        for lv in range(NLEV):
            sh = 1 << lv
            for d in range(2):
                dg = d * 12 + gl
                src = H[d][cur[d]]
                dst = H[d][1 - cur[d]]
                cur[d] = 1 - cur[d]
                m_ = MT[nmt % 4]
                nmt += 1
                k.op("dve", lambda e, m_=m_, lv=lv, dg=dg: e.tensor_scalar(out=m_[:], in0=ident[:], scalar1=AR[:, lv, dg:dg + 1],
                                                                         scalar2=None, op0=ALU.mult), r=[ident, AR], w=[m_])
                k.op("dve", lambda e, m_=m_, lv=lv, dg=dg: e.scalar_tensor_tensor(out=m_[:], in0=swp[:], scalar=AI[:, lv, dg:dg + 1],
                                                                                in1=m_[:], op0=ALU.mult, op1=ALU.add),
                     r=[swp, AI, m_], w=[m_])
                if d == 0:
                    k.op("pool", lambda e, src=src, dst=dst, sh=sh: e.tensor_copy(out=dst[:, 0:sh], in_=src[:, 0:sh]), r=[src], w=[dst])
                    lo = sh
                else:
                    k.op("pool", lambda e, src=src, dst=dst, sh=sh: e.tensor_copy(out=dst[:, T - sh:T], in_=src[:, T - sh:T]), r=[src], w=[dst])
                    lo = 0
                for (t0, n) in chunks(T - sh):
                    ta = lo + t0
                    tb = ta - sh if d == 0 else ta + sh
                    p_ = psr[nps % 4]
                    nps += 1
                    k.op("pe", lambda e, p_=p_, m_=m_, src=src, tb=tb, n=n: e.matmul(p_[:, 0:n], lhsT=m_[:], rhs=src[:, tb:tb + n],
                                                                                  start=True, stop=True), r=[m_, src], w=[p_])
                    k.op("dve", lambda e, p_=p_, src=src, dst=dst, ta=ta, n=n: e.tensor_tensor(
                        out=dst[:, ta:ta + n], in0=src[:, ta:ta + n], in1=p_[:, 0:n], op=ALU.add), r=[src, p_], w=[dst])
        Hf = H[0][cur[0]]
        Hb = H[1][cur[1]]
        for (t0, n) in [(0, NCTX)] + [(NCTX + a, b_) for (a, b_) in chunks(S)]:
            tb = (S + t0) if t0 < NCTX else (t0 - NCTX)
            p_ = psy[(t0 // 256) % 2]
            k.op("pe", lambda e, p_=p_, gl=gl, t0=t0, n=n: e.matmul(p_[0:16, 0:n], lhsT=cT[:, gl, :], rhs=Hf[:, t0:t0 + n],
                                                                   start=True, stop=False), r=[cT, Hf], w=[p_])
            k.op("pe", lambda e, p_=p_, gl=gl, tb=tb, n=n: e.matmul(p_[0:16, 0:n], lhsT=cT[:, 12 + gl, :], rhs=Hb[:, tb:tb + n],
                                                                   start=False, stop=True), r=[cT, Hb], w=[p_])
            k.op("dve", lambda e, p_=p_, u_=u_, y_=y_, gl=gl, t0=t0, n=n: e.scalar_tensor_tensor(
                out=y_[:, t0:t0 + n], in0=u_[:, t0:t0 + n], scalar=dsk[:, gl:gl + 1], in1=p_[0:16, 0:n], op0=ALU.mult, op1=ALU.add),
                r=[u_, dsk, p_], w=[y_])
        k.dma(ya[16 * gl:16 * (gl + 1), :], y_[:], r=[y_], q="pool")


def s5_host_params(inp, li, j):
    gs = slice(12 * j, 12 * (j + 1))
    lre = inp["s5_lambda_re"][li][:, gs]
    lim = inp["s5_lambda_im"][li][:, gs]
    ls = inp["s5_log_step"][li][:, gs]
    col = lambda a: np.ascontiguousarray(a.reshape(24, 64).T)
    lam128 = np.stack([np.concatenate([col(lre), col(lre)], 0), np.concatenate([col(lim), col(lim)], 0)], 1)
    ls128 = np.broadcast_to(ls.reshape(1, 24), (128, 24))
    lam16 = np.broadcast_to(np.stack([lre.reshape(24 * 64), lim.reshape(24 * 64)], 0)[None], (16, 2, 24 * 64))
    ls16 = np.broadcast_to(np.repeat(ls.reshape(24), 64)[None], (16, 24 * 64))
    bre = inp["s5_b_re"][li][:, gs]
    bim = inp["s5_b_im"][li][:, gs]
    b16 = np.stack([bre.reshape(24 * 64, 16).T, bim.reshape(24 * 64, 16).T], 1)
    cre = inp["s5_c_re"][li][:, gs]
    cim = inp["s5_c_im"][li][:, gs]
    c128 = np.concatenate([cre.reshape(24, 16, 64).transpose(2, 0, 1), cim.reshape(24, 16, 64).transpose(2, 0, 1)], 0)
    dd = inp["s5_d"][li][192 * j:192 * (j + 1)].reshape(12, 16).T
    f = lambda a: np.ascontiguousarray(a, dtype=np.float32)
    return {"s5_lam128": f(lam128), "s5_ls128": f(ls128), "s5_lam16": f(lam16), "s5_ls16": f(ls16), "s5_b16": f(b16),
            "s5_c128": f(c128), "s5_d": f(dd)}


def host_consts():
    ident = np.eye(128, dtype=np.float32)
    swap = np.zeros((128, 128), np.float32)
    for i in range(64):
        swap[i, i + 64] = 1.0
        swap[i + 64, i] = 1.0
    sign = np.ones((128, 1), np.float32)
    sign[64:] = -1.0
    return {"c_ident": ident, "c_swap": swap, "sign128": sign}


S5_SHAPES = {"s5_lam128": [128, 2, 24], "s5_ls128": [128, 24], "s5_lam16": [16, 2, 1536], "s5_ls16": [16, 1536],
             "s5_b16": [16, 2, 1536], "s5_c128": [128, 24, 16], "s5_d": [16, 12], "sign128": [128, 1]}


def build_am(parts=("A",)):
    nc = bass.Bass("TRN2", target_bir_lowering=False)
    k = KB(nc)
    xT = nc.dram_tensor("xT", [D, T], F32, kind="ExternalInput").ap()
    xTc = None
    modv = nc.dram_tensor("modv", [D, 4], F32, kind="ExternalInput").ap()
    wA = nc.dram_tensor("wA", [D, NA], F32, kind="ExternalInput").ap()
    pT = nc.dram_tensor("pT", [NMIX, T], F32, kind="ExternalOutput" if "dumpP" in parts else "Internal").ap()
    gates = nc.dram_tensor("gates", [D, T], BF16, kind="ExternalOutput").ap()
    prm = {}
    cst = {}
    ci = nc.dram_tensor("c_ident", [128, 128], F32, kind="ExternalInput").ap()
    cs_ = nc.dram_tensor("c_swap", [128, 128], F32, kind="ExternalInput").ap()
    cst["ident"] = k.sb([128, 128], F32, "ident")
    cst["swap"] = k.sb([128, 128], F32, "swap")
    k.dma(cst["ident"][:], ci, w=[cst["ident"]])
    k.dma(cst["swap"][:], cs_, w=[cst["swap"]])
    mk = k.mark()
    if "A" in parts:
        emit_inproj(k, nc, xT, xTc, modv, wA, pT, gates)
        k.release(mk)
    if "S5" in parts:
        for nm, shp in S5_SHAPES.items():
            prm[nm] = nc.dram_tensor(nm, shp, F32, kind="ExternalInput").ap()
        ya = nc.dram_tensor("ya", [192, T], F32, kind="ExternalOutput").ap()
        emit_s5(k, nc, pT, ya, cst, prm)
        k.release(mk)
    k.emit()
    return nc


SEGS_F = [(0, 256, False), (256, 1792, False), (1792, 3328, False), (3328, 4352, False)]
SEGS_B = [(0, 256, True), (3328, 4352, True), (1792, 3328, True), (256, 1792, True)]


def emit_outer_scan(k, cst, bufs, Xs, PX, Arep, Crep, dec_of_pt, d, yacc, first):
    selx, hm = cst["selx"], cst["hm"]
    U, Hs, carry, psx, psy = bufs
    npt = PX // 2
    it = 0
    for si, (a, b_, rev) in enumerate(SEGS_B if d == 1 else SEGS_F):
        L = b_ - a
        cks = chunks(L)
        for pt in range(npt):
            u_ = U[it % 2]
            h_ = Hs[it % 2]
            v_ = h_
            it += 1
            p0 = 0 if pt < 32 else 64
            for ci, (t0, n) in enumerate(cks):
                p_ = psx[(it + ci) % 2]
                k.op("pe", lambda e, p_=p_, pt=pt, p0=p0, t0=t0, n=n: e.matmul(
                    p_[:, 0:n], lhsT=selx[p0:p0 + 64, pt % 32, :], rhs=Xs[p0:p0 + 64, a + t0:a + t0 + n], start=True, stop=True),
                    r=[selx, Xs], w=[p_])
                k.op("dve", lambda e, p_=p_, u_=u_, t0=t0, n=n: e.tensor_tensor(
                    out=u_[:, t0:t0 + n], in0=p_[:, 0:n], in1=Arep[:, a + t0:a + t0 + n], op=ALU.mult), r=[p_, Arep], w=[u_])
            dec = dec_of_pt(pt)
            init = 0.0 if si == 0 else carry[:, pt:pt + 1]
            rd = [dec, u_] + ([] if si == 0 else [carry])
            if rev:
                k.op("dve", lambda e, h_=h_, u_=u_, dec=dec, init=init, L=L: e.tensor_tensor_scan(
                    out=h_[:, L - 1::-1] if False else h_[:, 0:L][:, ::-1], data0=dec[:, a:b_][:, ::-1], data1=u_[:, 0:L][:, ::-1],
                    initial=init, op0=ALU.mult, op1=ALU.add), r=rd, w=[h_])
                last = 0
            else:
                k.op("dve", lambda e, h_=h_, u_=u_, dec=dec, init=init, L=L: e.tensor_tensor_scan(
                    out=h_[:, 0:L], data0=dec[:, a:b_], data1=u_[:, 0:L], initial=init, op0=ALU.mult, op1=ALU.add), r=rd, w=[h_])
                last = L - 1
            if si < 3:
                k.op("act", lambda e, h_=h_, pt=pt, last=last: e.activation(out=carry[:, pt:pt + 1], in_=h_[:, last:last + 1], func=AF.Copy),
                     r=[h_], w=[carry])
            k.op("pool", lambda e, h_=h_, v_=v_, L=L: e.tensor_tensor(out=v_[:, 0:L], in0=h_[:, 0:L], in1=Crep[:, a:b_], op=ALU.mult),
                 r=[h_, Crep, carry], w=[v_])
            for ci, (t0, n) in enumerate(cks):
                k.op("pe", lambda e, ci=ci, pt=pt, v_=v_, t0=t0, n=n: e.matmul(
                    psy[ci][0:PX, 0:n], lhsT=hm[:, 128 - 2 * pt:128 - 2 * pt + PX], rhs=v_[:, t0:t0 + n], start=(pt == 0), stop=(pt == npt - 1)),
                    r=[hm, v_], w=[psy[ci]])
        for ci, (t0, n) in enumerate(cks):
            if first:
                k.op("act", lambda e, ci=ci, t0=t0, n=n: e.activation(out=yacc[0:PX, a + t0:a + t0 + n], in_=psy[ci][0:PX, 0:n], func=AF.Copy),
                     r=[psy[ci]], w=[yacc])
            else:
                k.op("dve", lambda e, ci=ci, t0=t0, n=n: e.tensor_tensor(out=yacc[0:PX, a + t0:a + t0 + n], in0=yacc[0:PX, a + t0:a + t0 + n],
                                                                        in1=psy[ci][0:PX, 0:n], op=ALU.add), r=[psy[ci], yacc], w=[yacc])


def emit_conv3(k, dst, src, w3, bias, P, silu, wt=None):
    for (a, b_) in ((0, NCTX), (NCTX, T)):
        k.op("dve", lambda e, a=a, b_=b_: e.tensor_scalar(out=dst[0:P, a:b_], in0=src[0:P, a:b_], scalar1=w3[0:P, 1:2], scalar2=bias[0:P, 0:1],
                                                         op0=ALU.mult, op1=ALU.add), r=[src, wt], w=[dst])
        k.op("dve", lambda e, a=a, b_=b_: e.scalar_tensor_tensor(out=dst[0:P, a + 1:b_], in0=src[0:P, a:b_ - 1], scalar=w3[0:P, 0:1],
                                                                in1=dst[0:P, a + 1:b_], op0=ALU.mult, op1=ALU.add), r=[src, wt, dst], w=[dst])
        k.op("dve", lambda e, a=a, b_=b_: e.scalar_tensor_tensor(out=dst[0:P, a:b_ - 1], in0=src[0:P, a + 1:b_], scalar=w3[0:P, 2:3],
                                                                in1=dst[0:P, a:b_ - 1], op0=ALU.mult, op1=ALU.add), r=[src, wt, dst], w=[dst])
    if silu:
        k.op("act", lambda e: e.activation(out=dst[0:P, :], in_=dst[0:P, :], func=AF.Silu), r=[dst], w=[dst])


def scan_bufs(k):
    U = [k.sb([128, 1536], F32, f"U{i}") for i in range(2)]
    Hs = [k.sb([128, 1536], F32, f"Hs{i}") for i in range(2)]
    carry = k.sb([128, 64], F32, "carry")
    psx = [k.ps() for _ in range(2)]
    psy = [k.ps() for _ in range(3)]
    return (U, Hs, carry, psx, psy)


def emit_ssd(k, nc, pT, yb, cst, prm):
    sp = k.sb([128, 2, 8], F32, "ssdp_x")
    spB = k.sb([128, 8], F32, "ssdp_B")
    spC = k.sb([128, 8], F32, "ssdp_C")
    sdt = k.sb([6, 4], F32, "ssdp_dt")
    one6 = k.sb([6, 1], F32, "one6")
    selr = k.sb([6, 2, 2, 128], F32, "selr")
    selh = k.sb([38, 6, 128], F32, "selh")
    k.dma(sp[:], prm["ssd_px"], w=[sp])
    k.dma(spB[:], prm["ssd_pB"], w=[spB])
    k.dma(spC[:], prm["ssd_pC"], w=[spC])
    k.dma(sdt[:], prm["ssd_pdt"], w=[sdt])
    k.dma(selr[:], prm["selr"], w=[selr])
    k.dma(selh[32:38], prm["selh"], w=[selh])
    k.op("dve", lambda e: e.memset(one6[:], 1.0), w=[one6])
    rz, rx, rB, rC, rdt = SEG["z"][0], SEG["x"][0], SEG["Bm"][0], SEG["Cm"][0], SEG["dt"][0]
    raw = k.sb([128, T], F32, "raw")
    xa = k.sb([128, T], F32, "xa")
    Brep = k.sb([128, T], F32, "Brep")
    Crep = k.sb([128, T], F32, "Crep")
    ya = k.sb([128, T], F32, "ssd_ya")
    decA = [k.sb([128, T], F32, f"decA{i}") for i in range(2)]
    dd = k.sb([38, T], F32, "dtdec")
    for c_ in range((NMIX - NSSD0) // 128):
        r0 = NSSD0 + 128 * c_
        k.dma(raw[:], pT[r0:r0 + 128, :], r=[pT.tensor], w=[raw])
        k.op("dve", lambda e: e.tensor_copy(out=xa[:, 0:NCTX], in_=raw[:, 0:NCTX]), r=[raw], w=[xa])
        k.op("dve", lambda e: e.tensor_copy(out=xa[:, NCTX:T].rearrange("p (w r) -> p w r", w=64),
                                            in_=raw[:, NCTX:T].rearrange("p (r w) -> p w r", w=64)), r=[raw], w=[xa])
        k.dma(pT[r0:r0 + 128, :], xa[:], r=[xa], w=[pT.tensor], q="pool")
    k.dma(raw[0:64, :], pT[rB:rB + 64, :], r=[pT.tensor], w=[raw])
    k.dma(raw[64:128, :], pT[rB:rB + 64, :], r=[pT.tensor], w=[raw])
    emit_conv3(k, Brep, raw, spB, spB[:, 3:4], 128, True, wt=spB)
    k.dma(raw[0:64, :], pT[rC:rC + 64, :], r=[pT.tensor], w=[raw])
    k.dma(raw[64:128, :], pT[rC:rC + 64, :], r=[pT.tensor], w=[raw])
    emit_conv3(k, Crep, raw, spC, spC[:, 3:4], 128, True, wt=spC)
    k.dma(dd[0:6, :], pT[rdt:rdt + 6, :], r=[pT.tensor], w=[dd])
    k.op("act", lambda e: e.activation(out=dd[0:6, :], in_=dd[0:6, :], func=AF.Exp, bias=sdt[:, 0:1], scale=1.0), r=[dd, sdt], w=[dd])
    k.op("act", lambda e: e.activation(out=dd[0:6, :], in_=dd[0:6, :], func=AF.Ln, bias=one6[:], scale=1.0), r=[dd, one6], w=[dd])
    k.op("act", lambda e: e.activation(out=sdt[:, 2:3], in_=sdt[:, 1:2], func=AF.Exp), r=[sdt], w=[sdt])
    k.op("dve", lambda e: e.tensor_scalar(out=sdt[:, 2:3], in0=sdt[:, 2:3], scalar1=-1.0, scalar2=None, op0=ALU.mult), r=[sdt], w=[sdt])
    k.op("act", lambda e: e.activation(out=raw[0:6, :], in_=dd[0:6, :], func=AF.Exp, scale=sdt[:, 2:3]), r=[dd, sdt], w=[raw])
    k.dma(dd[32:38, :], raw[0:6, :], r=[raw], w=[dd])
    bufs = scan_bufs(k)
    psx = bufs[3]
    Xs = raw
    for ti, PX in ((0, 128), (1, 64)):
        k.dma(raw[0:PX, :], pT[rx + 128 * ti:rx + 128 * ti + PX, :], r=[pT.tensor], w=[raw])
        emit_conv3(k, xa, raw, sp[:, ti, :], sp[:, ti, 3:4], PX, True, wt=sp)
        for d in range(2):
            for ci, (t0, n) in enumerate(chunks(T)):
                p_ = psx[ci % 2]
                k.op("pe", lambda e, p_=p_, d=d, ti=ti, t0=t0, n=n: e.matmul(p_[:, 0:n], lhsT=selr[:, d, ti, :], rhs=dd[0:6, t0:t0 + n],
                                                                           start=True, stop=True), r=[selr, dd], w=[p_])
                k.op("dve", lambda e, p_=p_, PX=PX, t0=t0, n=n: e.tensor_tensor(out=Xs[0:PX, t0:t0 + n], in0=xa[0:PX, t0:t0 + n],
                                                                              in1=p_[0:PX, 0:n], op=ALU.mult), r=[p_, xa], w=[Xs])
            for hh in range(2 if ti == 0 else 1):
                rr = 3 * d + 2 * ti + hh
                for ci, (t0, n) in enumerate(chunks(T)):
                    p_ = psx[ci % 2]
                    k.op("pe", lambda e, p_=p_, rr=rr, t0=t0, n=n: e.matmul(p_[:, 0:n], lhsT=selh[32:38, rr, :], rhs=dd[32:38, t0:t0 + n],
                                                                          start=True, stop=True), r=[selh, dd], w=[p_])
                    k.op("act", lambda e, p_=p_, hh=hh, t0=t0, n=n: e.activation(out=decA[hh][:, t0:t0 + n], in_=p_[:, 0:n], func=AF.Copy),
                         r=[p_], w=[decA[hh]])
            emit_outer_scan(k, cst, bufs, Xs, PX, Brep, Crep, (lambda pt: decA[0] if pt < 32 else decA[1]), d, ya, first=(d == 0))
        P = PX
        r_ = rz + 128 * ti
        k.op("dve", lambda e, P=P, ti=ti: e.scalar_tensor_tensor(out=ya[0:P, :], in0=xa[0:P, :], scalar=sp[0:P, ti, 4:5],
                                                                in1=ya[0:P, :], op0=ALU.mult, op1=ALU.add), r=[xa, sp, ya], w=[ya])
        k.dma(raw[0:P, :], pT[r_:r_ + P, :], r=[pT.tensor], w=[raw])
        k.op("act", lambda e, P=P: e.activation(out=raw[0:P, :], in_=raw[0:P, :], func=AF.Silu), r=[raw], w=[raw])
        k.op("dve", lambda e, P=P: e.tensor_tensor(out=ya[0:P, :], in0=ya[0:P, :], in1=raw[0:P, :], op=ALU.mult), r=[ya, raw], w=[ya])
        k.dma(yb[128 * ti:128 * ti + P, :], ya[0:P, :], r=[ya], q="pool")


def emit_gla(k, nc, pT, yc, cst, prm):
    gp = k.sb([128, 2, 2, 2], F32, "glap")
    gw = k.sb([16, 2, 2, 128], F32, "glaw")
    nw = k.sb([128, 1], F32, "glanw")
    one = k.sb([128, 1], F32, "gone")
    epsb = k.sb([128, 1], F32, "geps")
    ones = k.sb([128, 128], F32, "gones")
    k.dma(gp[:], prm["gla_p"], w=[gp])
    k.dma(gw[:], prm["gla_w"], w=[gw])
    k.dma(nw[:], prm["gla_nw"], w=[nw])
    k.op("dve", lambda e: e.memset(one[:], 1.0), w=[one])
    k.op("dve", lambda e: e.memset(epsb[:], EPS), w=[epsb])
    k.op("dve", lambda e: e.memset(ones[:], 1.0), w=[ones])
    k.op("dve", lambda e: e.tensor_scalar(out=gp[:], in0=gp[:], scalar1=-1.0, scalar2=None, op0=ALU.mult), r=[gp], w=[gp])
    rT = [k.sb([16, T], F32, f"rT{d}") for d in range(2)]
    rr = SEG["r"][0]
    for d in range(2):
        k.dma(rT[d][:], pT[rr + 16 * d:rr + 16 * (d + 1), :], r=[pT.tensor], w=[rT[d]])
    Krep = k.sb([128, T], F32, "Krep")
    Qrep = k.sb([128, T], F32, "Qrep")
    dec = [k.sb([128, T], F32, f"gdec{d}") for d in range(2)]
    vt = k.sb([128, T], F32, "gv")
    yo = k.sb([128, T], F32, "gyo")
    bufs = scan_bufs(k)
    psx = bufs[3]
    for hs in range(2):
        rq, rk, rv, rg = SEG[f"q{hs}"][0], SEG[f"k{hs}"][0], SEG[f"v{hs}"][0], SEG[f"g{hs}"][0]
        for half in range(2):
            k.dma(Krep[64 * half:64 * half + 64, :], pT[rk:rk + 64, :], r=[pT.tensor], w=[Krep])
            k.dma(Qrep[64 * half:64 * half + 64, :], pT[rq:rq + 64, :], r=[pT.tensor], w=[Qrep])
        k.op("dve", lambda e: e.tensor_scalar(out=Qrep[:], in0=Qrep[:], scalar1=0.125, scalar2=None, op0=ALU.mult), r=[Qrep], w=[Qrep])
        k.dma(vt[:], pT[rv:rv + 128, :], r=[pT.tensor], w=[vt])
        for d in range(2):
            for ci, (t0, n) in enumerate(chunks(T)):
                p_ = psx[ci % 2]
                k.op("pe", lambda e, p_=p_, hs=hs, d=d, t0=t0, n=n: e.matmul(p_[:, 0:n], lhsT=gw[:, hs, d, :], rhs=rT[d][:, t0:t0 + n],
                                                                           start=True, stop=True), r=[gw, rT[d]], w=[p_])
                k.op("act", lambda e, p_=p_, hs=hs, d=d, t0=t0, n=n: e.activation(out=dec[d][:, t0:t0 + n], in_=p_[:, 0:n], func=AF.Exp,
                                                                                bias=gp[:, hs, d, 0:1], scale=-1.0), r=[p_, gp], w=[dec[d]])
            k.op("act", lambda e, d=d: e.activation(out=dec[d][:], in_=dec[d][:], func=AF.Ln, bias=one[:], scale=1.0), r=[dec[d], one], w=[dec[d]])
            k.op("act", lambda e, d=d: e.activation(out=dec[d][:], in_=dec[d][:], func=AF.Exp, scale=-1.0 / 16.0), r=[dec[d]], w=[dec[d]])
            emit_outer_scan(k, cst, bufs, vt, 128, Krep, Qrep, (lambda pt, d=d: dec[d]), d, yo, first=(d == 0))
        k.dma(vt[:], pT[rg:rg + 128, :], r=[pT.tensor], w=[vt])
        k.op("act", lambda e: e.activation(out=vt[:], in_=vt[:], func=AF.Silu), r=[vt], w=[vt])
        for ci, (t0, n) in enumerate(chunks(T)):
            p_ = psx[ci % 2]
            k.op("act", lambda e, t0=t0, n=n: e.activation(out=Krep[:, t0:t0 + n], in_=yo[:, t0:t0 + n], func=AF.Square), r=[yo], w=[Krep])
            k.op("pe", lambda e, p_=p_, t0=t0, n=n: e.matmul(p_[:, 0:n], lhsT=ones[:], rhs=Krep[:, t0:t0 + n], start=True, stop=True),
                 r=[ones, Krep], w=[p_])
            k.op("act", lambda e, p_=p_, t0=t0, n=n: e.activation(out=Qrep[:, t0:t0 + n], in_=p_[:, 0:n], func=AF.Sqrt, bias=epsb[:], scale=1.0 / 128),
                 r=[p_, epsb], w=[Qrep])
        k.op("dve", lambda e: e.reciprocal(out=Qrep[:], in_=Qrep[:]), r=[Qrep], w=[Qrep])
        k.op("dve", lambda e: e.tensor_tensor(out=yo[:], in0=yo[:], in1=Qrep[:], op=ALU.mult), r=[yo, Qrep], w=[yo])
        k.op("dve", lambda e: e.scalar_tensor_tensor(out=yo[:], in0=yo[:], scalar=nw[:, 0:1], in1=vt[:], op0=ALU.mult, op1=ALU.mult),
             r=[yo, nw, vt], w=[yo])
        k.dma(yc[128 * hs:128 * (hs + 1), :], yo[:], r=[yo], q="pool")


def ssd_host_params(inp, li, j):
    cw = inp["ssd_conv_w"][li]
    cb = inp["ssd_conv_b"][li]
    px = np.zeros((128, 2, 8), np.float32)
    for ti in range(2):
        n = 128 if ti == 0 else 64
        c = 192 * j + 128 * ti + np.arange(n)
        px[:n, ti, 0:3] = cw[:, c].T
        px[:n, ti, 3] = cb[c]
        px[:n, ti, 4] = inp["ssd_d"][li][c // 64]
    g = j // 2
    pB = np.zeros((128, 8), np.float32)
    pC = np.zeros((128, 8), np.float32)
    for (arr, base) in ((pB, 768 + 64 * g), (pC, 768 + 128 + 64 * g)):
        c = base + (np.arange(128) % 64)
        arr[:, 0:3] = cw[:, c].T
        arr[:, 3] = cb[c]
    pdt = np.zeros((6, 4), np.float32)
    for d in range(2):
        for h in range(3):
            pdt[3 * d + h, 0] = inp["ssd_dt_bias"][li][d, 3 * j + h]
            pdt[3 * d + h, 1] = inp["ssd_a_log"][li][d, 3 * j + h]
    selr = np.zeros((6, 2, 2, 128), np.float32)
    for d in range(2):
        for m in range(128):
            selr[3 * d + m // 64, d, 0, m] = 1.0
            selr[3 * d + 2, d, 1, m] = 1.0
    selh = np.zeros((6, 6, 128), np.float32)
    for r in range(6):
        selh[r, r, :] = 1.0
    return {"ssd_px": px, "ssd_pB": pB, "ssd_pC": pC, "ssd_pdt": pdt, "selr": selr, "selh": selh}


SSD_SHAPES = {"ssd_px": [128, 2, 8], "ssd_pB": [128, 8], "ssd_pC": [128, 8], "ssd_pdt": [6, 4], "selr": [6, 2, 2, 128],
              "selh": [6, 6, 128]}


def gla_heads(j):
    return [j, j + 4 if j < 2 else j]


def gla_host_params(inp, li, j):
    gp = np.zeros((128, 2, 2, 2), np.float32)
    gw = np.zeros((16, 2, 2, 128), np.float32)
    for s_, h in enumerate(gla_heads(j)):
        for d in range(2):
            cols = 64 * h + (np.arange(128) % 64)
            gp[:, s_, d, 0] = inp["gla_gate_b"][li][d, cols]
            gw[:, s_, d, :] = inp["gla_gate_w"][li][d][:, cols]
    return {"gla_p": gp, "gla_w": gw, "gla_nw": np.ascontiguousarray(inp["gla_norm_w"][li][:, None], dtype=np.float32)}


GLA_SHAPES = {"gla_p": [128, 2, 2, 2], "gla_w": [16, 2, 2, 128], "gla_nw": [128, 1]}


def scan_consts():
    selx = np.zeros((128, 32, 128), np.float32)
    for p in range(128):
        q = p % 64
        selx[p, q // 2, 64 * (q % 2):64 * (q % 2) + 64] = 1.0
    hm = np.zeros((128, 256), np.float32)
    hm[0:64, 128] = 1.0
    hm[64:128, 129] = 1.0
    return {"c_selx": selx, "c_hm": hm}


def build_am(parts=("A",)):
    nc = bass.Bass("TRN2", target_bir_lowering=False)
    k = KB(nc)
    xT = nc.dram_tensor("xT", [D, T], F32, kind="ExternalInput").ap()
    xTc = None
    modv = nc.dram_tensor("modv", [D, 4], F32, kind="ExternalInput").ap()
    wA = nc.dram_tensor("wA", [D, NA], F32, kind="ExternalInput").ap()
    pT = nc.dram_tensor("pT", [NMIX, T], F32, kind="ExternalOutput" if "dumpP" in parts else "Internal").ap()
    gates = nc.dram_tensor("gates", [D, T], BF16, kind="ExternalOutput").ap()
    prm = {}
    cst = {}
    for nm, shp in (("ident", [128, 128]), ("swap", [128, 128]), ("selx", [128, 32, 128]), ("hm", [128, 256])):
        ap = nc.dram_tensor("c_" + nm, shp, F32, kind="ExternalInput").ap()
        cst[nm] = k.sb(shp, F32, nm)
        k.dma(cst[nm][:], ap, w=[cst[nm]])
    mk = k.mark()
    if "A" in parts:
        emit_inproj(k, nc, xT, xTc, modv, wA, pT, gates)
        k.release(mk)
    for (tag, shapes, fn, oname, orows) in (("S5", S5_SHAPES, emit_s5, "ya", 192), ("SSD", SSD_SHAPES, emit_ssd, "yb", 192),
                                            ("GLA", GLA_SHAPES, emit_gla, "yc", 256), ("HY", HY_SHAPES, emit_hyena, "yd", 192)):
        if tag in parts:
            for nm, shp in shapes.items():
                if nm not in prm:
                    prm[nm] = nc.dram_tensor(nm, shp, F32, kind="ExternalInput").ap()
            o = nc.dram_tensor(oname, [orows, T], F32, kind="ExternalOutput").ap()
            fn(k, nc, pT, o, cst, prm)
            k.release(mk)
    k.emit()
    return nc


HY_SHAPES = {"hy_cw": [128, 2, 3, 4], "hy_bias": [128, 2, 2], "hy_w1": [33, 64], "hy_w2": [64, 64], "hy_mlp": [64, 4],
             "hy_w3": [64, 2, 2, 192], "hy_embL": [33, S], "hy_embC": [33, NCTX], "hy_decL": [128, 2, S], "hy_decC": [128, 2, NCTX]}


def hy_host_params(inp, li, j):
    cw = np.zeros((128, 2, 3, 4), np.float32)
    hb = np.zeros((128, 2, 2), np.float32)
    for ti in range(2):
        n = 128 if ti == 0 else 64
        for s_ in range(3):
            c = 768 * s_ + 192 * j + 128 * ti + np.arange(n)
            cw[:n, ti, s_, 0:3] = inp["hy_conv_w"][li][:, c].T
            cw[:n, ti, s_, 3] = inp["hy_conv_b"][li][c]
        c = 192 * j + 128 * ti + np.arange(n)
        hb[:n, ti, :] = inp["hy_bias"][li][:, c].T
    mlp = np.stack([inp["hy_b1"][li], inp["hy_freq1"][li], inp["hy_b2"][li], inp["hy_freq2"][li]], 1)
    w3 = inp["hy_w3"][li].reshape(64, 2, 2, 768)[:, :, :, 192 * j:192 * (j + 1)]
    out = {"hy_cw": cw, "hy_bias": hb, "hy_w1": inp["hy_w1"][li], "hy_w2": inp["hy_w2"][li], "hy_mlp": mlp, "hy_w3": w3}
    deltas = np.abs(np.linspace(math.log(1e-2) / 1.5, math.log(1e-2) / 0.3, 768, dtype=np.float32))[192 * j:192 * (j + 1)]
    for tag, n in (("L", S), ("C", NCTX)):
        t = np.linspace(0.0, 1.0, n, dtype=np.float32)
        freqs = np.linspace(1e-4, 15.0, 16, dtype=np.float32)
        ang = (np.float32(2.0 * math.pi / n) * np.arange(n, dtype=np.float32)[:, None]) * freqs[None, :]
        emb = np.concatenate([t[:, None], np.cos(ang), -np.sin(ang)], -1)
        out["hy_emb" + tag] = emb.T
        dec = np.exp(-t[None, :] * deltas[:, None])
        dd = np.zeros((128, 2, n), np.float32)
        dd[:, 0] = dec[0:128]
        dd[0:64, 1] = dec[128:192]
        out["hy_dec" + tag] = dd
    return {kk: np.ascontiguousarray(v, dtype=np.float32) for kk, v in out.items()}


def emit_hyena(k, nc, pT, yd, cst, prm):
    cw = k.sb([128, 2, 3, 4], F32, "hy_cw")
    hbias = k.sb([128, 2, 2], F32, "hy_bias")
    w1 = k.sb([33, 64], F32, "hy_w1")
    w2 = k.sb([64, 64], F32, "hy_w2")
    mlp = k.sb([64, 4], F32, "hy_mlp")
    w3 = k.sb([64, 2, 2, 192], F32, "hy_w3")
    for t_, nm in ((cw, "hy_cw"), (hbias, "hy_bias"), (w1, "hy_w1"), (w2, "hy_w2"), (mlp, "hy_mlp"), (w3, "hy_w3")):
        k.dma(t_[:], prm[nm], w=[t_])
    fsc = k.sb([64, 2], F32, "hy_fsc")
    k.op("dve", lambda e: e.tensor_scalar(out=fsc[:, 0:1], in0=mlp[:, 1:2], scalar1=1.0 / TWO_PI, scalar2=None, op0=ALU.mult), r=[mlp], w=[fsc])
    k.op("dve", lambda e: e.tensor_scalar(out=fsc[:, 1:2], in0=mlp[:, 3:4], scalar1=1.0 / TWO_PI, scalar2=None, op0=ALU.mult), r=[mlp], w=[fsc])
    h2 = {"L": k.sb([64, S], F32, "h2L"), "C": k.sb([64, NCTX], F32, "h2C")}
    ps = [k.ps() for _ in range(2)]
    mk = k.mark()
    emb = k.sb([33, 512], F32, "emb")
    h1 = k.sb([64, 512], F32, "h1")
    xa_ = k.sb([64, 512], F32, "hyx")
    xi = k.sb([64, 512], I32, "hyxi")
    xf = k.sb([64, 512], F32, "hyxf")

    def sin_layer(dst, src_ps, n, bcol, fcol):
        k.op("dve", lambda e: e.tensor_scalar(out=xa_[:, 0:n], in0=src_ps[0:64, 0:n], scalar1=mlp[:, bcol:bcol + 1], scalar2=fsc[:, fcol:fcol + 1],
                                              op0=ALU.add, op1=ALU.mult), r=[src_ps, mlp, fsc], w=[xa_])
        k.op("dve", lambda e: e.tensor_copy(out=xi[:, 0:n], in_=xa_[:, 0:n]), r=[xa_], w=[xi])
        k.op("dve", lambda e: e.tensor_copy(out=xf[:, 0:n], in_=xi[:, 0:n]), r=[xi], w=[xf])
        k.op("dve", lambda e: e.tensor_tensor(out=xf[:, 0:n], in0=xa_[:, 0:n], in1=xf[:, 0:n], op=ALU.subtract), r=[xa_, xf], w=[xf])
        k.op("dve", lambda e: e.tensor_scalar(out=xf[:, 0:n], in0=xf[:, 0:n], scalar1=0.4999999, scalar2=-0.4999999, op0=ALU.min, op1=ALU.max),
             r=[xf], w=[xf])
        k.op("act", lambda e: e.activation(out=dst, in_=xf[:, 0:n], func=AF.Sin, scale=TWO_PI), r=[xf], w=[h1, h2["L"], h2["C"]])

    for tag, n_tot in (("L", S), ("C", NCTX)):
        for (t0, n) in chunks(n_tot):
            k.dma(emb[:, 0:n], prm["hy_emb" + tag][:, t0:t0 + n], w=[emb])
            k.op("pe", lambda e, n=n: e.matmul(ps[0][0:64, 0:n], lhsT=w1[:], rhs=emb[:, 0:n], start=True, stop=True), r=[w1, emb], w=[ps[0]])
            sin_layer(h1[:, 0:n], ps[0], n, 0, 0)
            k.op("pe", lambda e, n=n: e.matmul(ps[1][0:64, 0:n], lhsT=w2[:], rhs=h1[:, 0:n], start=True, stop=True), r=[w2, h1], w=[ps[1]])
            sin_layer(h2[tag][:, t0:t0 + n], ps[1], n, 2, 1)
    k.release(mk)
    u = k.sb([128, T], F32, "hy_u")
    g = [k.sb([128, T], F32, f"hy_g{i}") for i in range(2)]
    acc = k.sb([128, T], F32, "hy_acc")
    raw = k.sb([128, T], F32, "hy_raw")
    hf = k.sb([128, S], F32, "hy_hf")
    hb = k.sb([128, S], F32, "hy_hb")
    hfc = k.sb([128, NCTX], F32, "hy_hfc")
    hbc = k.sb([128, NCTX], F32, "hy_hbc")
    dec = k.sb([128, 512], F32, "hy_dec")
    rows = [SEG["hv"][0], SEG["hx1"][0], SEG["hx2"][0]]
    for ti, P in ((0, 128), (1, 64)):
        for s_, dst in ((0, u), (1, g[0]), (2, g[1])):
            k.dma(raw[0:P, :], pT[rows[s_] + 128 * ti:rows[s_] + 128 * ti + P, :], r=[pT.tensor], w=[raw])
            emit_conv3(k, dst, raw, cw[:, ti, s_, :], cw[:, ti, s_, 3:4], P, False, wt=cw)
        for o in range(2):
            for tag, n_tot, F_, B_ in (("L", S, hf, hb), ("C", NCTX, hfc, hbc)):
                for dd_, dstf in ((0, F_), (1, B_)):
                    for ci, (t0, n) in enumerate(chunks(n_tot)):
                        p_ = ps[ci % 2]
                        k.dma(dec[0:P, 0:n], prm["hy_dec" + tag][0:P, ti, t0:t0 + n], w=[dec])
                        k.op("pe", lambda e, p_=p_, o=o, dd_=dd_, ti=ti, P=P, tag=tag, t0=t0, n=n: e.matmul(
                            p_[0:P, 0:n], lhsT=w3[:, o, dd_, 128 * ti:128 * ti + P], rhs=h2[tag][:, t0:t0 + n], start=True, stop=True),
                            r=[w3, h2[tag]], w=[p_])
                        k.op("dve", lambda e, p_=p_, dstf=dstf, P=P, t0=t0, n=n: e.tensor_tensor(out=dstf[0:P, t0:t0 + n], in0=p_[0:P, 0:n],
                                                                                             in1=dec[0:P, 0:n], op=ALU.mult), r=[p_, dec], w=[dstf])
            for (a, n_tot, F_, B_) in ((NCTX, S, hf, hb), (0, NCTX, hfc, hbc)):
                k.op("dve", lambda e, a=a, n_tot=n_tot, P=P, ti=ti, o=o: e.tensor_scalar(
                    out=acc[0:P, a:a + n_tot], in0=u[0:P, a:a + n_tot], scalar1=hbias[0:P, ti, o:o + 1], scalar2=None, op0=ALU.mult),
                    r=[u, hbias], w=[acc])
                for tau in range(n_tot):
                    L = n_tot - tau
                    k.op("dve", lambda e, a=a, tau=tau, L=L, P=P, F_=F_: e.scalar_tensor_tensor(
                        out=acc[0:P, a + tau:a + tau + L], in0=u[0:P, a:a + L], scalar=F_[0:P, tau:tau + 1], in1=acc[0:P, a + tau:a + tau + L],
                        op0=ALU.mult, op1=ALU.add), r=[u, F_, acc], w=[acc])
                    if tau > 0:
                        k.op("dve", lambda e, a=a, tau=tau, L=L, P=P, B_=B_: e.scalar_tensor_tensor(
                            out=acc[0:P, a:a + L], in0=u[0:P, a + tau:a + tau + L], scalar=B_[0:P, tau:tau + 1], in1=acc[0:P, a:a + L],
                            op0=ALU.mult, op1=ALU.add), r=[u, B_, acc], w=[acc])
            k.op("dve", lambda e, o=o, P=P: e.tensor_tensor(out=u[0:P, :], in0=acc[0:P, :], in1=g[o][0:P, :], op=ALU.mult), r=[acc, g[o]], w=[u])
        k.dma(yd[128 * ti:128 * ti + P, :], u[0:P, :], r=[u], q="pool")


NT = 64 + 1024
TCH_C = [(0, 64), (64, 512), (576, 512)]
MW = 768


def emit_cast_w(k, dst_bf, src_dram, kch, ncols, wf, it0=0):
    it = it0
    step = 256 if kch <= 8 else 128
    for c0 in range(0, ncols, step):
        n = min(step, ncols - c0)
        w_ = wf[it % 2]
        it += 1
        wv = w_[:, 0:kch * n].rearrange("p (k c) -> p k c", k=kch)
        k.dma(wv, src_dram[:, c0:c0 + n].rearrange("(k p) c -> p k c", p=128), w=[w_])
        k.op("pool", lambda e, wv=wv, c0=c0, n=n: e.tensor_copy(out=dst_bf[:, :, c0:c0 + n], in_=wv), r=[w_], w=[dst_bf])
    return it


def build_c():
    nc = bass.Bass("TRN2", target_bir_lowering=False)
    k = KB(nc)
    di = lambda nm, shp, dt=F32: nc.dram_tensor(nm, shp, dt, kind="ExternalInput").ap()
    xT = di("xT", [D, NT])
    yT = di("yT", [4, MW, NT])
    gT = di("gT", [4, D, NT], BF16)
    gluw = di("gluw", [MW, MW])
    wbr = di("wbr", [4, MW, D])
    wout = di("wout", [D, D])
    mvd = di("modv", [D, 8])
    snw = di("ssd_nw", [128, 6])
    rwd = di("rw", [D, 20])
    rbd = di("rb", [1, 20])
    xmidT = nc.dram_tensor("xmidT", [D, NT], F32, kind="ExternalOutput").ap()
    h2T = nc.dram_tensor("h2T", [D, NT], BF16, kind="ExternalOutput").ap()
    comb = nc.dram_tensor("comb", [NT, 16], F32, kind="ExternalOutput").ap()

    ones = k.sb([128, 128], F32, "ones")
    k.op("dve", lambda e: e.memset(ones[:], 1.0), w=[ones])
    epsb = k.sb([128, 1], F32, "epsb")
    k.op("dve", lambda e: e.memset(epsb[:], EPS), w=[epsb])
    mv = k.sb([128, 16, 8], F32, "mv")
    k.dma(mv[:], mvd.rearrange("(k p) r -> p k r", p=128), w=[mv])
    for col in (3, 5):
        k.op("dve", lambda e, col=col: e.tensor_scalar(out=mv[:, :, col], in0=mv[:, :, col], scalar1=1.0, scalar2=None, op0=ALU.add), r=[mv], w=[mv])
    nw = k.sb([128, 6, 1], F32, "snw")
    k.dma(nw[:, :, 0], snw, w=[nw])
    wf = [k.sb([128, 2048], F32, f"wf{i}") for i in range(2)]
    macc = k.sb([128, 16, NT], F32, "macc")
    ps = [k.ps() for _ in range(4)]
    psn = 0
    mk = k.mark()
    yf = k.sb([128, 6, NT], F32, "yf")
    ybf = k.sb([128, 6, NT], BF16, "ybf")
    t1 = k.sb([128, 6, NT], F32, "t1")
    wb = k.sb([128, 6, D], BF16, "wb")
    gw = k.sb([128, 6, MW], BF16, "gw")
    gt = [k.sb([128, 512], BF16, f"gt{i}") for i in range(2)]
    tmp = [k.sb([128, 512], F32, f"tmp{i}") for i in range(2)]
    wit = 0
    git = 0
    for br in range(4):
        k.dma(yf[:], yT[br].rearrange("(k p) t -> p k t", p=128), w=[yf])
        if br == 0:
            k.op("dve", lambda e: e.tensor_tensor(out=t1[:], in0=yf[:], in1=yf[:], op=ALU.mult), r=[yf], w=[t1])
            k.op("dve", lambda e: e.tensor_scalar(out=t1[:], in0=t1[:], scalar1=0.044715, scalar2=1.0, op0=ALU.mult, op1=ALU.add), r=[t1], w=[t1])
            k.op("dve", lambda e: e.tensor_tensor(out=t1[:], in0=t1[:], in1=yf[:], op=ALU.mult), r=[t1, yf], w=[t1])
            k.op("act", lambda e: e.activation(out=t1[:], in_=t1[:], func=AF.Tanh, scale=math.sqrt(2.0 / math.pi)), r=[t1], w=[t1])
            k.op("dve", lambda e: e.tensor_scalar(out=t1[:], in0=t1[:], scalar1=1.0, scalar2=0.5, op0=ALU.add, op1=ALU.mult), r=[t1], w=[t1])
            k.op("dve", lambda e: e.tensor_tensor(out=yf[:], in0=t1[:], in1=yf[:], op=ALU.mult), r=[t1, yf], w=[yf])
            k.op("pool", lambda e: e.tensor_copy(out=ybf[:], in_=yf[:]), r=[yf], w=[ybf])
            wit = emit_cast_w(k, gw, gluw, 6, MW, wf, wit)
            for oc in range(6):
                for (t0, n) in TCH_C:
                    p_ = ps[psn % 4]
                    psn += 1
                    for kk in range(6):
                        k.op("pe", lambda e, p_=p_, oc=oc, kk=kk, t0=t0, n=n: e.matmul(p_[:, 0:n], lhsT=gw[:, kk, oc * 128:(oc + 1) * 128],
                                                                                     rhs=ybf[:, kk, t0:t0 + n], start=(kk == 0), stop=(kk == 5)),
                             r=[gw, ybf], w=[p_])
                    k.op("act", lambda e, p_=p_, oc=oc, t0=t0, n=n: e.activation(out=t1[:, oc, t0:t0 + n], in_=p_[:, 0:n], func=AF.Sigmoid),
                         r=[p_], w=[t1])
            k.op("dve", lambda e: e.tensor_tensor(out=yf[:], in0=yf[:], in1=t1[:], op=ALU.mult), r=[yf, t1], w=[yf])
        if br == 1:
            for (t0, n) in TCH_C:
                p_ = ps[psn % 4]
                psn += 1
                for kk in range(6):
                    k.op("act", lambda e, kk=kk, t0=t0, n=n: e.activation(out=t1[:, kk, t0:t0 + n], in_=yf[:, kk, t0:t0 + n], func=AF.Square),
                         r=[yf], w=[t1])
                    k.op("pe", lambda e, p_=p_, kk=kk, t0=t0, n=n: e.matmul(p_[:, 0:n], lhsT=ones[:], rhs=t1[:, kk, t0:t0 + n],
                                                                          start=(kk == 0), stop=(kk == 5)), r=[ones, t1], w=[p_])
                k.op("act", lambda e, p_=p_, t0=t0, n=n: e.activation(out=t1[:, 0, t0:t0 + n], in_=p_[:, 0:n], func=AF.Sqrt, bias=epsb[:], scale=1.0 / MW),
                     r=[p_, epsb], w=[t1])
            k.op("dve", lambda e: e.reciprocal(out=t1[:, 0, :], in_=t1[:, 0, :]), r=[t1], w=[t1])
            for kk in range(6):
                k.op("dve", lambda e, kk=kk: e.scalar_tensor_tensor(out=yf[:, kk, :], in0=yf[:, kk, :], scalar=nw[:, kk, 0:1], in1=t1[:, 0, :],
                                                                   op0=ALU.mult, op1=ALU.mult), r=[yf, nw, t1], w=[yf])
        k.op("pool", lambda e: e.tensor_copy(out=ybf[:], in_=yf[:]), r=[yf], w=[ybf])
        wit = emit_cast_w(k, wb, wbr[br], 6, D, wf, wit)
        for dc in range(16):
            for (t0, n) in TCH_C:
                p_ = ps[psn % 4]
                psn += 1
                g_ = gt[git % 2]
                m_ = tmp[git % 2]
                git += 1
                k.dma(g_[:, 0:n], gT[br, dc * 128:(dc + 1) * 128, t0:t0 + n], w=[g_])
                for kk in range(6):
                    k.op("pe", lambda e, p_=p_, dc=dc, kk=kk, t0=t0, n=n: e.matmul(p_[:, 0:n], lhsT=wb[:, kk, dc * 128:(dc + 1) * 128],
                                                                                 rhs=ybf[:, kk, t0:t0 + n], start=(kk == 0), stop=(kk == 5)),
                         r=[wb, ybf], w=[p_])
                if br == 0:
                    k.op("dve", lambda e, p_=p_, g_=g_, dc=dc, t0=t0, n=n: e.tensor_tensor(out=macc[:, dc, t0:t0 + n], in0=p_[:, 0:n], in1=g_[:, 0:n],
                                                                                         op=ALU.mult), r=[p_, g_], w=[macc])
                else:
                    k.op("dve", lambda e, p_=p_, g_=g_, m_=m_, n=n: e.tensor_tensor(out=m_[:, 0:n], in0=p_[:, 0:n], in1=g_[:, 0:n], op=ALU.mult),
                         r=[p_, g_], w=[m_])
                    k.op("pool", lambda e, m_=m_, dc=dc, t0=t0, n=n: e.tensor_tensor(out=macc[:, dc, t0:t0 + n], in0=macc[:, dc, t0:t0 + n],
                                                                                   in1=m_[:, 0:n], op=ALU.add), r=[m_, macc], w=[macc])
    k.release(mk)
    mk2 = k.mark()
    mbf = k.sb([128, 16, NT], BF16, "mbf")
    k.op("pool", lambda e: e.tensor_copy(out=mbf[:], in_=macc[:]), r=[macc], w=[mbf])
    xmid = macc
    wo = [k.sb([128, 16, 128], BF16, f"wo{i}") for i in range(2)]
    xin = [k.sb([128, NT], F32, f"xin{i}") for i in range(2)]
    for dc in range(16):
        w_ = wf[dc % 2]
        wo_ = wo[dc % 2]
        x_ = xin[dc % 2]
        wv = w_[:].rearrange("p (k c) -> p k c", k=16)
        k.dma(wv, wout[:, dc * 128:(dc + 1) * 128].rearrange("(k p) c -> p k c", p=128), w=[w_])
        k.op("pool", lambda e, wv=wv, wo_=wo_: e.tensor_copy(out=wo_[:], in_=wv), r=[w_], w=[wo_])
        k.dma(x_[:], xT[dc * 128:(dc + 1) * 128, :], w=[x_])
        for (t0, n) in TCH_C:
            p_ = ps[psn % 4]
            psn += 1
            for kk in range(16):
                k.op("pe", lambda e, p_=p_, wo_=wo_, kk=kk, t0=t0, n=n: e.matmul(p_[:, 0:n], lhsT=wo_[:, kk, :], rhs=mbf[:, kk, t0:t0 + n],
                                                                               start=(kk == 0), stop=(kk == 15)), r=[wo_, mbf], w=[p_])
            gcol = 1 if t0 == 0 else 0
            k.op("dve", lambda e, p_=p_, x_=x_, dc=dc, t0=t0, n=n, gcol=gcol: e.scalar_tensor_tensor(
                out=xmid[:, dc, t0:t0 + n], in0=p_[:, 0:n], scalar=mv[:, dc, gcol:gcol + 1], in1=x_[:, t0:t0 + n], op0=ALU.mult, op1=ALU.add),
                r=[p_, mv, x_, mbf], w=[xmid])
        k.dma(xmidT[dc * 128:(dc + 1) * 128, :], xmid[:, dc, :], r=[xmid], q="pool")
    k.release(mk2)
    h2 = k.sb([128, 16, NT], F32, "h2")
    rstd = k.sb([128, NT], F32, "rstd")
    sq = [k.sb([128, 512], F32, f"sq{i}") for i in range(2)]
    for (t0, n) in TCH_C:
        p_ = ps[psn % 4]
        psn += 1
        for kk in range(16):
            s_ = sq[kk % 2]
            k.op("act", lambda e, s_=s_, kk=kk, t0=t0, n=n: e.activation(out=s_[:, 0:n], in_=xmid[:, kk, t0:t0 + n], func=AF.Square), r=[xmid], w=[s_])
            k.op("pe", lambda e, p_=p_, s_=s_, kk=kk, n=n: e.matmul(p_[:, 0:n], lhsT=ones[:], rhs=s_[:, 0:n], start=(kk == 0), stop=(kk == 15)),
                 r=[ones, s_], w=[p_])
        k.op("act", lambda e, p_=p_, t0=t0, n=n: e.activation(out=rstd[:, t0:t0 + n], in_=p_[:, 0:n], func=AF.Sqrt, bias=epsb[:], scale=1.0 / D),
             r=[p_, epsb], w=[rstd])
    k.op("dve", lambda e: e.reciprocal(out=rstd[:], in_=rstd[:]), r=[rstd], w=[rstd])
    hb = [k.sb([128, NT], BF16, f"hb{i}") for i in range(2)]
    for kk in range(16):
        k.op("dve", lambda e, kk=kk: e.tensor_tensor(out=h2[:, kk, :], in0=xmid[:, kk, :], in1=rstd[:], op=ALU.mult), r=[xmid, rstd], w=[h2])
        for (t0, n) in TCH_C:
            mo = 4 if t0 == 0 else 2
            k.op("dve", lambda e, kk=kk, t0=t0, n=n, mo=mo: e.tensor_scalar(out=h2[:, kk, t0:t0 + n], in0=h2[:, kk, t0:t0 + n],
                                                                           scalar1=mv[:, kk, mo + 1:mo + 2], scalar2=mv[:, kk, mo:mo + 1],
                                                                           op0=ALU.mult, op1=ALU.add), r=[h2, mv], w=[h2])
        hb_ = hb[kk % 2]
        k.op("pool", lambda e, hb_=hb_, kk=kk: e.tensor_copy(out=hb_[:], in_=h2[:, kk, :]), r=[h2], w=[hb_])
        k.dma(h2T[kk * 128:(kk + 1) * 128, :], hb_[:], r=[hb_], q="pool")
    rw = k.sb([128, 16, 20], F32, "rw")
    rb = k.sb([1, 20], F32, "rb")
    k.dma(rw[:], rwd.rearrange("(k p) c -> p k c", p=128), w=[rw])
    k.dma(rb[:], rbd, w=[rb])
    lg = k.sb([128, 20], F32, "lg")
    sm = {nm: k.sb([128, 4], F32, "r_" + nm) for nm in ("ohg", "eg", "esel", "oh1", "msk", "oh2", "within")}
    sc = {nm: k.sb([128, 1], F32, "r_" + nm) for nm in ("gmax", "ngmax", "ssum", "pg", "m1", "nm1", "m2", "e2", "den", "w1", "w2")}
    cb = [k.sb([128, 16], F32, f"cb{i}") for i in range(2)]
    V = lambda e: e
    for ti, (t0, n) in enumerate([(0, 64)] + [(64 + 128 * i, 128) for i in range(8)]):
        p_ = ps[psn % 4]
        psn += 1
        for kk in range(16):
            k.op("pe", lambda e, p_=p_, kk=kk, t0=t0, n=n: e.matmul(p_[0:n, 0:20], lhsT=h2[:, kk, t0:t0 + n], rhs=rw[:, kk, :],
                                                                   start=(kk == 0), stop=False), r=[h2, rw], w=[p_])
        k.op("pe", lambda e, p_=p_, n=n: e.matmul(p_[0:n, 0:20], lhsT=ones[0:1, 0:n], rhs=rb[0:1, :], start=False, stop=True), r=[ones, rb], w=[p_])
        k.op("act", lambda e, p_=p_, n=n: e.activation(out=lg[0:n, :], in_=p_[0:n, 0:20], func=AF.Copy), r=[p_], w=[lg])
        P = n
        o = lambda eng, fn, r, w: k.op(eng, fn, r=r, w=w)
        o("dve", lambda e: e.reduce_max(out=sc["gmax"][0:P], in_=lg[0:P, 0:4], axis=AX.X), [lg], [sc["gmax"]])
        o("dve", lambda e: e.tensor_scalar(out=sm["ohg"][0:P], in0=lg[0:P, 0:4], scalar1=sc["gmax"][0:P, 0:1], scalar2=None, op0=ALU.is_ge),
          [lg, sc["gmax"]], [sm["ohg"]])
        o("dve", lambda e: e.tensor_scalar(out=sc["ngmax"][0:P], in0=sc["gmax"][0:P], scalar1=-1.0, scalar2=None, op0=ALU.mult), [sc["gmax"]], [sc["ngmax"]])
        o("act", lambda e: e.activation(out=sm["eg"][0:P], in_=lg[0:P, 0:4], func=AF.Exp, bias=sc["ngmax"][0:P], scale=1.0), [lg, sc["ngmax"]], [sm["eg"]])
        o("dve", lambda e: e.reduce_sum(out=sc["ssum"][0:P], in_=sm["eg"][0:P], axis=AX.X), [sm["eg"]], [sc["ssum"]])
        o("dve", lambda e: e.reciprocal(out=sc["pg"][0:P], in_=sc["ssum"][0:P]), [sc["ssum"]], [sc["pg"]])
        o("dve", lambda e: e.tensor_scalar(out=sm["esel"][0:P], in0=lg[0:P, 4:8], scalar1=sm["ohg"][0:P, 0:1], scalar2=None, op0=ALU.mult),
          [lg, sm["ohg"]], [sm["esel"]])
        for g_ in range(1, 4):
            o("dve", lambda e, g_=g_: e.scalar_tensor_tensor(out=sm["esel"][0:P], in0=lg[0:P, 4 + 4 * g_:8 + 4 * g_], scalar=sm["ohg"][0:P, g_:g_ + 1],
                                                           in1=sm["esel"][0:P], op0=ALU.mult, op1=ALU.add), [lg, sm["ohg"], sm["esel"]], [sm["esel"]])
        o("dve", lambda e: e.reduce_max(out=sc["m1"][0:P], in_=sm["esel"][0:P], axis=AX.X), [sm["esel"]], [sc["m1"]])
        o("dve", lambda e: e.tensor_scalar(out=sm["oh1"][0:P], in0=sm["esel"][0:P], scalar1=sc["m1"][0:P, 0:1], scalar2=None, op0=ALU.is_ge),
          [sm["esel"], sc["m1"]], [sm["oh1"]])
        o("dve", lambda e: e.scalar_tensor_tensor(out=sm["msk"][0:P], in0=sm["oh1"][0:P], scalar=-1.0e30, in1=sm["esel"][0:P], op0=ALU.mult, op1=ALU.add),
          [sm["oh1"], sm["esel"]], [sm["msk"]])
        o("dve", lambda e: e.reduce_max(out=sc["m2"][0:P], in_=sm["msk"][0:P], axis=AX.X), [sm["msk"]], [sc["m2"]])
        o("dve", lambda e: e.tensor_scalar(out=sm["oh2"][0:P], in0=sm["msk"][0:P], scalar1=sc["m2"][0:P, 0:1], scalar2=None, op0=ALU.is_ge),
          [sm["msk"], sc["m2"]], [sm["oh2"]])
        o("dve", lambda e: e.tensor_scalar(out=sc["nm1"][0:P], in0=sc["m1"][0:P], scalar1=-1.0, scalar2=None, op0=ALU.mult), [sc["m1"]], [sc["nm1"]])
        o("act", lambda e: e.activation(out=sc["e2"][0:P], in_=sc["m2"][0:P], func=AF.Exp, bias=sc["nm1"][0:P], scale=1.0), [sc["m2"], sc["nm1"]], [sc["e2"]])
        o("dve", lambda e: e.tensor_scalar(out=sc["den"][0:P], in0=sc["e2"][0:P], scalar1=1.0, scalar2=None, op0=ALU.add), [sc["e2"]], [sc["den"]])
        o("dve", lambda e: e.reciprocal(out=sc["den"][0:P], in_=sc["den"][0:P]), [sc["den"]], [sc["den"]])
        o("dve", lambda e: e.tensor_tensor(out=sc["w1"][0:P], in0=sc["den"][0:P], in1=sc["pg"][0:P], op=ALU.mult), [sc["den"], sc["pg"]], [sc["w1"]])
        o("dve", lambda e: e.tensor_tensor(out=sc["w2"][0:P], in0=sc["w1"][0:P], in1=sc["e2"][0:P], op=ALU.mult), [sc["w1"], sc["e2"]], [sc["w2"]])
        o("dve", lambda e: e.tensor_scalar(out=sm["within"][0:P], in0=sm["oh1"][0:P], scalar1=sc["w1"][0:P, 0:1], scalar2=None, op0=ALU.mult),
          [sm["oh1"], sc["w1"]], [sm["within"]])
        o("dve", lambda e: e.scalar_tensor_tensor(out=sm["within"][0:P], in0=sm["oh2"][0:P], scalar=sc["w2"][0:P, 0:1], in1=sm["within"][0:P],
                                                  op0=ALU.mult, op1=ALU.add), [sm["oh2"], sc["w2"], sm["within"]], [sm["within"]])
        c_ = cb[ti % 2]
        for g_ in range(4):
            o("dve", lambda e, g_=g_, c_=c_: e.tensor_scalar(out=c_[0:P, 4 * g_:4 * g_ + 4], in0=sm["within"][0:P], scalar1=sm["ohg"][0:P, g_:g_ + 1],
                                                           scalar2=None, op0=ALU.mult), [sm["within"], sm["ohg"]], [c_])
        k.dma(comb[t0:t0 + n, :], c_[0:P, :], r=[c_], q="pool")
    k.emit()
    return nc


TT = B * T
FF = 1024


def build_e():
    nc = bass.Bass("TRN2", target_bir_lowering=False)
    k = KB(nc)
    h2T = nc.dram_tensor("h2T", [D, TT], BF16, kind="ExternalInput").ap()
    cmb = nc.dram_tensor("cmb", [2, 128, TT], F32, kind="ExternalInput").ap()
    wg = nc.dram_tensor("wg", [2, D, FF], F32, kind="ExternalInput").ap()
    wu = nc.dram_tensor("wu", [2, D, FF], F32, kind="ExternalInput").ap()
    wd = nc.dram_tensor("wd", [2, FF, D], F32, kind="ExternalInput").ap()
    part = nc.dram_tensor("part", [D, TT], F32, kind="ExternalOutput").ap()
    Wg = k.sb([128, 16, FF], BF16, "Wg")
    Wu = k.sb([128, 16, FF], BF16, "Wu")
    Wd = k.sb([128, 8, D], BF16, "Wd")
    wf = [k.sb([128, 2048], F32, f"wf{i}") for i in range(2)]
    hch = [k.sb([128, 16, 512], BF16, f"hch{i}") for i in range(2)]
    abf = [k.sb([128, 8, 512], BF16, f"abf{i}") for i in range(2)]
    cch = [k.sb([128, 512], F32, f"cch{i}") for i in range(2)]
    tmp = [k.sb([128, 512], F32, f"tmp{i}") for i in range(2)]
    ost = [k.sb([128, 512], F32, f"ost{i}") for i in range(3)]
    prv = [k.sb([128, 512], F32, f"prv{i}") for i in range(3)]
    pg = [k.ps() for _ in range(2)]
    pu = [k.ps() for _ in range(2)]
    po = [k.ps() for _ in range(3)]
    wit = 0
    io = 0
    for ex in range(2):
        wit = emit_cast_w(k, Wg, wg[ex], 16, FF, wf, wit)
        wit = emit_cast_w(k, Wu, wu[ex], 16, FF, wf, wit)
        wit = emit_cast_w(k, Wd, wd[ex], 8, D, wf, wit)
        for tc in range(TT // 512):
            t0 = tc * 512
            h_ = hch[tc % 2]
            a_ = abf[tc % 2]
            c_ = cch[tc % 2]
            k.dma(h_[:], h2T[:, t0:t0 + 512].rearrange("(k p) t -> p k t", p=128), w=[h_])
            k.dma(c_[:], cmb[ex, :, t0:t0 + 512], w=[c_])
            for fc in range(8):
                g_ = pg[fc % 2]
                u_ = pu[fc % 2]
                m_ = tmp[fc % 2]
                for kk in range(16):
                    k.op("pe", lambda e, g_=g_, h_=h_, fc=fc, kk=kk: e.matmul(g_[:], lhsT=Wg[:, kk, fc * 128:(fc + 1) * 128], rhs=h_[:, kk, :],
                                                                            start=(kk == 0), stop=(kk == 15)), r=[Wg, h_], w=[g_])
                for kk in range(16):
                    k.op("pe", lambda e, u_=u_, h_=h_, fc=fc, kk=kk: e.matmul(u_[:], lhsT=Wu[:, kk, fc * 128:(fc + 1) * 128], rhs=h_[:, kk, :],
                                                                            start=(kk == 0), stop=(kk == 15)), r=[Wu, h_], w=[u_])
                k.op("act", lambda e, g_=g_, m_=m_: e.activation(out=m_[:], in_=g_[:], func=AF.Silu), r=[g_], w=[m_])
                k.op("dve", lambda e, u_=u_, m_=m_: e.tensor_tensor(out=m_[:], in0=m_[:], in1=u_[:], op=ALU.mult), r=[m_, u_], w=[m_])
                k.op("pool", lambda e, m_=m_, a_=a_, c_=c_, fc=fc: e.tensor_tensor(out=a_[:, fc, :], in0=m_[:], in1=c_[:], op=ALU.mult),
                     r=[m_, c_], w=[a_])
            for dc in range(16):
                o_ = po[io % 3]
                s_ = ost[io % 3]
                p_ = prv[io % 3]
                io += 1
                key = f"part_{dc}_{tc}"
                for fc in range(8):
                    k.op("pe", lambda e, o_=o_, a_=a_, dc=dc, fc=fc: e.matmul(o_[:], lhsT=Wd[:, fc, dc * 128:(dc + 1) * 128], rhs=a_[:, fc, :],
                                                                            start=(fc == 0), stop=(fc == 7)), r=[Wd, a_], w=[o_])
                if ex == 0:
                    k.op("act", lambda e, o_=o_, s_=s_: e.activation(out=s_[:], in_=o_[:], func=AF.Copy), r=[o_], w=[s_])
                else:
                    k.dma(p_[:], part[dc * 128:(dc + 1) * 128, t0:t0 + 512], r=[key], w=[p_])
                    k.op("dve", lambda e, o_=o_, s_=s_, p_=p_: e.tensor_tensor(out=s_[:], in0=o_[:], in1=p_[:], op=ALU.add), r=[o_, p_], w=[s_])
                k.dma(part[dc * 128:(dc + 1) * 128, t0:t0 + 512], s_[:], r=[s_], w=[key], q="pool")
    k.emit()
    return nc


def run_e(inp, li, h2T, comb):
    maps = []
    for e in range(NCORES):
        cb = np.ascontiguousarray(np.broadcast_to(comb[:, 2 * e:2 * e + 2].T[:, None, :], (2, 128, TT)))
        maps.append({"h2T": h2T, "cmb": cb, "wg": inp["moe_w_gate"][li][2 * e:2 * e + 2], "wu": inp["moe_w_up"][li][2 * e:2 * e + 2],
                     "wd": inp["moe_w_down"][li][2 * e:2 * e + 2]})
    res = _run(build_e(), maps)
    return [r["part"] for r in res]


def build_r():
    nc = bass.Bass("TRN2", target_bir_lowering=False)
    k = KB(nc)
    xmidT = nc.dram_tensor("xmidT", [D, NT], F32, kind="ExternalInput").ap()
    parts = nc.dram_tensor("parts", [NCORES, D, NT], F32, kind="ExternalInput").ap()
    gvd = nc.dram_tensor("gv", [D, 4], F32, kind="ExternalInput").ap()
    xendT = nc.dram_tensor("xendT", [D, NT], F32, kind="ExternalOutput").ap()
    outT = nc.dram_tensor("outT", [D, NT], F32, kind="ExternalOutput").ap()
    ones = k.sb([128, 128], F32, "ones")
    k.op("dve", lambda e: e.memset(ones[:], 1.0), w=[ones])
    epsb = k.sb([128, 1], F32, "epsb")
    k.op("dve", lambda e: e.memset(epsb[:], EPS), w=[epsb])
    gv = k.sb([128, 16, 4], F32, "gv")
    k.dma(gv[:], gvd.rearrange("(k p) r -> p k r", p=128), w=[gv])
    xend = k.sb([128, 16, NT], F32, "xend")
    pb = [[k.sb([128, NT], F32, f"pb{i}_{e}") for e in range(NCORES)] for i in range(2)]
    xm = [k.sb([128, NT], F32, f"xm{i}") for i in range(2)]
    ps = [k.ps() for _ in range(3)]
    for dc in range(16):
        bufs = pb[dc % 2]
        x_ = xm[dc % 2]
        k.dma(x_[:], xmidT[dc * 128:(dc + 1) * 128, :], w=[x_])
        for e_ in range(NCORES):
            k.dma(bufs[e_][:], parts[e_, dc * 128:(dc + 1) * 128, :], w=[bufs[e_]])
        for (a_, b_, eng) in ((0, 1, "dve"), (2, 3, "pool"), (4, 5, "dve"), (6, 7, "pool"), (0, 2, "dve"), (4, 6, "pool"), (0, 4, "dve")):
            k.op(eng, lambda e, a_=a_, b_=b_, bufs=bufs: e.tensor_tensor(out=bufs[a_][:], in0=bufs[a_][:], in1=bufs[b_][:], op=ALU.add),
                 r=[bufs[a_], bufs[b_]], w=[bufs[a_]])
        for (t0, n, gc) in ((0, 64, 1), (64, 1024, 0)):
            k.op("dve", lambda e, bufs=bufs, x_=x_, dc=dc, t0=t0, n=n, gc=gc: e.scalar_tensor_tensor(
                out=xend[:, dc, t0:t0 + n], in0=bufs[0][:, t0:t0 + n], scalar=gv[:, dc, gc:gc + 1], in1=x_[:, t0:t0 + n], op0=ALU.mult, op1=ALU.add),
                r=[bufs[0], gv, x_], w=[xend])
        k.dma(xendT[dc * 128:(dc + 1) * 128, :], xend[:, dc, :], r=[xend], q="pool")
    rstd = k.sb([128, NT], F32, "rstd")
    sq = [k.sb([128, 512], F32, f"sq{i}") for i in range(2)]
    for ci, (t0, n) in enumerate(TCH_C):
        p_ = ps[ci % 3]
        for kk in range(16):
            s_ = sq[kk % 2]
            k.op("act", lambda e, s_=s_, kk=kk, t0=t0, n=n: e.activation(out=s_[:, 0:n], in_=xend[:, kk, t0:t0 + n], func=AF.Square), r=[xend], w=[s_])
            k.op("pe", lambda e, p_=p_, s_=s_, kk=kk, n=n: e.matmul(p_[:, 0:n], lhsT=ones[:], rhs=s_[:, 0:n], start=(kk == 0), stop=(kk == 15)),
                 r=[ones, s_], w=[p_])
        k.op("act", lambda e, p_=p_, t0=t0, n=n: e.activation(out=rstd[:, t0:t0 + n], in_=p_[:, 0:n], func=AF.Sqrt, bias=epsb[:], scale=1.0 / D),
             r=[p_, epsb], w=[rstd])
    k.op("dve", lambda e: e.reciprocal(out=rstd[:], in_=rstd[:]), r=[rstd], w=[rstd])
    ob = pb[0]
    for kk in range(16):
        o_ = ob[kk % 4]
        k.op("dve", lambda e, o_=o_, kk=kk: e.scalar_tensor_tensor(out=o_[:], in0=xend[:, kk, :], scalar=gv[:, kk, 2:3], in1=rstd[:],
                                                                 op0=ALU.mult, op1=ALU.mult), r=[xend, gv, rstd], w=[o_])
        k.dma(outT[kk * 128:(kk + 1) * 128, :], o_[:], r=[o_], q="pool")
    k.emit()
    return nc


def run_r(inp, mods, li, xmid, parts):
    m = mods[li]
    maps = []
    for core in range(NCORES):
        b, q = core // 4, core % 4
        idx = _tok_idx(q)
        gv = np.stack([m[b, 5 * D:6 * D], m[2, 5 * D:6 * D], inp["final_norm_w"], np.zeros(D, np.float32)], 1)
        maps.append({"xmidT": np.ascontiguousarray(xmid[b, idx].T),
                     "parts": np.ascontiguousarray(np.stack([p[:, b * T + idx] for p in parts], 0)),
                     "gv": np.ascontiguousarray(gv)})
    res = _run(build_r(), maps)
    xend = np.zeros((B, T, D), np.float32)
    out = np.zeros((B, T, D), np.float32)
    for core in range(NCORES):
        b, q = core // 4, core % 4
        idx = _tok_idx(q)
        xend[b, idx] = res[core]["xendT"].T
        out[b, idx] = res[core]["outT"].T
    return xend, out


def _colmajor(a):
    return a.reshape(64, 64, -1).transpose(1, 0, 2).reshape(S, -1)


ALL_PARTS = ("A", "S5", "SSD", "GLA", "HY")


def run_mixers(inp, mods, li, x_lat, x_ctx, parts=ALL_PARTS):
    maps = []
    for core in range(NCORES):
        b, j = core // 4, core % 4
        xT = np.ascontiguousarray(np.concatenate([x_ctx[b], x_lat[b]], 0).T)
        cols, _ = core_cols(j)
        m = mods[li]
        modv = np.stack([m[b, 0:D], m[b, D:2 * D], m[2, 0:D], m[2, D:2 * D]], 1)
        mp = {"xT": xT, "modv": np.ascontiguousarray(modv), "wA": np.ascontiguousarray(inp["w_in"][li][:, cols])}
        mp.update(host_consts())
        mp.update(scan_consts())
        mp.update(s5_host_params(inp, li, j))
        mp.update(ssd_host_params(inp, li, j))
        mp.update(gla_host_params(inp, li, j))
        mp.update(hy_host_params(inp, li, j))
        maps.append(mp)
    nc = build_am(parts=parts)
    return _run(nc, maps)


def assemble_mixers(res):
    ys, gs = [], []
    for b in range(B):
        r = res[4 * b:4 * b + 4]
        ya = np.concatenate([r[j]["ya"] for j in range(4)], 0)
        yb = np.concatenate([r[j]["yb"] for j in range(4)], 0)
        lat = yb[:, NCTX:].reshape(MW, 64, 64).transpose(0, 2, 1).reshape(MW, S)
        yb = np.concatenate([yb[:, :NCTX], lat], 1)
        yc = np.concatenate([r[0]["yc"][0:128], r[1]["yc"][0:128], r[2]["yc"][0:128], r[3]["yc"][0:128],
                             r[0]["yc"][128:256], r[1]["yc"][128:256]], 0)
        yd = np.concatenate([r[j]["yd"] for j in range(4)], 0)
        ys.append(np.stack([ya, yb, yc, yd], 0))
        gs.append(np.stack([r[j]["gates"] for j in range(4)], 0))
    return ys, gs


def _tok_idx(q):
    return np.concatenate([np.arange(64 * q, 64 * q + 64), NCTX + np.arange(1024 * q, 1024 * q + 1024)])


def run_c(inp, mods, li, x_lat, x_ctx, res_am):
    ys, gs = assemble_mixers(res_am)
    m = mods[li]
    maps = []
    rw = np.ascontiguousarray(np.concatenate([inp["moe_group_w"][li], inp["moe_expert_w"][li]], 1))
    rb = np.ascontiguousarray(np.concatenate([inp["moe_group_b"][li], inp["moe_expert_b"][li]])[None, :])
    for core in range(NCORES):
        b, q = core // 4, core % 4
        idx = _tok_idx(q)
        xall = np.concatenate([x_ctx[b], x_lat[b]], 0)
        z = np.zeros(D, np.float32)
        modv = np.stack([m[b, 2 * D:3 * D], m[2, 2 * D:3 * D], m[b, 3 * D:4 * D], m[b, 4 * D:5 * D], m[2, 3 * D:4 * D], m[2, 4 * D:5 * D], z, z], 1)
        maps.append({"xT": np.ascontiguousarray(xall[idx].T), "yT": np.ascontiguousarray(ys[b][:, :, idx]),
                     "gT": np.ascontiguousarray(gs[b][:, :, idx]), "gluw": inp["s5_glu_w"][li], "wbr": inp["w_branch"][li],
                     "wout": inp["w_out"][li], "modv": np.ascontiguousarray(modv),
                     "ssd_nw": np.ascontiguousarray(inp["ssd_norm_w"][li].reshape(6, 128).T), "rw": rw, "rb": rb})
    res = _run(build_c(), maps)
    xmid = np.zeros((B, T, D), np.float32)
    h2T = np.zeros((D, B * T), ml_dtypes.bfloat16)
    comb = np.zeros((B * T, 16), np.float32)
    for core in range(NCORES):
        b, q = core // 4, core % 4
        idx = _tok_idx(q)
        xmid[b, idx] = res[core]["xmidT"].T
        h2T[:, b * T + idx] = res[core]["h2T"]
        comb[b * T + idx] = res[core]["comb"]
    return xmid, h2T, comb


def kernel(**inputs):
    inp = {k_: np.asarray(v) for k_, v in inputs.items()}
    mods = run_mods(inp["c"], inp["c_ctx"], inp["ada_w"], inp["ada_b"])
    x_lat, x_ctx = inp["x"], inp["ctx"]
    out = None
    for li in range(DEPTH):
        res = run_mixers(inp, mods, li, x_lat, x_ctx)
        xmid, h2T, comb = run_c(inp, mods, li, x_lat, x_ctx, res)
        del res
        parts = run_e(inp, li, h2T, comb)
        xend, out = run_r(inp, mods, li, xmid, parts)
        del parts
        x_ctx, x_lat = xend[:, :NCTX], xend[:, NCTX:]
    return np.ascontiguousarray(out[:, NCTX:])
```

```python
import math
from contextlib import ExitStack
import numpy as np
import ml_dtypes
import concourse.bass as bass
import concourse.mybir as mybir
from concourse.bass_utils import run_bass_kernel_spmd

F32 = mybir.dt.float32
BF16 = mybir.dt.bfloat16
I32 = mybir.dt.int32
AF = mybir.ActivationFunctionType
ALU = mybir.AluOpType
AX = mybir.AxisListType

D = 2048
B = 2
S = 4096
DEPTH = 2
NCTX = 256
T = NCTX + S
EPS = 1e-6
NCORES = 8


class _Op:
    pass


class Tile:
    def __init__(self, ap, name):
        self.ap = ap
        self.name = name

    def __getitem__(self, idx):
        return self.ap[idx]


class _Rec:
    def __init__(self):
        self.call = None

    def __getattr__(self, name):
        def f(*a, **kw):
            self.call = (name, a, kw)
            return self
        return f


class KB:
    COMPUTE = ("pe", "dve", "act", "pool")

    def __init__(self, nc, n_dma_sems=20):
        self.nc = nc
        self.es = ExitStack()
        self.eng = {"pe": nc.tensor, "dve": nc.vector, "act": nc.scalar, "pool": nc.gpsimd, "sp": nc.sync}
        self.ops = []
        self.lastw = {}
        self.reads = {}
        self.n_dma_sems = n_dma_sems
        self._uid = 0
        self.psum_banks = None
        self.bar_from = 0

    ARENA_WORDS = 52500

    def _arena(self):
        if getattr(self, "arena", None) is None:
            self.arena = self.es.enter_context(self.nc.sbuf_tensor("arena", [128, self.ARENA_WORDS], F32))
            self.top = 0
            self.psum = [self.es.enter_context(self.nc.psum_tensor(f"psb{i}", [128, 512], F32)) for i in range(8)]
            self.psn = 0
        return self.arena

    def sb(self, shape, dtype=F32, name=None):
        ar = self._arena()
        self._uid += 1
        name = f"{name or 't'}_{self._uid}"
        P = shape[0]
        n = int(np.prod(shape[1:]))
        esz = 2 if dtype == BF16 else 4
        words = (n * esz + 3) // 4
        assert self.top + words <= self.ARENA_WORDS, f"arena overflow allocating {name} {shape}: top={self.top}"
        ap = ar[0:P, self.top:self.top + words]
        self.top += words
        if dtype != F32:
            ap = ap.bitcast(dtype)
        if esz == 2 and (n % 2):
            ap = ap[:, 0:n]
        if len(shape) == 3:
            ap = ap.rearrange("p (a b) -> p a b", a=shape[1])
        elif len(shape) == 4:
            ap = ap.rearrange("p (a b c) -> p a b c", a=shape[1], b=shape[2])
        return Tile(ap, name)

    def ps(self, shape=(128, 512), dtype=F32, name=None):
        self._arena()
        t = self.psum[self.psn % 8]
        self.psn += 1
        assert self.psn <= 8, "only 8 PSUM banks"
        return t

    def mark(self):
        self._arena()
        return (self.top, self.psn)

    def release(self, mark):
        self.barrier()
        self.top, self.psn = mark

    def barrier(self):
        last = {}
        dmas = []
        for o in self.ops[self.bar_from:]:
            if o.isdma:
                dmas.append(o)
            else:
                last[o.eng] = o
        deps = list(last.values()) + dmas
        for e in ("pe", "dve", "act", "pool", "sp"):
            o = self.op(e, lambda en: en.nop())
            o.deps = list(deps)
        self.bar_from = len(self.ops)
        self.lastw = {}
        self.reads = {}

    def dram(self, name, shape, dtype=F32, kind="Internal"):
        return self.nc.dram_tensor(name, list(shape), dtype, kind=kind)

    @staticmethod
    def _key(t):
        return t if isinstance(t, str) else t.name

    def op(self, eng, fn, r=(), w=()):
        o = _Op()
        o.eng = eng
        rec = _Rec()
        fn(rec)
        name_, a_, kw_ = rec.call
        o.fn = lambda e: getattr(e, name_)(*a_, **kw_)
        o.needed = False
        o.sig = None
        o.isdma = False
        o.seq = len(self.ops)
        deps = []
        for t in r:
            k = self._key(t)
            lw = self.lastw.get(k)
            if lw is not None:
                deps.append(lw)
        for t in w:
            k = self._key(t)
            lw = self.lastw.get(k)
            if lw is not None:
                deps.append(lw)
            deps.extend(self.reads.get(k, ()))
        dd = []
        seen = set()
        for d in deps:
            if d.seq in seen:
                continue
            seen.add(d.seq)
            if eng == "pe" and d.eng == "pe" and not d.isdma:
                continue
            dd.append(d)
        o.deps = dd
        for t in w:
            k = self._key(t)
            self.lastw[k] = o
            self.reads[k] = []
        for t in r:
            k = self._key(t)
            self.reads.setdefault(k, []).append(o)
        self.ops.append(o)
        return o

    def dma(self, out, in_, r=(), w=(), q="sp", **kw):
        o = self.op(q, lambda e: e.dma_start(out=out, in_=in_, **kw), r=r, w=w)
        o.isdma = True
        return o

    def emit(self, final_wait=()):
        nc = self.nc
        for o in self.ops:
            for d in o.deps:
                d.needed = True
        sems = {e: self.es.enter_context(nc.semaphore(f"s_{e}")) for e in self.COMPUTE}
        sigcnt = {e: 0 for e in self.COMPUTE}
        queues = sorted({o.eng for o in self.ops if o.isdma})
        dsems = {q: [self.es.enter_context(nc.semaphore(f"d_{q}{i}")) for i in range(self.n_dma_sems)] for q in queues}
        dcnt = {q: [0] * self.n_dma_sems for q in queues}
        dnext = {q: 0 for q in queues}
        waited = {}
        all_dma = []

        def wait(engname, sem, key, val):
            if waited.get((engname, key), 0) >= val:
                return
            self.eng[engname].wait_ge(sem, val)
            waited[(engname, key)] = val

        for o in self.ops:
            e = self.eng[o.eng]
            for d in o.deps:
                if d.isdma:
                    wait(o.eng, d.dsem, ("d", d.eng, d.dslot), d.dval)
                else:
                    wait(o.eng, sems[d.eng], ("c", d.eng), d.sig)
            if o.isdma:
                q = o.eng
                slot = dnext[q]
                dnext[q] = (slot + 1) % self.n_dma_sems
                sem = dsems[q][slot]
                if dcnt[q][slot] > 0:
                    wait(o.eng, sem, ("d", q, slot), dcnt[q][slot])
                dcnt[q][slot] += 16
                o.dsem = sem
                o.dslot = slot
                o.dval = dcnt[q][slot]
                o.fn(e).then_inc(sem, 16)
                all_dma.append(o)
            else:
                ins = o.fn(e)
                if o.needed:
                    sigcnt[o.eng] += 1
                    o.sig = sigcnt[o.eng]
                    ins.then_inc(sems[o.eng], 1)
        for q in queues:
            for slot in range(self.n_dma_sems):
                if dcnt[q][slot] > 0:
                    wait("sp", dsems[q][slot], ("d", q, slot), dcnt[q][slot])


def _run(nc, in_maps):
    res = run_bass_kernel_spmd(nc, in_maps, core_ids=list(range(NCORES)))
    return res.results


MODC = 6 * D // NCORES


def build_mods():
    nc = bass.Bass("TRN2", target_bir_lowering=False)
    k = KB(nc)
    cinT = nc.dram_tensor("cinT", [D, 3], F32, kind="ExternalInput").ap()
    aw = nc.dram_tensor("aw", [DEPTH, D, MODC], F32, kind="ExternalInput").ap()
    ab = nc.dram_tensor("ab", [DEPTH, 1, MODC], F32, kind="ExternalInput").ap()
    out = nc.dram_tensor("mod", [DEPTH, 3, MODC], F32, kind="ExternalOutput").ap()
    cs = k.sb([128, 16, 3], F32, "cs")
    ones = k.sb([1, 4], F32, "ones")
    wt = [k.sb([128, 16, 512], F32, f"wt{i}") for i in range(2)]
    bt = k.sb([1, DEPTH, MODC], F32, "bt")
    ot = [k.sb([3, 512], F32, f"ot{i}") for i in range(2)]
    pst = [k.ps() for _ in range(2)]
    k.dma(cs[:], cinT.rearrange("(k p) r -> p k r", p=128), w=[cs])
    k.dma(bt[:], ab.rearrange("l o c -> o l c"), w=[bt])
    k.op("dve", lambda e: e.memset(ones[:], 1.0), w=[ones])
    k.op("act", lambda e: e.activation(out=cs[:], in_=cs[:], func=AF.Silu), r=[cs], w=[cs])
    it = 0
    for li in range(DEPTH):
        for cc in range(MODC // 512):
            w_ = wt[it % 2]
            p_ = pst[it % 2]
            o_ = ot[it % 2]
            it += 1
            k.dma(w_[:], aw[li, :, cc * 512:(cc + 1) * 512].rearrange("(k p) c -> p k c", p=128), w=[w_])
            for kk in range(16):
                k.op("pe", lambda e, w_=w_, p_=p_, kk=kk: e.matmul(p_[0:3, :], lhsT=cs[:, kk, :], rhs=w_[:, kk, :],
                                                                   start=(kk == 0), stop=False), r=[cs, w_], w=[p_])
            k.op("pe", lambda e, p_=p_, li=li, cc=cc: e.matmul(p_[0:3, :], lhsT=ones[0:1, 0:3],
                                                              rhs=bt[0:1, li, cc * 512:(cc + 1) * 512],
                                                              start=False, stop=True), r=[ones, bt], w=[p_])
            k.op("dve", lambda e, p_=p_, o_=o_: e.tensor_copy(out=o_[:], in_=p_[0:3, :]), r=[p_], w=[o_])
            k.dma(out[li, :, cc * 512:(cc + 1) * 512], o_[:], r=[o_], q="pool")
    k.emit()
    return nc


def run_mods(c, c_ctx, ada_w, ada_b):
    cinT = np.ascontiguousarray(np.concatenate([c, c_ctx[None]], 0).T)
    nc = build_mods()
    maps = []
    for i in range(NCORES):
        sl = slice(i * MODC, (i + 1) * MODC)
        maps.append({"cinT": cinT, "aw": np.ascontiguousarray(ada_w[:, :, sl]),
                     "ab": np.ascontiguousarray(ada_b[:, None, sl])})
    res = _run(nc, maps)
    return np.concatenate([r["mod"] for r in res], axis=2)


IN_SIZES = (768, 768, 1024, 24, 384, 384, 768, 768, 32, 2304, 4 * D)
IN_OFF = np.concatenate([[0], np.cumsum(IN_SIZES)]).astype(int)
(O_S5, O_Z, O_XBC, O_DT, O_Q, O_K, O_V, O_G, O_R, O_HY, O_GT) = [int(v) for v in IN_OFF[:11]]


def core_cols(j):
    cols = []
    seg = {}

    def add(name, lst):
        seg[name] = (len(cols), len(lst))
        cols.extend(lst)

    def pad():
        while len(cols) % 128:
            cols.append(0)
    add("s5u", list(range(O_S5 + 192 * j, O_S5 + 192 * (j + 1))))
    heads = [j, j + 4 if j < 2 else j]
    for hi, h in enumerate(heads):
        add(f"q{hi}", list(range(O_Q + 64 * h, O_Q + 64 * (h + 1))))
        add(f"k{hi}", list(range(O_K + 64 * h, O_K + 64 * (h + 1))))
        add(f"v{hi}", list(range(O_V + 128 * h, O_V + 128 * (h + 1))))
        add(f"g{hi}", list(range(O_G + 128 * h, O_G + 128 * (h + 1))))
    add("r", list(range(O_R, O_R + 32)))
    for i, nm in enumerate(("hv", "hx1", "hx2")):
        add(nm, list(range(O_HY + 768 * i + 192 * j, O_HY + 768 * i + 192 * (j + 1))))
    pad()
    seg["ssd_start"] = (len(cols), 0)
    add("z", list(range(O_Z + 192 * j, O_Z + 192 * (j + 1))))
    add("x", list(range(O_XBC + 192 * j, O_XBC + 192 * (j + 1))))
    g = j // 2
    add("Bm", list(range(O_XBC + 768 + 64 * g, O_XBC + 768 + 64 * (g + 1))))
    add("Cm", list(range(O_XBC + 768 + 128 + 64 * g, O_XBC + 768 + 128 + 64 * (g + 1))))
    add("dt", [O_DT + 3 * j + i for i in range(3)] + [O_DT + 12 + 3 * j + i for i in range(3)])
    pad()
    seg["mix_end"] = (len(cols), 0)
    add("gate", list(range(O_GT + D * j, O_GT + D * (j + 1))))
    return cols, seg


NMIX = core_cols(0)[1]["mix_end"][0]
NA = NMIX + D
SEG = core_cols(0)[1]
NSSD0 = SEG["ssd_start"][0]

TCH = 256
NTCH = T // TCH


def emit_inproj(k, nc, xT, xTc, modv, wA, pT, gates):
    ones = k.sb([128, 128], F32, "ones")
    k.op("dve", lambda e: e.memset(ones[:], 1.0), w=[ones])
    mv = k.sb([128, 16, 4], F32, "mv")
    k.dma(mv[:], modv.rearrange("(k p) r -> p k r", p=128), w=[mv])
    k.op("dve", lambda e: e.tensor_scalar(out=mv[:, :, 1], in0=mv[:, :, 1], scalar1=1.0, scalar2=None, op0=ALU.add),
         r=[mv], w=[mv])
    k.op("dve", lambda e: e.tensor_scalar(out=mv[:, :, 3], in0=mv[:, :, 3], scalar1=1.0, scalar2=None, op0=ALU.add),
         r=[mv], w=[mv])
    epsb = k.sb([128, 1], F32, "epsb")
    k.op("dve", lambda e: e.memset(epsb[:], EPS), w=[epsb])
    xt = [k.sb([128, 16, TCH], F32, f"xt{i}") for i in range(2)]
    sq = [k.sb([128, TCH], F32, f"sq{i}") for i in range(2)]
    rstd = k.sb([128, TCH], F32, "rstd")
    tmp = [k.sb([128, TCH], F32, f"tmp{i}") for i in range(2)]
    NP0 = 9 * TCH
    hT = k.sb([128, 16, NP0], BF16, "hT")
    wf = [k.sb([128, 16, 128], F32, f"wf{i}") for i in range(2)]
    wb = [k.sb([128, 16, 128], BF16, f"wb{i}") for i in range(2)]
    ost = [k.sb([128, 512], F32, f"ost{i}") for i in range(3)]
    ostb = [k.sb([128, 512], BF16, f"ostb{i}") for i in range(3)]
    ps_s = k.ps()
    ps_o = [k.ps() for _ in range(3)]
    it_o = 0
    ssd_ch = list(range(NSSD0 // 128, NMIX // 128))
    for part in (0, 1):
        colchunks = list(range(NA // 128))
        ch0, nch = (0, 9) if part == 0 else (9, 8)
        for ci in range(nch):
            ch = ch0 + ci
            x_ = xt[ch % 2]
            k.dma(x_[:], xT[:, ch * TCH:(ch + 1) * TCH].rearrange("(k p) t -> p k t", p=128), w=[x_])
            for kk in range(16):
                s_ = sq[kk % 2]
                k.op("act", lambda e, s_=s_, x_=x_, kk=kk: e.activation(out=s_[:], in_=x_[:, kk, :], func=AF.Square),
                     r=[x_], w=[s_])
                k.op("pe", lambda e, s_=s_, kk=kk: e.matmul(ps_s[:, 0:TCH], lhsT=ones[:], rhs=s_[:],
                                                           start=(kk == 0), stop=(kk == 15)), r=[ones, s_], w=[ps_s])
            k.op("act", lambda e: e.activation(out=rstd[:], in_=ps_s[:, 0:TCH], func=AF.Sqrt, bias=epsb[:], scale=1.0 / D),
                 r=[ps_s, epsb], w=[rstd])
            k.op("dve", lambda e: e.reciprocal(out=rstd[:], in_=rstd[:]), r=[rstd], w=[rstd])
            mo = 2 if ch == 0 else 0
            for kk in range(16):
                t_ = tmp[kk % 2]
                k.op("dve", lambda e, t_=t_, x_=x_, kk=kk: e.tensor_tensor(out=t_[:], in0=x_[:, kk, :], in1=rstd[:], op=ALU.mult),
                     r=[x_, rstd], w=[t_])
                k.op("dve", lambda e, t_=t_, kk=kk, ci=ci, mo=mo: e.tensor_scalar(
                    out=hT[:, kk, ci * TCH:(ci + 1) * TCH], in0=t_[:], scalar1=mv[:, kk, mo + 1:mo + 2],
                    scalar2=mv[:, kk, mo:mo + 1], op0=ALU.mult, op1=ALU.add), r=[t_, mv], w=[hT])
        ntok = nch * TCH
        tok0 = ch0 * TCH
        for cc in colchunks:
            wf_ = wf[cc % 2]
            wb_ = wb[cc % 2]
            k.dma(wf_[:], wA[:, cc * 128:(cc + 1) * 128].rearrange("(k p) c -> p k c", p=128), w=[wf_])
            k.op("pool", lambda e, wf_=wf_, wb_=wb_: e.tensor_copy(out=wb_[:], in_=wf_[:]), r=[wf_], w=[wb_])
            for t0 in range(0, ntok, 512):
                tn = min(512, ntok - t0)
                p_ = ps_o[it_o % 3]
                o_ = ost[it_o % 3]
                it_o += 1
                for kk in range(16):
                    k.op("pe", lambda e, p_=p_, wb_=wb_, kk=kk, t0=t0, tn=tn: e.matmul(
                        p_[:, 0:tn], lhsT=wb_[:, kk, :], rhs=hT[:, kk, t0:t0 + tn], start=(kk == 0), stop=(kk == 15)),
                        r=[wb_, hT], w=[p_])
                if cc * 128 < NMIX:
                    k.op("act", lambda e, p_=p_, o_=o_, tn=tn: e.activation(out=o_[:, 0:tn], in_=p_[:, 0:tn], func=AF.Copy),
                         r=[p_], w=[o_])
                    k.dma(pT[cc * 128:(cc + 1) * 128, tok0 + t0:tok0 + t0 + tn], o_[:, 0:tn], r=[o_], w=[pT.tensor], q="pool")
                else:
                    ob_ = ostb[it_o % 3]
                    k.op("act", lambda e, p_=p_, ob_=ob_, tn=tn: e.activation(out=ob_[:, 0:tn], in_=p_[:, 0:tn], func=AF.Sigmoid),
                         r=[p_], w=[ob_])
                    g0 = cc * 128 - NMIX
                    k.dma(gates[g0:g0 + 128, tok0 + t0:tok0 + t0 + tn], ob_[:, 0:tn], r=[ob_], q="pool")


def build_am(parts=("A",)):
    nc = bass.Bass("TRN2", target_bir_lowering=False)
    k = KB(nc)
    xT = nc.dram_tensor("xT", [D, T], F32, kind="ExternalInput").ap()
    modv = nc.dram_tensor("modv", [D, 4], F32, kind="ExternalInput").ap()
    wA = nc.dram_tensor("wA", [D, NA], F32, kind="ExternalInput").ap()
    pT = nc.dram_tensor("pT", [NMIX, T], F32, kind="ExternalOutput" if "dumpP" in parts else "Internal").ap()
    gates = nc.dram_tensor("gates", [D, T], BF16, kind="ExternalOutput").ap()
    if "A" in parts:
        emit_inproj(k, nc, xT, xTc, modv, wA, pT, gates)
    k.emit()
    return nc


TWO_PI = 2.0 * math.pi


def chunks(n, c=512):
    return [(t0, min(c, n - t0)) for t0 in range(0, n, c)]


def emit_sin_turns(k, out, x, shape, name):
    xi = k.sb(shape, I32, name + "_i")
    xf = k.sb(shape, F32, name + "_f")
    k.op("dve", lambda e: e.tensor_copy(out=xi[:], in_=x[:]), r=[x], w=[xi])
    k.op("dve", lambda e: e.tensor_copy(out=xf[:], in_=xi[:]), r=[xi], w=[xf])
    k.op("dve", lambda e: e.tensor_tensor(out=xf[:], in0=x[:], in1=xf[:], op=ALU.subtract), r=[x, xf], w=[xf])
    k.op("dve", lambda e: e.tensor_scalar(out=xf[:], in0=xf[:], scalar1=0.4999999, scalar2=-0.4999999, op0=ALU.min, op1=ALU.max),
         r=[xf], w=[xf])
    k.op("act", lambda e: e.activation(out=out[:], in_=xf[:], func=AF.Sin, scale=TWO_PI), r=[xf], w=[out])


def emit_s5_abar(k, lamre, lamim, ls, shape, name):
    step = k.sb(shape, F32, name + "_step")
    rho = k.sb(shape, F32, name + "_rho")
    th = k.sb(shape, F32, name + "_th")
    th2 = k.sb(shape, F32, name + "_th2")
    sn = k.sb(shape, F32, name + "_sn")
    cs = k.sb(shape, F32, name + "_cs")
    k.op("act", lambda e: e.activation(out=step[:], in_=ls[:], func=AF.Exp), r=[ls], w=[step])
    k.op("dve", lambda e: e.tensor_tensor(out=rho[:], in0=lamre[:], in1=step[:], op=ALU.mult), r=[lamre, step], w=[rho])
    k.op("act", lambda e: e.activation(out=rho[:], in_=rho[:], func=AF.Exp), r=[rho], w=[rho])
    k.op("dve", lambda e: e.tensor_tensor(out=th[:], in0=lamim[:], in1=step[:], op=ALU.mult), r=[lamim, step], w=[th])
    k.op("dve", lambda e: e.tensor_scalar(out=th[:], in0=th[:], scalar1=1.0 / TWO_PI, scalar2=None, op0=ALU.mult), r=[th], w=[th])
    k.op("dve", lambda e: e.tensor_scalar(out=th2[:], in0=th[:], scalar1=0.25, scalar2=None, op0=ALU.add), r=[th], w=[th2])
    emit_sin_turns(k, sn, th, shape, name + "_s")
    emit_sin_turns(k, cs, th2, shape, name + "_c")
    k.op("dve", lambda e: e.tensor_tensor(out=sn[:], in0=sn[:], in1=rho[:], op=ALU.mult), r=[sn, rho], w=[sn])
    k.op("dve", lambda e: e.tensor_tensor(out=cs[:], in0=cs[:], in1=rho[:], op=ALU.mult), r=[cs, rho], w=[cs])
    return cs, sn, step


NLEV = 13


def emit_s5(k, nc, pT, ya, cst, prm):
    ident, swp = cst["ident"], cst["swap"]
    AR = k.sb([128, NLEV, 24], F32, "AR")
    AI = k.sb([128, NLEV, 24], F32, "AI")
    bbT = k.sb([16, 24, 128], F32, "bbT")
    cT = k.sb([128, 24, 16], F32, "cT")
    dsk = k.sb([16, 12], F32, "dsk")
    mk_tmp = k.mark()
    l128 = k.sb([128, 2, 24], F32, "l128")
    ls128 = k.sb([128, 24], F32, "ls128")
    sg128 = k.sb([128, 1], F32, "sg128")
    k.dma(l128[:], prm["s5_lam128"], w=[l128])
    k.dma(ls128[:], prm["s5_ls128"], w=[ls128])
    k.dma(sg128[:], prm["sign128"], w=[sg128])
    lre = k.sb([128, 24], F32, "lre")
    lim = k.sb([128, 24], F32, "lim")
    k.op("dve", lambda e: e.tensor_copy(out=lre[:], in_=l128[:, 0, :]), r=[l128], w=[lre])
    k.op("dve", lambda e: e.tensor_copy(out=lim[:], in_=l128[:, 1, :]), r=[l128], w=[lim])
    ar0, ai0, _ = emit_s5_abar(k, lre, lim, ls128, [128, 24], "c128")
    k.op("dve", lambda e: e.tensor_copy(out=AR[:, 0, :], in_=ar0[:]), r=[ar0], w=[AR])
    k.op("dve", lambda e: e.tensor_scalar(out=AI[:, 0, :], in0=ai0[:], scalar1=sg128[:, 0:1], scalar2=None, op0=ALU.mult),
         r=[ai0, sg128], w=[AI])
    t1 = k.sb([128, 24], F32, "sqt1")
    t2 = k.sb([128, 24], F32, "sqt2")
    for lv in range(1, NLEV):
        k.op("dve", lambda e, lv=lv: e.tensor_tensor(out=t1[:], in0=AR[:, lv - 1, :], in1=AR[:, lv - 1, :], op=ALU.mult), r=[AR], w=[t1])
        k.op("dve", lambda e, lv=lv: e.tensor_tensor(out=t2[:], in0=AI[:, lv - 1, :], in1=AI[:, lv - 1, :], op=ALU.mult), r=[AI], w=[t2])
        k.op("dve", lambda e, lv=lv: e.tensor_tensor(out=AR[:, lv, :], in0=t1[:], in1=t2[:], op=ALU.subtract), r=[t1, t2], w=[AR])
        k.op("dve", lambda e, lv=lv: e.tensor_tensor(out=t1[:], in0=AR[:, lv - 1, :], in1=AI[:, lv - 1, :], op=ALU.mult), r=[AR, AI], w=[t1])
        k.op("dve", lambda e, lv=lv: e.tensor_scalar(out=AI[:, lv, :], in0=t1[:], scalar1=2.0, scalar2=None, op0=ALU.mult), r=[t1], w=[AI])
    l16 = k.sb([16, 2, 24 * 64], F32, "l16")
    ls16 = k.sb([16, 24 * 64], F32, "ls16")
    b16 = k.sb([16, 2, 24 * 64], F32, "b16")
    k.dma(l16[:], prm["s5_lam16"], w=[l16])
    k.dma(ls16[:], prm["s5_ls16"], w=[ls16])
    k.dma(b16[:], prm["s5_b16"], w=[b16])
    SH = [16, 24 * 64]
    lre16 = k.sb(SH, F32, "lre16")
    lim16 = k.sb(SH, F32, "lim16")
    k.op("dve", lambda e: e.tensor_copy(out=lre16[:], in_=l16[:, 0, :]), r=[l16], w=[lre16])
    k.op("dve", lambda e: e.tensor_copy(out=lim16[:], in_=l16[:, 1, :]), r=[l16], w=[lim16])
    ar, ai, _ = emit_s5_abar(k, lre16, lim16, ls16, SH, "c16")
    den = k.sb(SH, F32, "den")
    q1 = k.sb(SH, F32, "q1")
    zr = k.sb(SH, F32, "zr")
    zi = k.sb(SH, F32, "zi")
    k.op("dve", lambda e: e.tensor_tensor(out=den[:], in0=lre16[:], in1=lre16[:], op=ALU.mult), r=[lre16], w=[den])
    k.op("dve", lambda e: e.tensor_tensor(out=q1[:], in0=lim16[:], in1=lim16[:], op=ALU.mult), r=[lim16], w=[q1])
    k.op("dve", lambda e: e.tensor_tensor(out=den[:], in0=den[:], in1=q1[:], op=ALU.add), r=[den, q1], w=[den])
    k.op("dve", lambda e: e.reciprocal(out=den[:], in_=den[:]), r=[den], w=[den])
    k.op("dve", lambda e: e.tensor_scalar(out=ar[:], in0=ar[:], scalar1=-1.0, scalar2=None, op0=ALU.add), r=[ar], w=[ar])
    k.op("dve", lambda e: e.tensor_tensor(out=zr[:], in0=ar[:], in1=lre16[:], op=ALU.mult), r=[ar, lre16], w=[zr])
    k.op("dve", lambda e: e.tensor_tensor(out=q1[:], in0=ai[:], in1=lim16[:], op=ALU.mult), r=[ai, lim16], w=[q1])
    k.op("dve", lambda e: e.tensor_tensor(out=zr[:], in0=zr[:], in1=q1[:], op=ALU.add), r=[zr, q1], w=[zr])
    k.op("dve", lambda e: e.tensor_tensor(out=zr[:], in0=zr[:], in1=den[:], op=ALU.mult), r=[zr, den], w=[zr])
    k.op("dve", lambda e: e.tensor_tensor(out=zi[:], in0=ai[:], in1=lre16[:], op=ALU.mult), r=[ai, lre16], w=[zi])
    k.op("dve", lambda e: e.tensor_tensor(out=q1[:], in0=ar[:], in1=lim16[:], op=ALU.mult), r=[ar, lim16], w=[q1])
    k.op("dve", lambda e: e.tensor_tensor(out=zi[:], in0=zi[:], in1=q1[:], op=ALU.subtract), r=[zi, q1], w=[zi])
    k.op("dve", lambda e: e.tensor_tensor(out=zi[:], in0=zi[:], in1=den[:], op=ALU.mult), r=[zi, den], w=[zi])
    bre = b16[:, 0, :].rearrange("h (g p) -> h g p", p=64)
    bim = b16[:, 1, :].rearrange("h (g p) -> h g p", p=64)
    zr3 = zr[:].rearrange("h (g p) -> h g p", p=64)
    zi3 = zi[:].rearrange("h (g p) -> h g p", p=64)
    q3 = k.sb([16, 24, 64], F32, "q3")
    k.op("dve", lambda e: e.tensor_tensor(out=bbT[:, :, 0:64], in0=zr3, in1=bre, op=ALU.mult), r=[zr, b16], w=[bbT])
    k.op("dve", lambda e: e.tensor_tensor(out=q3[:], in0=zi3, in1=bim, op=ALU.mult), r=[zi, b16], w=[q3])
    k.op("dve", lambda e: e.tensor_tensor(out=bbT[:, :, 0:64], in0=bbT[:, :, 0:64], in1=q3[:], op=ALU.subtract), r=[bbT, q3], w=[bbT])
    k.op("dve", lambda e: e.tensor_tensor(out=bbT[:, :, 64:128], in0=zr3, in1=bim, op=ALU.mult), r=[zr, b16], w=[bbT])
    k.op("dve", lambda e: e.tensor_tensor(out=q3[:], in0=zi3, in1=bre, op=ALU.mult), r=[zi, b16], w=[q3])
    k.op("dve", lambda e: e.tensor_tensor(out=bbT[:, :, 64:128], in0=bbT[:, :, 64:128], in1=q3[:], op=ALU.add), r=[bbT, q3], w=[bbT])
    k.dma(cT[:], prm["s5_c128"], w=[cT])
    k.op("dve", lambda e: e.tensor_scalar(out=cT[64:128], in0=cT[64:128], scalar1=-1.0, scalar2=None, op0=ALU.mult), r=[cT], w=[cT])
    k.dma(dsk[:], prm["s5_d"], w=[dsk])
    k.release(mk_tmp)

    u3 = [k.sb([16, T + NCTX], F32, f"u3_{i}") for i in range(2)]
    H = [[k.sb([128, T], F32, f"H{d}{i}") for i in range(2)] for d in range(2)]
    MT = [k.sb([128, 128], F32, f"MT{i}") for i in range(4)]
    yt = [k.sb([16, T], F32, f"yt{i}") for i in range(2)]
    psr = [k.ps() for _ in range(4)]
    psy = [k.ps() for _ in range(2)]
    nps = 0
    nmt = 0
    r0 = SEG["s5u"][0]
    for gl in range(12):
        u_ = u3[gl % 2]
        y_ = yt[gl % 2]
        k.dma(u_[:, 0:T], pT[r0 + 16 * gl:r0 + 16 * (gl + 1), :], r=[pT.tensor], w=[u_])
        k.dma(u_[:, T:T + NCTX], pT[r0 + 16 * gl:r0 + 16 * (gl + 1), 0:NCTX], r=[pT.tensor], w=[u_])
        cur = [0, 0]
        for d in range(2):
            dg = d * 12 + gl
            c0 = 0 if d == 0 else NCTX
            for (t0, n) in chunks(T):
                p_ = psr[nps % 4]
                nps += 1
                k.op("pe", lambda e, p_=p_, dg=dg, u_=u_, c0=c0, t0=t0, n=n: e.matmul(
                    p_[:, 0:n], lhsT=bbT[:, dg, :], rhs=u_[:, c0 + t0:c0 + t0 + n], start=True, stop=True), r=[bbT, u_], w=[p_])
                k.op("act", lambda e, p_=p_, d=d, t0=t0, n=n: e.activation(out=H[d][0][:, t0:t0 + n], in_=p_[:, 0:n], func=AF.Copy),
                     r=[p_], w=[H[d][0]])
        for lv in range(NLEV):
            sh = 1 << lv
            for d in range(2):
                dg = d * 12 + gl
                src = H[d][cur[d]]
                dst = H[d][1 - cur[d]]
                cur[d] = 1 - cur[d]
                m_ = MT[nmt % 4]
                nmt += 1
                k.op("dve", lambda e, m_=m_, lv=lv, dg=dg: e.tensor_scalar(out=m_[:], in0=ident[:], scalar1=AR[:, lv, dg:dg + 1],
                                                                         scalar2=None, op0=ALU.mult), r=[ident, AR], w=[m_])
                k.op("dve", lambda e, m_=m_, lv=lv, dg=dg: e.scalar_tensor_tensor(out=m_[:], in0=swp[:], scalar=AI[:, lv, dg:dg + 1],
                                                                                in1=m_[:], op0=ALU.mult, op1=ALU.add),
                     r=[swp, AI, m_], w=[m_])
                if d == 0:
                    k.op("pool", lambda e, src=src, dst=dst, sh=sh: e.tensor_copy(out=dst[:, 0:sh], in_=src[:, 0:sh]), r=[src], w=[dst])
                    lo = sh
                else:
                    k.op("pool", lambda e, src=src, dst=dst, sh=sh: e.tensor_copy(out=dst[:, T - sh:T], in_=src[:, T - sh:T]), r=[src], w=[dst])
                    lo = 0
                for (t0, n) in chunks(T - sh):
                    ta = lo + t0
                    tb = ta - sh if d == 0 else ta + sh
                    p_ = psr[nps % 4]
                    nps += 1
                    k.op("pe", lambda e, p_=p_, m_=m_, src=src, tb=tb, n=n: e.matmul(p_[:, 0:n], lhsT=m_[:], rhs=src[:, tb:tb + n],
                                                                                  start=True, stop=True), r=[m_, src], w=[p_])
                    k.op("dve", lambda e, p_=p_, src=src, dst=dst, ta=ta, n=n: e.tensor_tensor(
                        out=dst[:, ta:ta + n], in0=src[:, ta:ta + n], in1=p_[:, 0:n], op=ALU.add), r=[src, p_], w=[dst])
        Hf = H[0][cur[0]]
        Hb = H[1][cur[1]]
        for (t0, n) in [(0, NCTX)] + [(NCTX + a, b_) for (a, b_) in chunks(S)]:
            tb = (S + t0) if t0 < NCTX else (t0 - NCTX)
            p_ = psy[(t0 // 256) % 2]
            k.op("pe", lambda e, p_=p_, gl=gl, t0=t0, n=n: e.matmul(p_[0:16, 0:n], lhsT=cT[:, gl, :], rhs=Hf[:, t0:t0 + n],
                                                                   start=True, stop=False), r=[cT, Hf], w=[p_])
            k.op("pe", lambda e, p_=p_, gl=gl, tb=tb, n=n: e.matmul(p_[0:16, 0:n], lhsT=cT[:, 12 + gl, :], rhs=Hb[:, tb:tb + n],
                                                                   start=False, stop=True), r=[cT, Hb], w=[p_])
            k.op("dve", lambda e, p_=p_, u_=u_, y_=y_, gl=gl, t0=t0, n=n: e.scalar_tensor_tensor(
                out=y_[:, t0:t0 + n], in0=u_[:, t0:t0 + n], scalar=dsk[:, gl:gl + 1], in1=p_[0:16, 0:n], op0=ALU.mult, op1=ALU.add),
                r=[u_, dsk, p_], w=[y_])
        k.dma(ya[16 * gl:16 * (gl + 1), :], y_[:], r=[y_], q="pool")


def s5_host_params(inp, li, j):
    gs = slice(12 * j, 12 * (j + 1))
    lre = inp["s5_lambda_re"][li][:, gs]
    lim = inp["s5_lambda_im"][li][:, gs]
    ls = inp["s5_log_step"][li][:, gs]
    col = lambda a: np.ascontiguousarray(a.reshape(24, 64).T)
    lam128 = np.stack([np.concatenate([col(lre), col(lre)], 0), np.concatenate([col(lim), col(lim)], 0)], 1)
    ls128 = np.broadcast_to(ls.reshape(1, 24), (128, 24))
    lam16 = np.broadcast_to(np.stack([lre.reshape(24 * 64), lim.reshape(24 * 64)], 0)[None], (16, 2, 24 * 64))
    ls16 = np.broadcast_to(np.repeat(ls.reshape(24), 64)[None], (16, 24 * 64))
    bre = inp["s5_b_re"][li][:, gs]
    bim = inp["s5_b_im"][li][:, gs]
    b16 = np.stack([bre.reshape(24 * 64, 16).T, bim.reshape(24 * 64, 16).T], 1)
    cre = inp["s5_c_re"][li][:, gs]
    cim = inp["s5_c_im"][li][:, gs]
    c128 = np.concatenate([cre.reshape(24, 16, 64).transpose(2, 0, 1), cim.reshape(24, 16, 64).transpose(2, 0, 1)], 0)
    dd = inp["s5_d"][li][192 * j:192 * (j + 1)].reshape(12, 16).T
    f = lambda a: np.ascontiguousarray(a, dtype=np.float32)
    return {"s5_lam128": f(lam128), "s5_ls128": f(ls128), "s5_lam16": f(lam16), "s5_ls16": f(ls16), "s5_b16": f(b16),
            "s5_c128": f(c128), "s5_d": f(dd)}


def host_consts():
    ident = np.eye(128, dtype=np.float32)
    swap = np.zeros((128, 128), np.float32)
    for i in range(64):
        swap[i, i + 64] = 1.0
        swap[i + 64, i] = 1.0
    sign = np.ones((128, 1), np.float32)
    sign[64:] = -1.0
    return {"c_ident": ident, "c_swap": swap, "sign128": sign}


S5_SHAPES = {"s5_lam128": [128, 2, 24], "s5_ls128": [128, 24], "s5_lam16": [16, 2, 1536], "s5_ls16": [16, 1536],
             "s5_b16": [16, 2, 1536], "s5_c128": [128, 24, 16], "s5_d": [16, 12], "sign128": [128, 1]}


def build_am(parts=("A",)):
    nc = bass.Bass("TRN2", target_bir_lowering=False)
    k = KB(nc)
    xT = nc.dram_tensor("xT", [D, T], F32, kind="ExternalInput").ap()
    xTc = None
    modv = nc.dram_tensor("modv", [D, 4], F32, kind="ExternalInput").ap()
    wA = nc.dram_tensor("wA", [D, NA], F32, kind="ExternalInput").ap()
    pT = nc.dram_tensor("pT", [NMIX, T], F32, kind="ExternalOutput" if "dumpP" in parts else "Internal").ap()
    gates = nc.dram_tensor("gates", [D, T], BF16, kind="ExternalOutput").ap()
    prm = {}
    cst = {}
    ci = nc.dram_tensor("c_ident", [128, 128], F32, kind="ExternalInput").ap()
    cs_ = nc.dram_tensor("c_swap", [128, 128], F32, kind="ExternalInput").ap()
    cst["ident"] = k.sb([128, 128], F32, "ident")
    cst["swap"] = k.sb([128, 128], F32, "swap")
    k.dma(cst["ident"][:], ci, w=[cst["ident"]])
    k.dma(cst["swap"][:], cs_, w=[cst["swap"]])
    mk = k.mark()
    if "A" in parts:
        emit_inproj(k, nc, xT, xTc, modv, wA, pT, gates)
        k.release(mk)
    if "S5" in parts:
        for nm, shp in S5_SHAPES.items():
            prm[nm] = nc.dram_tensor(nm, shp, F32, kind="ExternalInput").ap()
        ya = nc.dram_tensor("ya", [192, T], F32, kind="ExternalOutput").ap()
        emit_s5(k, nc, pT, ya, cst, prm)
        k.release(mk)
    k.emit()
    return nc


SEGS_F = [(0, 256, False), (256, 1792, False), (1792, 3328, False), (3328, 4352, False)]
SEGS_B = [(0, 256, True), (3328, 4352, True), (1792, 3328, True), (256, 1792, True)]


def emit_outer_scan(k, cst, bufs, Xs, PX, Arep, Crep, dec_of_pt, d, yacc, first):
    selx, hm = cst["selx"], cst["hm"]
    U, Hs, carry, psx, psy = bufs
    npt = PX // 2
    it = 0
    for si, (a, b_, rev) in enumerate(SEGS_B if d == 1 else SEGS_F):
        L = b_ - a
        cks = chunks(L)
        for pt in range(npt):
            u_ = U[it % 2]
            h_ = Hs[it % 2]
            v_ = h_
            it += 1
            p0 = 0 if pt < 32 else 64
            for ci, (t0, n) in enumerate(cks):
                p_ = psx[(it + ci) % 2]
                k.op("pe", lambda e, p_=p_, pt=pt, p0=p0, t0=t0, n=n: e.matmul(
                    p_[:, 0:n], lhsT=selx[p0:p0 + 64, pt % 32, :], rhs=Xs[p0:p0 + 64, a + t0:a + t0 + n], start=True, stop=True),
                    r=[selx, Xs], w=[p_])
                k.op("dve", lambda e, p_=p_, u_=u_, t0=t0, n=n: e.tensor_tensor(
                    out=u_[:, t0:t0 + n], in0=p_[:, 0:n], in1=Arep[:, a + t0:a + t0 + n], op=ALU.mult), r=[p_, Arep], w=[u_])
            dec = dec_of_pt(pt)
            init = 0.0 if si == 0 else carry[:, pt:pt + 1]
            rd = [dec, u_] + ([] if si == 0 else [carry])
            if rev:
                k.op("dve", lambda e, h_=h_, u_=u_, dec=dec, init=init, L=L: e.tensor_tensor_scan(
                    out=h_[:, L - 1::-1] if False else h_[:, 0:L][:, ::-1], data0=dec[:, a:b_][:, ::-1], data1=u_[:, 0:L][:, ::-1],
                    initial=init, op0=ALU.mult, op1=ALU.add), r=rd, w=[h_])
                last = 0
            else:
                k.op("dve", lambda e, h_=h_, u_=u_, dec=dec, init=init, L=L: e.tensor_tensor_scan(
                    out=h_[:, 0:L], data0=dec[:, a:b_], data1=u_[:, 0:L], initial=init, op0=ALU.mult, op1=ALU.add), r=rd, w=[h_])
                last = L - 1
            if si < 3:
                k.op("act", lambda e, h_=h_, pt=pt, last=last: e.activation(out=carry[:, pt:pt + 1], in_=h_[:, last:last + 1], func=AF.Copy),
                     r=[h_], w=[carry])
            k.op("pool", lambda e, h_=h_, v_=v_, L=L: e.tensor_tensor(out=v_[:, 0:L], in0=h_[:, 0:L], in1=Crep[:, a:b_], op=ALU.mult),
                 r=[h_, Crep, carry], w=[v_])
            for ci, (t0, n) in enumerate(cks):
                k.op("pe", lambda e, ci=ci, pt=pt, v_=v_, t0=t0, n=n: e.matmul(
                    psy[ci][0:PX, 0:n], lhsT=hm[:, 128 - 2 * pt:128 - 2 * pt + PX], rhs=v_[:, t0:t0 + n], start=(pt == 0), stop=(pt == npt - 1)),
                    r=[hm, v_], w=[psy[ci]])
        for ci, (t0, n) in enumerate(cks):
            if first:
                k.op("act", lambda e, ci=ci, t0=t0, n=n: e.activation(out=yacc[0:PX, a + t0:a + t0 + n], in_=psy[ci][0:PX, 0:n], func=AF.Copy),
                     r=[psy[ci]], w=[yacc])
            else:
                k.op("dve", lambda e, ci=ci, t0=t0, n=n: e.tensor_tensor(out=yacc[0:PX, a + t0:a + t0 + n], in0=yacc[0:PX, a + t0:a + t0 + n],
                                                                        in1=psy[ci][0:PX, 0:n], op=ALU.add), r=[psy[ci], yacc], w=[yacc])


def emit_conv3(k, dst, src, w3, bias, P, silu, wt=None):
    for (a, b_) in ((0, NCTX), (NCTX, T)):
        k.op("dve", lambda e, a=a, b_=b_: e.tensor_scalar(out=dst[0:P, a:b_], in0=src[0:P, a:b_], scalar1=w3[0:P, 1:2], scalar2=bias[0:P, 0:1],
                                                         op0=ALU.mult, op1=ALU.add), r=[src, wt], w=[dst])
        k.op("dve", lambda e, a=a, b_=b_: e.scalar_tensor_tensor(out=dst[0:P, a + 1:b_], in0=src[0:P, a:b_ - 1], scalar=w3[0:P, 0:1],
                                                                in1=dst[0:P, a + 1:b_], op0=ALU.mult, op1=ALU.add), r=[src, wt, dst], w=[dst])
        k.op("dve", lambda e, a=a, b_=b_: e.scalar_tensor_tensor(out=dst[0:P, a:b_ - 1], in0=src[0:P, a + 1:b_], scalar=w3[0:P, 2:3],
                                                                in1=dst[0:P, a:b_ - 1], op0=ALU.mult, op1=ALU.add), r=[src, wt, dst], w=[dst])
    if silu:
        k.op("act", lambda e: e.activation(out=dst[0:P, :], in_=dst[0:P, :], func=AF.Silu), r=[dst], w=[dst])


def scan_bufs(k):
    U = [k.sb([128, 1536], F32, f"U{i}") for i in range(2)]
    Hs = [k.sb([128, 1536], F32, f"Hs{i}") for i in range(2)]
    carry = k.sb([128, 64], F32, "carry")
    psx = [k.ps() for _ in range(2)]
    psy = [k.ps() for _ in range(3)]
    return (U, Hs, carry, psx, psy)


def emit_ssd(k, nc, pT, yb, cst, prm):
    sp = k.sb([128, 2, 8], F32, "ssdp_x")
    spB = k.sb([128, 8], F32, "ssdp_B")
    spC = k.sb([128, 8], F32, "ssdp_C")
    sdt = k.sb([6, 4], F32, "ssdp_dt")
    one6 = k.sb([6, 1], F32, "one6")
    selr = k.sb([6, 2, 2, 128], F32, "selr")
    selh = k.sb([38, 6, 128], F32, "selh")
    k.dma(sp[:], prm["ssd_px"], w=[sp])
    k.dma(spB[:], prm["ssd_pB"], w=[spB])
    k.dma(spC[:], prm["ssd_pC"], w=[spC])
    k.dma(sdt[:], prm["ssd_pdt"], w=[sdt])
    k.dma(selr[:], prm["selr"], w=[selr])
    k.dma(selh[32:38], prm["selh"], w=[selh])
    k.op("dve", lambda e: e.memset(one6[:], 1.0), w=[one6])
    rz, rx, rB, rC, rdt = SEG["z"][0], SEG["x"][0], SEG["Bm"][0], SEG["Cm"][0], SEG["dt"][0]
    raw = k.sb([128, T], F32, "raw")
    xa = k.sb([128, T], F32, "xa")
    Brep = k.sb([128, T], F32, "Brep")
    Crep = k.sb([128, T], F32, "Crep")
    ya = k.sb([128, T], F32, "ssd_ya")
    decA = [k.sb([128, T], F32, f"decA{i}") for i in range(2)]
    dd = k.sb([38, T], F32, "dtdec")
    for c_ in range((NMIX - NSSD0) // 128):
        r0 = NSSD0 + 128 * c_
        k.dma(raw[:], pT[r0:r0 + 128, :], r=[pT.tensor], w=[raw])
        k.op("dve", lambda e: e.tensor_copy(out=xa[:, 0:NCTX], in_=raw[:, 0:NCTX]), r=[raw], w=[xa])
        k.op("dve", lambda e: e.tensor_copy(out=xa[:, NCTX:T].rearrange("p (w r) -> p w r", w=64),
                                            in_=raw[:, NCTX:T].rearrange("p (r w) -> p w r", w=64)), r=[raw], w=[xa])
        k.dma(pT[r0:r0 + 128, :], xa[:], r=[xa], w=[pT.tensor], q="pool")
    k.dma(raw[0:64, :], pT[rB:rB + 64, :], r=[pT.tensor], w=[raw])
    k.dma(raw[64:128, :], pT[rB:rB + 64, :], r=[pT.tensor], w=[raw])
    emit_conv3(k, Brep, raw, spB, spB[:, 3:4], 128, True, wt=spB)
    k.dma(raw[0:64, :], pT[rC:rC + 64, :], r=[pT.tensor], w=[raw])
    k.dma(raw[64:128, :], pT[rC:rC + 64, :], r=[pT.tensor], w=[raw])
    emit_conv3(k, Crep, raw, spC, spC[:, 3:4], 128, True, wt=spC)
    k.dma(dd[0:6, :], pT[rdt:rdt + 6, :], r=[pT.tensor], w=[dd])
    k.op("act", lambda e: e.activation(out=dd[0:6, :], in_=dd[0:6, :], func=AF.Exp, bias=sdt[:, 0:1], scale=1.0), r=[dd, sdt], w=[dd])
    k.op("act", lambda e: e.activation(out=dd[0:6, :], in_=dd[0:6, :], func=AF.Ln, bias=one6[:], scale=1.0), r=[dd, one6], w=[dd])
    k.op("act", lambda e: e.activation(out=sdt[:, 2:3], in_=sdt[:, 1:2], func=AF.Exp), r=[sdt], w=[sdt])
    k.op("dve", lambda e: e.tensor_scalar(out=sdt[:, 2:3], in0=sdt[:, 2:3], scalar1=-1.0, scalar2=None, op0=ALU.mult), r=[sdt], w=[sdt])
    k.op("act", lambda e: e.activation(out=raw[0:6, :], in_=dd[0:6, :], func=AF.Exp, scale=sdt[:, 2:3]), r=[dd, sdt], w=[raw])
    k.dma(dd[32:38, :], raw[0:6, :], r=[raw], w=[dd])
    bufs = scan_bufs(k)
    psx = bufs[3]
    Xs = raw
    for ti, PX in ((0, 128), (1, 64)):
        k.dma(raw[0:PX, :], pT[rx + 128 * ti:rx + 128 * ti + PX, :], r=[pT.tensor], w=[raw])
        emit_conv3(k, xa, raw, sp[:, ti, :], sp[:, ti, 3:4], PX, True, wt=sp)
        for d in range(2):
            for ci, (t0, n) in enumerate(chunks(T)):
                p_ = psx[ci % 2]
                k.op("pe", lambda e, p_=p_, d=d, ti=ti, t0=t0, n=n: e.matmul(p_[:, 0:n], lhsT=selr[:, d, ti, :], rhs=dd[0:6, t0:t0 + n],
                                                                           start=True, stop=True), r=[selr, dd], w=[p_])
                k.op("dve", lambda e, p_=p_, PX=PX, t0=t0, n=n: e.tensor_tensor(out=Xs[0:PX, t0:t0 + n], in0=xa[0:PX, t0:t0 + n],
                                                                              in1=p_[0:PX, 0:n], op=ALU.mult), r=[p_, xa], w=[Xs])
            for hh in range(2 if ti == 0 else 1):
                rr = 3 * d + 2 * ti + hh
                for ci, (t0, n) in enumerate(chunks(T)):
                    p_ = psx[ci % 2]
                    k.op("pe", lambda e, p_=p_, rr=rr, t0=t0, n=n: e.matmul(p_[:, 0:n], lhsT=selh[32:38, rr, :], rhs=dd[32:38, t0:t0 + n],
                                                                          start=True, stop=True), r=[selh, dd], w=[p_])
                    k.op("act", lambda e, p_=p_, hh=hh, t0=t0, n=n: e.activation(out=decA[hh][:, t0:t0 + n], in_=p_[:, 0:n], func=AF.Copy),
                         r=[p_], w=[decA[hh]])
            emit_outer_scan(k, cst, bufs, Xs, PX, Brep, Crep, (lambda pt: decA[0] if pt < 32 else decA[1]), d, ya, first=(d == 0))
        P = PX
        r_ = rz + 128 * ti
        k.op("dve", lambda e, P=P, ti=ti: e.scalar_tensor_tensor(out=ya[0:P, :], in0=xa[0:P, :], scalar=sp[0:P, ti, 4:5],
                                                                in1=ya[0:P, :], op0=ALU.mult, op1=ALU.add), r=[xa, sp, ya], w=[ya])
        k.dma(raw[0:P, :], pT[r_:r_ + P, :], r=[pT.tensor], w=[raw])
        k.op("act", lambda e, P=P: e.activation(out=raw[0:P, :], in_=raw[0:P, :], func=AF.Silu), r=[raw], w=[raw])
        k.op("dve", lambda e, P=P: e.tensor_tensor(out=ya[0:P, :], in0=ya[0:P, :], in1=raw[0:P, :], op=ALU.mult), r=[ya, raw], w=[ya])
        k.dma(yb[128 * ti:128 * ti + P, :], ya[0:P, :], r=[ya], q="pool")


def emit_gla(k, nc, pT, yc, cst, prm):
    gp = k.sb([128, 2, 2, 2], F32, "glap")
    gw = k.sb([16, 2, 2, 128], F32, "glaw")
    nw = k.sb([128, 1], F32, "glanw")
    one = k.sb([128, 1], F32, "gone")
    epsb = k.sb([128, 1], F32, "geps")
    ones = k.sb([128, 128], F32, "gones")
    k.dma(gp[:], prm["gla_p"], w=[gp])
    k.dma(gw[:], prm["gla_w"], w=[gw])
    k.dma(nw[:], prm["gla_nw"], w=[nw])
    k.op("dve", lambda e: e.memset(one[:], 1.0), w=[one])
    k.op("dve", lambda e: e.memset(epsb[:], EPS), w=[epsb])
    k.op("dve", lambda e: e.memset(ones[:], 1.0), w=[ones])
    k.op("dve", lambda e: e.tensor_scalar(out=gp[:], in0=gp[:], scalar1=-1.0, scalar2=None, op0=ALU.mult), r=[gp], w=[gp])
    rT = [k.sb([16, T], F32, f"rT{d}") for d in range(2)]
    rr = SEG["r"][0]
    for d in range(2):
        k.dma(rT[d][:], pT[rr + 16 * d:rr + 16 * (d + 1), :], r=[pT.tensor], w=[rT[d]])
    Krep = k.sb([128, T], F32, "Krep")
    Qrep = k.sb([128, T], F32, "Qrep")
    dec = [k.sb([128, T], F32, f"gdec{d}") for d in range(2)]
    vt = k.sb([128, T], F32, "gv")
    yo = k.sb([128, T], F32, "gyo")
    bufs = scan_bufs(k)
    psx = bufs[3]
    for hs in range(2):
        rq, rk, rv, rg = SEG[f"q{hs}"][0], SEG[f"k{hs}"][0], SEG[f"v{hs}"][0], SEG[f"g{hs}"][0]
        for half in range(2):
            k.dma(Krep[64 * half:64 * half + 64, :], pT[rk:rk + 64, :], r=[pT.tensor], w=[Krep])
            k.dma(Qrep[64 * half:64 * half + 64, :], pT[rq:rq + 64, :], r=[pT.tensor], w=[Qrep])
        k.op("dve", lambda e: e.tensor_scalar(out=Qrep[:], in0=Qrep[:], scalar1=0.125, scalar2=None, op0=ALU.mult), r=[Qrep], w=[Qrep])
        k.dma(vt[:], pT[rv:rv + 128, :], r=[pT.tensor], w=[vt])
        for d in range(2):
            for ci, (t0, n) in enumerate(chunks(T)):
                p_ = psx[ci % 2]
                k.op("pe", lambda e, p_=p_, hs=hs, d=d, t0=t0, n=n: e.matmul(p_[:, 0:n], lhsT=gw[:, hs, d, :], rhs=rT[d][:, t0:t0 + n],
                                                                           start=True, stop=True), r=[gw, rT[d]], w=[p_])
                k.op("act", lambda e, p_=p_, hs=hs, d=d, t0=t0, n=n: e.activation(out=dec[d][:, t0:t0 + n], in_=p_[:, 0:n], func=AF.Exp,
                                                                                bias=gp[:, hs, d, 0:1], scale=-1.0), r=[p_, gp], w=[dec[d]])
            k.op("act", lambda e, d=d: e.activation(out=dec[d][:], in_=dec[d][:], func=AF.Ln, bias=one[:], scale=1.0), r=[dec[d], one], w=[dec[d]])
            k.op("act", lambda e, d=d: e.activation(out=dec[d][:], in_=dec[d][:], func=AF.Exp, scale=-1.0 / 16.0), r=[dec[d]], w=[dec[d]])
            emit_outer_scan(k, cst, bufs, vt, 128, Krep, Qrep, (lambda pt, d=d: dec[d]), d, yo, first=(d == 0))
        k.dma(vt[:], pT[rg:rg + 128, :], r=[pT.tensor], w=[vt])
        k.op("act", lambda e: e.activation(out=vt[:], in_=vt[:], func=AF.Silu), r=[vt], w=[vt])
        for ci, (t0, n) in enumerate(chunks(T)):
            p_ = psx[ci % 2]
            k.op("act", lambda e, t0=t0, n=n: e.activation(out=Krep[:, t0:t0 + n], in_=yo[:, t0:t0 + n], func=AF.Square), r=[yo], w=[Krep])
            k.op("pe", lambda e, p_=p_, t0=t0, n=n: e.matmul(p_[:, 0:n], lhsT=ones[:], rhs=Krep[:, t0:t0 + n], start=True, stop=True),
                 r=[ones, Krep], w=[p_])
            k.op("act", lambda e, p_=p_, t0=t0, n=n: e.activation(out=Qrep[:, t0:t0 + n], in_=p_[:, 0:n], func=AF.Sqrt, bias=epsb[:], scale=1.0 / 128),
                 r=[p_, epsb], w=[Qrep])
        k.op("dve", lambda e: e.reciprocal(out=Qrep[:], in_=Qrep[:]), r=[Qrep], w=[Qrep])
        k.op("dve", lambda e: e.tensor_tensor(out=yo[:], in0=yo[:], in1=Qrep[:], op=ALU.mult), r=[yo, Qrep], w=[yo])
        k.op("dve", lambda e: e.scalar_tensor_tensor(out=yo[:], in0=yo[:], scalar=nw[:, 0:1], in1=vt[:], op0=ALU.mult, op1=ALU.mult),
             r=[yo, nw, vt], w=[yo])
        k.dma(yc[128 * hs:128 * (hs + 1), :], yo[:], r=[yo], q="pool")


def ssd_host_params(inp, li, j):
    cw = inp["ssd_conv_w"][li]
    cb = inp["ssd_conv_b"][li]
    px = np.zeros((128, 2, 8), np.float32)
    for ti in range(2):
        n = 128 if ti == 0 else 64
        c = 192 * j + 128 * ti + np.arange(n)
        px[:n, ti, 0:3] = cw[:, c].T
        px[:n, ti, 3] = cb[c]
        px[:n, ti, 4] = inp["ssd_d"][li][c // 64]
    g = j // 2
    pB = np.zeros((128, 8), np.float32)
    pC = np.zeros((128, 8), np.float32)
    for (arr, base) in ((pB, 768 + 64 * g), (pC, 768 + 128 + 64 * g)):
        c = base + (np.arange(128) % 64)
        arr[:, 0:3] = cw[:, c].T
        arr[:, 3] = cb[c]
    pdt = np.zeros((6, 4), np.float32)
    for d in range(2):
        for h in range(3):
            pdt[3 * d + h, 0] = inp["ssd_dt_bias"][li][d, 3 * j + h]
            pdt[3 * d + h, 1] = inp["ssd_a_log"][li][d, 3 * j + h]
    selr = np.zeros((6, 2, 2, 128), np.float32)
    for d in range(2):
        for m in range(128):
            selr[3 * d + m // 64, d, 0, m] = 1.0
            selr[3 * d + 2, d, 1, m] = 1.0
    selh = np.zeros((6, 6, 128), np.float32)
    for r in range(6):
        selh[r, r, :] = 1.0
    return {"ssd_px": px, "ssd_pB": pB, "ssd_pC": pC, "ssd_pdt": pdt, "selr": selr, "selh": selh}


SSD_SHAPES = {"ssd_px": [128, 2, 8], "ssd_pB": [128, 8], "ssd_pC": [128, 8], "ssd_pdt": [6, 4], "selr": [6, 2, 2, 128],
              "selh": [6, 6, 128]}


def gla_heads(j):
    return [j, j + 4 if j < 2 else j]


def gla_host_params(inp, li, j):
    gp = np.zeros((128, 2, 2, 2), np.float32)
    gw = np.zeros((16, 2, 2, 128), np.float32)
    for s_, h in enumerate(gla_heads(j)):
        for d in range(2):
            cols = 64 * h + (np.arange(128) % 64)
            gp[:, s_, d, 0] = inp["gla_gate_b"][li][d, cols]
            gw[:, s_, d, :] = inp["gla_gate_w"][li][d][:, cols]
    return {"gla_p": gp, "gla_w": gw, "gla_nw": np.ascontiguousarray(inp["gla_norm_w"][li][:, None], dtype=np.float32)}


GLA_SHAPES = {"gla_p": [128, 2, 2, 2], "gla_w": [16, 2, 2, 128], "gla_nw": [128, 1]}


def scan_consts():
    selx = np.zeros((128, 32, 128), np.float32)
    for p in range(128):
        q = p % 64
        selx[p, q // 2, 64 * (q % 2):64 * (q % 2) + 64] = 1.0
    hm = np.zeros((128, 256), np.float32)
    hm[0:64, 128] = 1.0
    hm[64:128, 129] = 1.0
    return {"c_selx": selx, "c_hm": hm}


def build_am(parts=("A",)):
    nc = bass.Bass("TRN2", target_bir_lowering=False)
    k = KB(nc)
    xT = nc.dram_tensor("xT", [D, T], F32, kind="ExternalInput").ap()
    xTc = None
    modv = nc.dram_tensor("modv", [D, 4], F32, kind="ExternalInput").ap()
    wA = nc.dram_tensor("wA", [D, NA], F32, kind="ExternalInput").ap()
    pT = nc.dram_tensor("pT", [NMIX, T], F32, kind="ExternalOutput" if "dumpP" in parts else "Internal").ap()
    gates = nc.dram_tensor("gates", [D, T], BF16, kind="ExternalOutput").ap()
    prm = {}
    cst = {}
    for nm, shp in (("ident", [128, 128]), ("swap", [128, 128]), ("selx", [128, 32, 128]), ("hm", [128, 256])):
        ap = nc.dram_tensor("c_" + nm, shp, F32, kind="ExternalInput").ap()
        cst[nm] = k.sb(shp, F32, nm)
        k.dma(cst[nm][:], ap, w=[cst[nm]])
    mk = k.mark()
    if "A" in parts:
        emit_inproj(k, nc, xT, xTc, modv, wA, pT, gates)
        k.release(mk)
    for (tag, shapes, fn, oname, orows) in (("S5", S5_SHAPES, emit_s5, "ya", 192), ("SSD", SSD_SHAPES, emit_ssd, "yb", 192),
                                            ("GLA", GLA_SHAPES, emit_gla, "yc", 256), ("HY", HY_SHAPES, emit_hyena, "yd", 192)):
        if tag in parts:
            for nm, shp in shapes.items():
                if nm not in prm:
                    prm[nm] = nc.dram_tensor(nm, shp, F32, kind="ExternalInput").ap()
            o = nc.dram_tensor(oname, [orows, T], F32, kind="ExternalOutput").ap()
            fn(k, nc, pT, o, cst, prm)
            k.release(mk)
    k.emit()
    return nc


HY_SHAPES = {"hy_cw": [128, 2, 3, 4], "hy_bias": [128, 2, 2], "hy_w1": [33, 64], "hy_w2": [64, 64], "hy_mlp": [64, 4],
             "hy_w3": [64, 2, 2, 192], "hy_embL": [33, S], "hy_embC": [33, NCTX], "hy_decL": [128, 2, S], "hy_decC": [128, 2, NCTX]}


def hy_host_params(inp, li, j):
    cw = np.zeros((128, 2, 3, 4), np.float32)
    hb = np.zeros((128, 2, 2), np.float32)
    for ti in range(2):
        n = 128 if ti == 0 else 64
        for s_ in range(3):
            c = 768 * s_ + 192 * j + 128 * ti + np.arange(n)
            cw[:n, ti, s_, 0:3] = inp["hy_conv_w"][li][:, c].T
            cw[:n, ti, s_, 3] = inp["hy_conv_b"][li][c]
        c = 192 * j + 128 * ti + np.arange(n)
        hb[:n, ti, :] = inp["hy_bias"][li][:, c].T
    mlp = np.stack([inp["hy_b1"][li], inp["hy_freq1"][li], inp["hy_b2"][li], inp["hy_freq2"][li]], 1)
    w3 = inp["hy_w3"][li].reshape(64, 2, 2, 768)[:, :, :, 192 * j:192 * (j + 1)]
    out = {"hy_cw": cw, "hy_bias": hb, "hy_w1": inp["hy_w1"][li], "hy_w2": inp["hy_w2"][li], "hy_mlp": mlp, "hy_w3": w3}
    deltas = np.abs(np.linspace(math.log(1e-2) / 1.5, math.log(1e-2) / 0.3, 768, dtype=np.float32))[192 * j:192 * (j + 1)]
    for tag, n in (("L", S), ("C", NCTX)):
        t = np.linspace(0.0, 1.0, n, dtype=np.float32)
        freqs = np.linspace(1e-4, 15.0, 16, dtype=np.float32)
        ang = (np.float32(2.0 * math.pi / n) * np.arange(n, dtype=np.float32)[:, None]) * freqs[None, :]
        emb = np.concatenate([t[:, None], np.cos(ang), -np.sin(ang)], -1)
        out["hy_emb" + tag] = emb.T
        dec = np.exp(-t[None, :] * deltas[:, None])
        dd = np.zeros((128, 2, n), np.float32)
        dd[:, 0] = dec[0:128]
        dd[0:64, 1] = dec[128:192]
        dd[64:128, 1] = dec[128:192]
        out["hy_dec" + tag] = dd
    return {kk: np.ascontiguousarray(v, dtype=np.float32) for kk, v in out.items()}


def emit_hyena(k, nc, pT, yd, cst, prm):
    cw = k.sb([128, 2, 3, 4], F32, "hy_cw")
    hbias = k.sb([128, 2, 2], F32, "hy_bias")
    w1 = k.sb([33, 64], F32, "hy_w1")
    w2 = k.sb([64, 64], F32, "hy_w2")
    mlp = k.sb([64, 4], F32, "hy_mlp")
    w3 = k.sb([64, 2, 2, 192], F32, "hy_w3")
    for t_, nm in ((cw, "hy_cw"), (hbias, "hy_bias"), (w1, "hy_w1"), (w2, "hy_w2"), (mlp, "hy_mlp"), (w3, "hy_w3")):
        k.dma(t_[:], prm[nm], w=[t_])
    fsc = k.sb([64, 2], F32, "hy_fsc")
    k.op("dve", lambda e: e.tensor_scalar(out=fsc[:, 0:1], in0=mlp[:, 1:2], scalar1=1.0 / TWO_PI, scalar2=None, op0=ALU.mult), r=[mlp], w=[fsc])
    k.op("dve", lambda e: e.tensor_scalar(out=fsc[:, 1:2], in0=mlp[:, 3:4], scalar1=1.0 / TWO_PI, scalar2=None, op0=ALU.mult), r=[mlp], w=[fsc])
    h2 = {"L": k.sb([64, S], F32, "h2L"), "C": k.sb([64, NCTX], F32, "h2C")}
    ps = [k.ps() for _ in range(2)]
    mk = k.mark()
    emb = k.sb([33, 512], F32, "emb")
    h1 = k.sb([64, 512], F32, "h1")
    xa_ = k.sb([64, 512], F32, "hyx")
    xi = k.sb([64, 512], I32, "hyxi")
    xf = k.sb([64, 512], F32, "hyxf")

    def sin_layer(dst, src_ps, n, bcol, fcol):
        k.op("dve", lambda e: e.tensor_scalar(out=xa_[:, 0:n], in0=src_ps[0:64, 0:n], scalar1=mlp[:, bcol:bcol + 1], scalar2=fsc[:, fcol:fcol + 1],
                                              op0=ALU.add, op1=ALU.mult), r=[src_ps, mlp, fsc], w=[xa_])
        k.op("dve", lambda e: e.tensor_copy(out=xi[:, 0:n], in_=xa_[:, 0:n]), r=[xa_], w=[xi])
        k.op("dve", lambda e: e.tensor_copy(out=xf[:, 0:n], in_=xi[:, 0:n]), r=[xi], w=[xf])
        k.op("dve", lambda e: e.tensor_tensor(out=xf[:, 0:n], in0=xa_[:, 0:n], in1=xf[:, 0:n], op=ALU.subtract), r=[xa_, xf], w=[xf])
        k.op("dve", lambda e: e.tensor_scalar(out=xf[:, 0:n], in0=xf[:, 0:n], scalar1=0.4999999, scalar2=-0.4999999, op0=ALU.min, op1=ALU.max),
             r=[xf], w=[xf])
        k.op("act", lambda e: e.activation(out=dst, in_=xf[:, 0:n], func=AF.Sin, scale=TWO_PI), r=[xf], w=[h1, h2["L"], h2["C"]])

    for tag, n_tot in (("L", S), ("C", NCTX)):
        for (t0, n) in chunks(n_tot):
            k.dma(emb[:, 0:n], prm["hy_emb" + tag][:, t0:t0 + n], w=[emb])
            k.op("pe", lambda e, n=n: e.matmul(ps[0][0:64, 0:n], lhsT=w1[:], rhs=emb[:, 0:n], start=True, stop=True), r=[w1, emb], w=[ps[0]])
            sin_layer(h1[:, 0:n], ps[0], n, 0, 0)
            k.op("pe", lambda e, n=n: e.matmul(ps[1][0:64, 0:n], lhsT=w2[:], rhs=h1[:, 0:n], start=True, stop=True), r=[w2, h1], w=[ps[1]])
            sin_layer(h2[tag][:, t0:t0 + n], ps[1], n, 2, 1)
    k.release(mk)
    u = k.sb([128, T], F32, "hy_u")
    g = [k.sb([128, T], F32, f"hy_g{i}") for i in range(2)]
    acc = k.sb([128, T], F32, "hy_acc")
    raw = k.sb([128, T], F32, "hy_raw")
    hf = k.sb([128, S], F32, "hy_hf")
    hb = k.sb([128, S], F32, "hy_hb")
    hfc = k.sb([128, NCTX], F32, "hy_hfc")
    hbc = k.sb([128, NCTX], F32, "hy_hbc")
    dec = k.sb([128, 512], F32, "hy_dec")
    w3b = k.sb([64, 128], F32, "hy_w3b")
    rows = [SEG["hv"][0], SEG["hx1"][0], SEG["hx2"][0]]
    for ti, P in ((0, 128), (1, 64)):
        for s_, dst in ((0, u), (1, g[0]), (2, g[1])):
            k.dma(raw[0:P, :], pT[rows[s_] + 128 * ti:rows[s_] + 128 * ti + P, :], r=[pT.tensor], w=[raw])
            emit_conv3(k, dst, raw, cw[:, ti, s_, :], cw[:, ti, s_, 3:4], P, False, wt=cw)
        for o in range(2):
            if ti == 1:
                k.op("dve", lambda e, o=o: e.tensor_copy(out=w3b[:, 0:64], in_=w3[:, o, 0, 128:192]), r=[w3], w=[w3b])
                k.op("dve", lambda e, o=o: e.tensor_copy(out=w3b[:, 64:128], in_=w3[:, o, 1, 128:192]), r=[w3], w=[w3b])
                for tag, n_tot, F_ in (("L", S, hf), ("C", NCTX, hfc)):
                    for ci, (t0, n) in enumerate(chunks(n_tot)):
                        p_ = ps[ci % 2]
                        k.dma(dec[:, 0:n], prm["hy_dec" + tag][:, 1, t0:t0 + n], w=[dec])
                        k.op("pe", lambda e, p_=p_, tag=tag, t0=t0, n=n: e.matmul(p_[:, 0:n], lhsT=w3b[:], rhs=h2[tag][:, t0:t0 + n],
                                                                                start=True, stop=True), r=[w3b, h2[tag]], w=[p_])
                        k.op("dve", lambda e, p_=p_, F_=F_, t0=t0, n=n: e.tensor_tensor(out=F_[:, t0:t0 + n], in0=p_[:, 0:n], in1=dec[:, 0:n],
                                                                                      op=ALU.mult), r=[p_, dec], w=[F_])
                    k.op("dve", lambda e, F_=F_: e.memset(F_[64:128, 0:1], 0.0), r=[F_], w=[F_])
                for (a, n) in ((0, NCTX), (NCTX, S)):
                    k.op("dve", lambda e, a=a, n=n: e.tensor_copy(out=raw[0:64, a:a + n], in_=u[0:64, a:a + n][:, ::-1]), r=[u], w=[raw])
                k.dma(u[64:128, :], raw[0:64, :], r=[raw], w=[u])
                for (a, n_tot, F_) in ((NCTX, S, hf), (0, NCTX, hfc)):
                    k.op("dve", lambda e, a=a, n_tot=n_tot, o=o: e.tensor_scalar(out=acc[:, a:a + n_tot], in0=u[:, a:a + n_tot],
                                                                                scalar1=hbias[:, 1, o:o + 1], scalar2=None, op0=ALU.mult),
                         r=[u, hbias], w=[acc])
                    for tau in range(n_tot):
                        L = n_tot - tau
                        k.op("dve", lambda e, a=a, tau=tau, L=L, F_=F_: e.scalar_tensor_tensor(
                            out=acc[:, a + tau:a + tau + L], in0=u[:, a:a + L], scalar=F_[:, tau:tau + 1], in1=acc[:, a + tau:a + tau + L],
                            op0=ALU.mult, op1=ALU.add), r=[u, F_, acc], w=[acc])
                k.dma(raw[0:64, :], acc[64:128, :], r=[acc], w=[raw])
                for (a, n) in ((0, NCTX), (NCTX, S)):
                    k.op("dve", lambda e, a=a, n=n: e.tensor_tensor(out=acc[0:64, a:a + n], in0=acc[0:64, a:a + n],
                                                                   in1=raw[0:64, a:a + n][:, ::-1], op=ALU.add), r=[acc, raw], w=[acc])
                k.op("dve", lambda e, o=o: e.tensor_tensor(out=u[0:64, :], in0=acc[0:64, :], in1=g[o][0:64, :], op=ALU.mult), r=[acc, g[o]], w=[u])
                continue
            for tag, n_tot, F_, B_ in (("L", S, hf, hb), ("C", NCTX, hfc, hbc)):
                for dd_, dstf in ((0, F_), (1, B_)):
                    for ci, (t0, n) in enumerate(chunks(n_tot)):
                        p_ = ps[ci % 2]
                        k.dma(dec[0:P, 0:n], prm["hy_dec" + tag][0:P, ti, t0:t0 + n], w=[dec])
                        k.op("pe", lambda e, p_=p_, o=o, dd_=dd_, ti=ti, P=P, tag=tag, t0=t0, n=n: e.matmul(
                            p_[0:P, 0:n], lhsT=w3[:, o, dd_, 128 * ti:128 * ti + P], rhs=h2[tag][:, t0:t0 + n], start=True, stop=True),
                            r=[w3, h2[tag]], w=[p_])
                        k.op("dve", lambda e, p_=p_, dstf=dstf, P=P, t0=t0, n=n: e.tensor_tensor(out=dstf[0:P, t0:t0 + n], in0=p_[0:P, 0:n],
                                                                                             in1=dec[0:P, 0:n], op=ALU.mult), r=[p_, dec], w=[dstf])
            for (a, n_tot, F_, B_) in ((NCTX, S, hf, hb), (0, NCTX, hfc, hbc)):
                k.op("dve", lambda e, a=a, n_tot=n_tot, P=P, ti=ti, o=o: e.tensor_scalar(
                    out=acc[0:P, a:a + n_tot], in0=u[0:P, a:a + n_tot], scalar1=hbias[0:P, ti, o:o + 1], scalar2=None, op0=ALU.mult),
                    r=[u, hbias], w=[acc])
                for tau in range(n_tot):
                    L = n_tot - tau
                    k.op("dve", lambda e, a=a, tau=tau, L=L, P=P, F_=F_: e.scalar_tensor_tensor(
                        out=acc[0:P, a + tau:a + tau + L], in0=u[0:P, a:a + L], scalar=F_[0:P, tau:tau + 1], in1=acc[0:P, a + tau:a + tau + L],
                        op0=ALU.mult, op1=ALU.add), r=[u, F_, acc], w=[acc])
                    if tau > 0:
                        k.op("dve", lambda e, a=a, tau=tau, L=L, P=P, B_=B_: e.scalar_tensor_tensor(
                            out=acc[0:P, a:a + L], in0=u[0:P, a + tau:a + tau + L], scalar=B_[0:P, tau:tau + 1], in1=acc[0:P, a:a + L],
                            op0=ALU.mult, op1=ALU.add), r=[u, B_, acc], w=[acc])
            k.op("dve", lambda e, o=o, P=P: e.tensor_tensor(out=u[0:P, :], in0=acc[0:P, :], in1=g[o][0:P, :], op=ALU.mult), r=[acc, g[o]], w=[u])
        k.dma(yd[128 * ti:128 * ti + P, :], u[0:P, :], r=[u], q="pool")


NT = 64 + 1024
TCH_C = [(0, 64), (64, 512), (576, 512)]
MW = 768


def emit_cast_w(k, dst_bf, src_dram, kch, ncols, wf, it0=0):
    it = it0
    step = 256 if kch <= 8 else 128
    for c0 in range(0, ncols, step):
        n = min(step, ncols - c0)
        w_ = wf[it % 2]
        it += 1
        wv = w_[:, 0:kch * n].rearrange("p (k c) -> p k c", k=kch)
        k.dma(wv, src_dram[:, c0:c0 + n].rearrange("(k p) c -> p k c", p=128), w=[w_])
        k.op("pool", lambda e, wv=wv, c0=c0, n=n: e.tensor_copy(out=dst_bf[:, :, c0:c0 + n], in_=wv), r=[w_], w=[dst_bf])
    return it


def build_c():
    nc = bass.Bass("TRN2", target_bir_lowering=False)
    k = KB(nc)
    di = lambda nm, shp, dt=F32: nc.dram_tensor(nm, shp, dt, kind="ExternalInput").ap()
    xT = di("xT", [D, NT])
    yT = di("yT", [4, MW, NT])
    gT = di("gT", [4, D, NT], BF16)
    gluw = di("gluw", [MW, MW])
    wbr = di("wbr", [4, MW, D])
    wout = di("wout", [D, D])
    mvd = di("modv", [D, 8])
    snw = di("ssd_nw", [128, 6])
    rwd = di("rw", [D, 20])
    rbd = di("rb", [1, 20])
    xmidT = nc.dram_tensor("xmidT", [D, NT], F32, kind="ExternalOutput").ap()
    h2T = nc.dram_tensor("h2T", [D, NT], BF16, kind="ExternalOutput").ap()
    comb = nc.dram_tensor("comb", [NT, 16], F32, kind="ExternalOutput").ap()

    ones = k.sb([128, 128], F32, "ones")
    k.op("dve", lambda e: e.memset(ones[:], 1.0), w=[ones])
    epsb = k.sb([128, 1], F32, "epsb")
    k.op("dve", lambda e: e.memset(epsb[:], EPS), w=[epsb])
    mv = k.sb([128, 16, 8], F32, "mv")
    k.dma(mv[:], mvd.rearrange("(k p) r -> p k r", p=128), w=[mv])
    for col in (3, 5):
        k.op("dve", lambda e, col=col: e.tensor_scalar(out=mv[:, :, col], in0=mv[:, :, col], scalar1=1.0, scalar2=None, op0=ALU.add), r=[mv], w=[mv])
    nw = k.sb([128, 6, 1], F32, "snw")
    k.dma(nw[:, :, 0], snw, w=[nw])
    wf = [k.sb([128, 2048], F32, f"wf{i}") for i in range(2)]
    macc = k.sb([128, 16, NT], F32, "macc")
    ps = [k.ps() for _ in range(4)]
    psn = 0
    mk = k.mark()
    yf = k.sb([128, 6, NT], F32, "yf")
    ybf = k.sb([128, 6, NT], BF16, "ybf")
    t1 = k.sb([128, 6, NT], F32, "t1")
    wb = k.sb([128, 6, D], BF16, "wb")
    gw = k.sb([128, 6, MW], BF16, "gw")
    gt = [k.sb([128, 512], BF16, f"gt{i}") for i in range(2)]
    tmp = [k.sb([128, 512], F32, f"tmp{i}") for i in range(2)]
    wit = 0
    git = 0
    for br in range(4):
        k.dma(yf[:], yT[br].rearrange("(k p) t -> p k t", p=128), w=[yf])
        if br == 0:
            k.op("dve", lambda e: e.tensor_tensor(out=t1[:], in0=yf[:], in1=yf[:], op=ALU.mult), r=[yf], w=[t1])
            k.op("dve", lambda e: e.tensor_scalar(out=t1[:], in0=t1[:], scalar1=0.044715, scalar2=1.0, op0=ALU.mult, op1=ALU.add), r=[t1], w=[t1])
            k.op("dve", lambda e: e.tensor_tensor(out=t1[:], in0=t1[:], in1=yf[:], op=ALU.mult), r=[t1, yf], w=[t1])
            k.op("act", lambda e: e.activation(out=t1[:], in_=t1[:], func=AF.Tanh, scale=math.sqrt(2.0 / math.pi)), r=[t1], w=[t1])
            k.op("dve", lambda e: e.tensor_scalar(out=t1[:], in0=t1[:], scalar1=1.0, scalar2=0.5, op0=ALU.add, op1=ALU.mult), r=[t1], w=[t1])
            k.op("dve", lambda e: e.tensor_tensor(out=yf[:], in0=t1[:], in1=yf[:], op=ALU.mult), r=[t1, yf], w=[yf])
            k.op("pool", lambda e: e.tensor_copy(out=ybf[:], in_=yf[:]), r=[yf], w=[ybf])
            wit = emit_cast_w(k, gw, gluw, 6, MW, wf, wit)
            for oc in range(6):
                for (t0, n) in TCH_C:
                    p_ = ps[psn % 4]
                    psn += 1
                    for kk in range(6):
                        k.op("pe", lambda e, p_=p_, oc=oc, kk=kk, t0=t0, n=n: e.matmul(p_[:, 0:n], lhsT=gw[:, kk, oc * 128:(oc + 1) * 128],
                                                                                     rhs=ybf[:, kk, t0:t0 + n], start=(kk == 0), stop=(kk == 5)),
                             r=[gw, ybf], w=[p_])
                    k.op("act", lambda e, p_=p_, oc=oc, t0=t0, n=n: e.activation(out=t1[:, oc, t0:t0 + n], in_=p_[:, 0:n], func=AF.Sigmoid),
                         r=[p_], w=[t1])
            k.op("dve", lambda e: e.tensor_tensor(out=yf[:], in0=yf[:], in1=t1[:], op=ALU.mult), r=[yf, t1], w=[yf])
        if br == 1:
            for (t0, n) in TCH_C:
                p_ = ps[psn % 4]
                psn += 1
                for kk in range(6):
                    k.op("act", lambda e, kk=kk, t0=t0, n=n: e.activation(out=t1[:, kk, t0:t0 + n], in_=yf[:, kk, t0:t0 + n], func=AF.Square),
                         r=[yf], w=[t1])
                    k.op("pe", lambda e, p_=p_, kk=kk, t0=t0, n=n: e.matmul(p_[:, 0:n], lhsT=ones[:], rhs=t1[:, kk, t0:t0 + n],
                                                                          start=(kk == 0), stop=(kk == 5)), r=[ones, t1], w=[p_])
                k.op("act", lambda e, p_=p_, t0=t0, n=n: e.activation(out=t1[:, 0, t0:t0 + n], in_=p_[:, 0:n], func=AF.Sqrt, bias=epsb[:], scale=1.0 / MW),
                     r=[p_, epsb], w=[t1])
            k.op("dve", lambda e: e.reciprocal(out=t1[:, 0, :], in_=t1[:, 0, :]), r=[t1], w=[t1])
            for kk in range(6):
                k.op("dve", lambda e, kk=kk: e.scalar_tensor_tensor(out=yf[:, kk, :], in0=yf[:, kk, :], scalar=nw[:, kk, 0:1], in1=t1[:, 0, :],
                                                                   op0=ALU.mult, op1=ALU.mult), r=[yf, nw, t1], w=[yf])
        k.op("pool", lambda e: e.tensor_copy(out=ybf[:], in_=yf[:]), r=[yf], w=[ybf])
        wit = emit_cast_w(k, wb, wbr[br], 6, D, wf, wit)
        for dc in range(16):
            for (t0, n) in TCH_C:
                p_ = ps[psn % 4]
                psn += 1
                g_ = gt[git % 2]
                m_ = tmp[git % 2]
                git += 1
                k.dma(g_[:, 0:n], gT[br, dc * 128:(dc + 1) * 128, t0:t0 + n], w=[g_])
                for kk in range(6):
                    k.op("pe", lambda e, p_=p_, dc=dc, kk=kk, t0=t0, n=n: e.matmul(p_[:, 0:n], lhsT=wb[:, kk, dc * 128:(dc + 1) * 128],
                                                                                 rhs=ybf[:, kk, t0:t0 + n], start=(kk == 0), stop=(kk == 5)),
                         r=[wb, ybf], w=[p_])
                if br == 0:
                    k.op("dve", lambda e, p_=p_, g_=g_, dc=dc, t0=t0, n=n: e.tensor_tensor(out=macc[:, dc, t0:t0 + n], in0=p_[:, 0:n], in1=g_[:, 0:n],
                                                                                         op=ALU.mult), r=[p_, g_], w=[macc])
                else:
                    k.op("dve", lambda e, p_=p_, g_=g_, m_=m_, n=n: e.tensor_tensor(out=m_[:, 0:n], in0=p_[:, 0:n], in1=g_[:, 0:n], op=ALU.mult),
                         r=[p_, g_], w=[m_])
                    k.op("pool", lambda e, m_=m_, dc=dc, t0=t0, n=n: e.tensor_tensor(out=macc[:, dc, t0:t0 + n], in0=macc[:, dc, t0:t0 + n],
                                                                                   in1=m_[:, 0:n], op=ALU.add), r=[m_, macc], w=[macc])
    k.release(mk)
    mk2 = k.mark()
    mbf = k.sb([128, 16, NT], BF16, "mbf")
    k.op("pool", lambda e: e.tensor_copy(out=mbf[:], in_=macc[:]), r=[macc], w=[mbf])
    xmid = macc
    wo = [k.sb([128, 16, 128], BF16, f"wo{i}") for i in range(2)]
    xin = [k.sb([128, NT], F32, f"xin{i}") for i in range(2)]
    for dc in range(16):
        w_ = wf[dc % 2]
        wo_ = wo[dc % 2]
        x_ = xin[dc % 2]
        wv = w_[:].rearrange("p (k c) -> p k c", k=16)
        k.dma(wv, wout[:, dc * 128:(dc + 1) * 128].rearrange("(k p) c -> p k c", p=128), w=[w_])
        k.op("pool", lambda e, wv=wv, wo_=wo_: e.tensor_copy(out=wo_[:], in_=wv), r=[w_], w=[wo_])
        k.dma(x_[:], xT[dc * 128:(dc + 1) * 128, :], w=[x_])
        for (t0, n) in TCH_C:
            p_ = ps[psn % 4]
            psn += 1
            for kk in range(16):
                k.op("pe", lambda e, p_=p_, wo_=wo_, kk=kk, t0=t0, n=n: e.matmul(p_[:, 0:n], lhsT=wo_[:, kk, :], rhs=mbf[:, kk, t0:t0 + n],
                                                                               start=(kk == 0), stop=(kk == 15)), r=[wo_, mbf], w=[p_])
            gcol = 1 if t0 == 0 else 0
            k.op("dve", lambda e, p_=p_, x_=x_, dc=dc, t0=t0, n=n, gcol=gcol: e.scalar_tensor_tensor(
                out=xmid[:, dc, t0:t0 + n], in0=p_[:, 0:n], scalar=mv[:, dc, gcol:gcol + 1], in1=x_[:, t0:t0 + n], op0=ALU.mult, op1=ALU.add),
                r=[p_, mv, x_, mbf], w=[xmid])
        k.dma(xmidT[dc * 128:(dc + 1) * 128, :], xmid[:, dc, :], r=[xmid], q="pool")
    k.release(mk2)
    h2 = k.sb([128, 16, NT], F32, "h2")
    rstd = k.sb([128, NT], F32, "rstd")
    sq = [k.sb([128, 512], F32, f"sq{i}") for i in range(2)]
    for (t0, n) in TCH_C:
        p_ = ps[psn % 4]
        psn += 1
        for kk in range(16):
            s_ = sq[kk % 2]
            k.op("act", lambda e, s_=s_, kk=kk, t0=t0, n=n: e.activation(out=s_[:, 0:n], in_=xmid[:, kk, t0:t0 + n], func=AF.Square), r=[xmid], w=[s_])
            k.op("pe", lambda e, p_=p_, s_=s_, kk=kk, n=n: e.matmul(p_[:, 0:n], lhsT=ones[:], rhs=s_[:, 0:n], start=(kk == 0), stop=(kk == 15)),
                 r=[ones, s_], w=[p_])
        k.op("act", lambda e, p_=p_, t0=t0, n=n: e.activation(out=rstd[:, t0:t0 + n], in_=p_[:, 0:n], func=AF.Sqrt, bias=epsb[:], scale=1.0 / D),
             r=[p_, epsb], w=[rstd])
    k.op("dve", lambda e: e.reciprocal(out=rstd[:], in_=rstd[:]), r=[rstd], w=[rstd])
    hb = [k.sb([128, NT], BF16, f"hb{i}") for i in range(2)]
    for kk in range(16):
        k.op("dve", lambda e, kk=kk: e.tensor_tensor(out=h2[:, kk, :], in0=xmid[:, kk, :], in1=rstd[:], op=ALU.mult), r=[xmid, rstd], w=[h2])
        for (t0, n) in TCH_C:
            mo = 4 if t0 == 0 else 2
            k.op("dve", lambda e, kk=kk, t0=t0, n=n, mo=mo: e.tensor_scalar(out=h2[:, kk, t0:t0 + n], in0=h2[:, kk, t0:t0 + n],
                                                                           scalar1=mv[:, kk, mo + 1:mo + 2], scalar2=mv[:, kk, mo:mo + 1],
                                                                           op0=ALU.mult, op1=ALU.add), r=[h2, mv], w=[h2])
        hb_ = hb[kk % 2]
        k.op("pool", lambda e, hb_=hb_, kk=kk: e.tensor_copy(out=hb_[:], in_=h2[:, kk, :]), r=[h2], w=[hb_])
        k.dma(h2T[kk * 128:(kk + 1) * 128, :], hb_[:], r=[hb_], q="pool")
    rw = k.sb([128, 16, 20], F32, "rw")
    rb = k.sb([1, 20], F32, "rb")
    k.dma(rw[:], rwd.rearrange("(k p) c -> p k c", p=128), w=[rw])
    k.dma(rb[:], rbd, w=[rb])
    lg = k.sb([128, 20], F32, "lg")
    sm = {nm: k.sb([128, 4], F32, "r_" + nm) for nm in ("ohg", "eg", "esel", "oh1", "msk", "oh2", "within")}
    sc = {nm: k.sb([128, 1], F32, "r_" + nm) for nm in ("gmax", "ngmax", "ssum", "pg", "m1", "nm1", "m2", "e2", "den", "w1", "w2")}
    cb = [k.sb([128, 16], F32, f"cb{i}") for i in range(2)]
    V = lambda e: e
    for ti, (t0, n) in enumerate([(0, 64)] + [(64 + 128 * i, 128) for i in range(8)]):
        p_ = ps[psn % 4]
        psn += 1
        for kk in range(16):
            k.op("pe", lambda e, p_=p_, kk=kk, t0=t0, n=n: e.matmul(p_[0:n, 0:20], lhsT=h2[:, kk, t0:t0 + n], rhs=rw[:, kk, :],
                                                                   start=(kk == 0), stop=False), r=[h2, rw], w=[p_])
        k.op("pe", lambda e, p_=p_, n=n: e.matmul(p_[0:n, 0:20], lhsT=ones[0:1, 0:n], rhs=rb[0:1, :], start=False, stop=True), r=[ones, rb], w=[p_])
        k.op("act", lambda e, p_=p_, n=n: e.activation(out=lg[0:n, :], in_=p_[0:n, 0:20], func=AF.Copy), r=[p_], w=[lg])
        P = n
        o = lambda eng, fn, r, w: k.op(eng, fn, r=r, w=w)
        o("dve", lambda e: e.reduce_max(out=sc["gmax"][0:P], in_=lg[0:P, 0:4], axis=AX.X), [lg], [sc["gmax"]])
        o("dve", lambda e: e.tensor_scalar(out=sm["ohg"][0:P], in0=lg[0:P, 0:4], scalar1=sc["gmax"][0:P, 0:1], scalar2=None, op0=ALU.is_ge),
          [lg, sc["gmax"]], [sm["ohg"]])
        o("dve", lambda e: e.tensor_scalar(out=sc["ngmax"][0:P], in0=sc["gmax"][0:P], scalar1=-1.0, scalar2=None, op0=ALU.mult), [sc["gmax"]], [sc["ngmax"]])
        o("act", lambda e: e.activation(out=sm["eg"][0:P], in_=lg[0:P, 0:4], func=AF.Exp, bias=sc["ngmax"][0:P], scale=1.0), [lg, sc["ngmax"]], [sm["eg"]])
        o("dve", lambda e: e.reduce_sum(out=sc["ssum"][0:P], in_=sm["eg"][0:P], axis=AX.X), [sm["eg"]], [sc["ssum"]])
        o("dve", lambda e: e.reciprocal(out=sc["pg"][0:P], in_=sc["ssum"][0:P]), [sc["ssum"]], [sc["pg"]])
        o("dve", lambda e: e.tensor_scalar(out=sm["esel"][0:P], in0=lg[0:P, 4:8], scalar1=sm["ohg"][0:P, 0:1], scalar2=None, op0=ALU.mult),
          [lg, sm["ohg"]], [sm["esel"]])
        for g_ in range(1, 4):
            o("dve", lambda e, g_=g_: e.scalar_tensor_tensor(out=sm["esel"][0:P], in0=lg[0:P, 4 + 4 * g_:8 + 4 * g_], scalar=sm["ohg"][0:P, g_:g_ + 1],
                                                           in1=sm["esel"][0:P], op0=ALU.mult, op1=ALU.add), [lg, sm["ohg"], sm["esel"]], [sm["esel"]])
        o("dve", lambda e: e.reduce_max(out=sc["m1"][0:P], in_=sm["esel"][0:P], axis=AX.X), [sm["esel"]], [sc["m1"]])
        o("dve", lambda e: e.tensor_scalar(out=sm["oh1"][0:P], in0=sm["esel"][0:P], scalar1=sc["m1"][0:P, 0:1], scalar2=None, op0=ALU.is_ge),
          [sm["esel"], sc["m1"]], [sm["oh1"]])
        o("dve", lambda e: e.scalar_tensor_tensor(out=sm["msk"][0:P], in0=sm["oh1"][0:P], scalar=-1.0e30, in1=sm["esel"][0:P], op0=ALU.mult, op1=ALU.add),
          [sm["oh1"], sm["esel"]], [sm["msk"]])
        o("dve", lambda e: e.reduce_max(out=sc["m2"][0:P], in_=sm["msk"][0:P], axis=AX.X), [sm["msk"]], [sc["m2"]])
        o("dve", lambda e: e.tensor_scalar(out=sm["oh2"][0:P], in0=sm["msk"][0:P], scalar1=sc["m2"][0:P, 0:1], scalar2=None, op0=ALU.is_ge),
          [sm["msk"], sc["m2"]], [sm["oh2"]])
        o("dve", lambda e: e.tensor_scalar(out=sc["nm1"][0:P], in0=sc["m1"][0:P], scalar1=-1.0, scalar2=None, op0=ALU.mult), [sc["m1"]], [sc["nm1"]])
        o("act", lambda e: e.activation(out=sc["e2"][0:P], in_=sc["m2"][0:P], func=AF.Exp, bias=sc["nm1"][0:P], scale=1.0), [sc["m2"], sc["nm1"]], [sc["e2"]])
        o("dve", lambda e: e.tensor_scalar(out=sc["den"][0:P], in0=sc["e2"][0:P], scalar1=1.0, scalar2=None, op0=ALU.add), [sc["e2"]], [sc["den"]])
        o("dve", lambda e: e.reciprocal(out=sc["den"][0:P], in_=sc["den"][0:P]), [sc["den"]], [sc["den"]])
        o("dve", lambda e: e.tensor_tensor(out=sc["w1"][0:P], in0=sc["den"][0:P], in1=sc["pg"][0:P], op=ALU.mult), [sc["den"], sc["pg"]], [sc["w1"]])
        o("dve", lambda e: e.tensor_tensor(out=sc["w2"][0:P], in0=sc["w1"][0:P], in1=sc["e2"][0:P], op=ALU.mult), [sc["w1"], sc["e2"]], [sc["w2"]])
        o("dve", lambda e: e.tensor_scalar(out=sm["within"][0:P], in0=sm["oh1"][0:P], scalar1=sc["w1"][0:P, 0:1], scalar2=None, op0=ALU.mult),
          [sm["oh1"], sc["w1"]], [sm["within"]])
        o("dve", lambda e: e.scalar_tensor_tensor(out=sm["within"][0:P], in0=sm["oh2"][0:P], scalar=sc["w2"][0:P, 0:1], in1=sm["within"][0:P],
                                                  op0=ALU.mult, op1=ALU.add), [sm["oh2"], sc["w2"], sm["within"]], [sm["within"]])
        c_ = cb[ti % 2]
        for g_ in range(4):
            o("dve", lambda e, g_=g_, c_=c_: e.tensor_scalar(out=c_[0:P, 4 * g_:4 * g_ + 4], in0=sm["within"][0:P], scalar1=sm["ohg"][0:P, g_:g_ + 1],
                                                           scalar2=None, op0=ALU.mult), [sm["within"], sm["ohg"]], [c_])
        k.dma(comb[t0:t0 + n, :], c_[0:P, :], r=[c_], q="pool")
    k.emit()
    return nc


TT = B * T
FF = 1024


def build_e():
    nc = bass.Bass("TRN2", target_bir_lowering=False)
    k = KB(nc)
    h2T = nc.dram_tensor("h2T", [D, TT], BF16, kind="ExternalInput").ap()
    cmb = nc.dram_tensor("cmb", [2, 128, TT], F32, kind="ExternalInput").ap()
    wg = nc.dram_tensor("wg", [2, D, FF], F32, kind="ExternalInput").ap()
    wu = nc.dram_tensor("wu", [2, D, FF], F32, kind="ExternalInput").ap()
    wd = nc.dram_tensor("wd", [2, FF, D], F32, kind="ExternalInput").ap()
    part = nc.dram_tensor("part", [D, TT], F32, kind="ExternalOutput").ap()
    Wg = k.sb([128, 16, FF], BF16, "Wg")
    Wu = k.sb([128, 16, FF], BF16, "Wu")
    Wd = k.sb([128, 8, D], BF16, "Wd")
    wf = [k.sb([128, 2048], F32, f"wf{i}") for i in range(2)]
    hch = [k.sb([128, 16, 512], BF16, f"hch{i}") for i in range(2)]
    abf = [k.sb([128, 8, 512], BF16, f"abf{i}") for i in range(2)]
    cch = [k.sb([128, 512], F32, f"cch{i}") for i in range(2)]
    tmp = [k.sb([128, 512], F32, f"tmp{i}") for i in range(2)]
    ost = [k.sb([128, 512], F32, f"ost{i}") for i in range(3)]
    prv = [k.sb([128, 512], F32, f"prv{i}") for i in range(3)]
    pg = [k.ps() for _ in range(2)]
    pu = [k.ps() for _ in range(2)]
    po = [k.ps() for _ in range(3)]
    wit = 0
    io = 0
    for ex in range(2):
        wit = emit_cast_w(k, Wg, wg[ex], 16, FF, wf, wit)
        wit = emit_cast_w(k, Wu, wu[ex], 16, FF, wf, wit)
        wit = emit_cast_w(k, Wd, wd[ex], 8, D, wf, wit)
        for tc in range(TT // 512):
            t0 = tc * 512
            h_ = hch[tc % 2]
            a_ = abf[tc % 2]
            c_ = cch[tc % 2]
            k.dma(h_[:], h2T[:, t0:t0 + 512].rearrange("(k p) t -> p k t", p=128), w=[h_])
            k.dma(c_[:], cmb[ex, :, t0:t0 + 512], w=[c_])
            for fc in range(8):
                g_ = pg[fc % 2]
                u_ = pu[fc % 2]
                m_ = tmp[fc % 2]
                for kk in range(16):
                    k.op("pe", lambda e, g_=g_, h_=h_, fc=fc, kk=kk: e.matmul(g_[:], lhsT=Wg[:, kk, fc * 128:(fc + 1) * 128], rhs=h_[:, kk, :],
                                                                            start=(kk == 0), stop=(kk == 15)), r=[Wg, h_], w=[g_])
                for kk in range(16):
                    k.op("pe", lambda e, u_=u_, h_=h_, fc=fc, kk=kk: e.matmul(u_[:], lhsT=Wu[:, kk, fc * 128:(fc + 1) * 128], rhs=h_[:, kk, :],
                                                                            start=(kk == 0), stop=(kk == 15)), r=[Wu, h_], w=[u_])
                k.op("act", lambda e, g_=g_, m_=m_: e.activation(out=m_[:], in_=g_[:], func=AF.Silu), r=[g_], w=[m_])
                k.op("dve", lambda e, u_=u_, m_=m_: e.tensor_tensor(out=m_[:], in0=m_[:], in1=u_[:], op=ALU.mult), r=[m_, u_], w=[m_])
                k.op("pool", lambda e, m_=m_, a_=a_, c_=c_, fc=fc: e.tensor_tensor(out=a_[:, fc, :], in0=m_[:], in1=c_[:], op=ALU.mult),
                     r=[m_, c_], w=[a_])
            for dc in range(16):
                o_ = po[io % 3]
                s_ = ost[io % 3]
                p_ = prv[io % 3]
                io += 1
                key = f"part_{dc}_{tc}"
                for fc in range(8):
                    k.op("pe", lambda e, o_=o_, a_=a_, dc=dc, fc=fc: e.matmul(o_[:], lhsT=Wd[:, fc, dc * 128:(dc + 1) * 128], rhs=a_[:, fc, :],
                                                                            start=(fc == 0), stop=(fc == 7)), r=[Wd, a_], w=[o_])
                if ex == 0:
                    k.op("act", lambda e, o_=o_, s_=s_: e.activation(out=s_[:], in_=o_[:], func=AF.Copy), r=[o_], w=[s_])
                else:
                    k.dma(p_[:], part[dc * 128:(dc + 1) * 128, t0:t0 + 512], r=[key], w=[p_])
                    k.op("dve", lambda e, o_=o_, s_=s_, p_=p_: e.tensor_tensor(out=s_[:], in0=o_[:], in1=p_[:], op=ALU.add), r=[o_, p_], w=[s_])
                k.dma(part[dc * 128:(dc + 1) * 128, t0:t0 + 512], s_[:], r=[s_], w=[key], q="pool")
    k.emit()
    return nc


def run_e(inp, li, h2T, comb):
    maps = []
    for e in range(NCORES):
        cb = np.ascontiguousarray(np.broadcast_to(comb[:, 2 * e:2 * e + 2].T[:, None, :], (2, 128, TT)))
        maps.append({"h2T": h2T, "cmb": cb, "wg": inp["moe_w_gate"][li][2 * e:2 * e + 2], "wu": inp["moe_w_up"][li][2 * e:2 * e + 2],
                     "wd": inp["moe_w_down"][li][2 * e:2 * e + 2]})
    res = _run(build_e(), maps)
    return [r["part"] for r in res]


def build_r():
    nc = bass.Bass("TRN2", target_bir_lowering=False)
    k = KB(nc)
    xmidT = nc.dram_tensor("xmidT", [D, NT], F32, kind="ExternalInput").ap()
    parts = nc.dram_tensor("parts", [NCORES, D, NT], F32, kind="ExternalInput").ap()
    gvd = nc.dram_tensor("gv", [D, 4], F32, kind="ExternalInput").ap()
    xendT = nc.dram_tensor("xendT", [D, NT], F32, kind="ExternalOutput").ap()
    outT = nc.dram_tensor("outT", [D, NT], F32, kind="ExternalOutput").ap()
    ones = k.sb([128, 128], F32, "ones")
    k.op("dve", lambda e: e.memset(ones[:], 1.0), w=[ones])
    epsb = k.sb([128, 1], F32, "epsb")
    k.op("dve", lambda e: e.memset(epsb[:], EPS), w=[epsb])
    gv = k.sb([128, 16, 4], F32, "gv")
    k.dma(gv[:], gvd.rearrange("(k p) r -> p k r", p=128), w=[gv])
    xend = k.sb([128, 16, NT], F32, "xend")
    pb = [[k.sb([128, NT], F32, f"pb{i}_{e}") for e in range(NCORES)] for i in range(2)]
    xm = [k.sb([128, NT], F32, f"xm{i}") for i in range(2)]
    ps = [k.ps() for _ in range(3)]
    for dc in range(16):
        bufs = pb[dc % 2]
        x_ = xm[dc % 2]
        k.dma(x_[:], xmidT[dc * 128:(dc + 1) * 128, :], w=[x_])
        for e_ in range(NCORES):
            k.dma(bufs[e_][:], parts[e_, dc * 128:(dc + 1) * 128, :], w=[bufs[e_]])
        for (a_, b_, eng) in ((0, 1, "dve"), (2, 3, "pool"), (4, 5, "dve"), (6, 7, "pool"), (0, 2, "dve"), (4, 6, "pool"), (0, 4, "dve")):
            k.op(eng, lambda e, a_=a_, b_=b_, bufs=bufs: e.tensor_tensor(out=bufs[a_][:], in0=bufs[a_][:], in1=bufs[b_][:], op=ALU.add),
                 r=[bufs[a_], bufs[b_]], w=[bufs[a_]])
        for (t0, n, gc) in ((0, 64, 1), (64, 1024, 0)):
            k.op("dve", lambda e, bufs=bufs, x_=x_, dc=dc, t0=t0, n=n, gc=gc: e.scalar_tensor_tensor(
                out=xend[:, dc, t0:t0 + n], in0=bufs[0][:, t0:t0 + n], scalar=gv[:, dc, gc:gc + 1], in1=x_[:, t0:t0 + n], op0=ALU.mult, op1=ALU.add),
                r=[bufs[0], gv, x_], w=[xend])
        k.dma(xendT[dc * 128:(dc + 1) * 128, :], xend[:, dc, :], r=[xend], q="pool")
    rstd = k.sb([128, NT], F32, "rstd")
    sq = [k.sb([128, 512], F32, f"sq{i}") for i in range(2)]
    for ci, (t0, n) in enumerate(TCH_C):
        p_ = ps[ci % 3]
        for kk in range(16):
            s_ = sq[kk % 2]
            k.op("act", lambda e, s_=s_, kk=kk, t0=t0, n=n: e.activation(out=s_[:, 0:n], in_=xend[:, kk, t0:t0 + n], func=AF.Square), r=[xend], w=[s_])
            k.op("pe", lambda e, p_=p_, s_=s_, kk=kk, n=n: e.matmul(p_[:, 0:n], lhsT=ones[:], rhs=s_[:, 0:n], start=(kk == 0), stop=(kk == 15)),
                 r=[ones, s_], w=[p_])
        k.op("act", lambda e, p_=p_, t0=t0, n=n: e.activation(out=rstd[:, t0:t0 + n], in_=p_[:, 0:n], func=AF.Sqrt, bias=epsb[:], scale=1.0 / D),
             r=[p_, epsb], w=[rstd])
    k.op("dve", lambda e: e.reciprocal(out=rstd[:], in_=rstd[:]), r=[rstd], w=[rstd])
    ob = pb[0]
    for kk in range(16):
        o_ = ob[kk % 4]
        k.op("dve", lambda e, o_=o_, kk=kk: e.scalar_tensor_tensor(out=o_[:], in0=xend[:, kk, :], scalar=gv[:, kk, 2:3], in1=rstd[:],
                                                                 op0=ALU.mult, op1=ALU.mult), r=[xend, gv, rstd], w=[o_])
        k.dma(outT[kk * 128:(kk + 1) * 128, :], o_[:], r=[o_], q="pool")
    k.emit()
    return nc


def run_r(inp, mods, li, xmid, parts):
    m = mods[li]
    maps = []
    for core in range(NCORES):
        b, q = core // 4, core % 4
        idx = _tok_idx(q)
        gv = np.stack([m[b, 5 * D:6 * D], m[2, 5 * D:6 * D], inp["final_norm_w"], np.zeros(D, np.float32)], 1)
        maps.append({"xmidT": np.ascontiguousarray(xmid[b, idx].T),
                     "parts": np.ascontiguousarray(np.stack([p[:, b * T + idx] for p in parts], 0)),
                     "gv": np.ascontiguousarray(gv)})
    res = _run(build_r(), maps)
    xend = np.zeros((B, T, D), np.float32)
    out = np.zeros((B, T, D), np.float32)
    for core in range(NCORES):
        b, q = core // 4, core % 4
        idx = _tok_idx(q)
        xend[b, idx] = res[core]["xendT"].T
        out[b, idx] = res[core]["outT"].T
    return xend, out


def _colmajor(a):
    return a.reshape(64, 64, -1).transpose(1, 0, 2).reshape(S, -1)


ALL_PARTS = ("A", "S5", "SSD", "GLA", "HY")


def run_mixers(inp, mods, li, x_lat, x_ctx, parts=ALL_PARTS):
    maps = []
    for core in range(NCORES):
        b, j = core // 4, core % 4
        xT = np.ascontiguousarray(np.concatenate([x_ctx[b], x_lat[b]], 0).T)
        cols, _ = core_cols(j)
        m = mods[li]
        modv = np.stack([m[b, 0:D], m[b, D:2 * D], m[2, 0:D], m[2, D:2 * D]], 1)
        mp = {"xT": xT, "modv": np.ascontiguousarray(modv), "wA": np.ascontiguousarray(inp["w_in"][li][:, cols])}
        mp.update(host_consts())
        mp.update(scan_consts())
        mp.update(s5_host_params(inp, li, j))
        mp.update(ssd_host_params(inp, li, j))
        mp.update(gla_host_params(inp, li, j))
        mp.update(hy_host_params(inp, li, j))
        maps.append(mp)
    nc = build_am(parts=parts)
    return _run(nc, maps)


def assemble_mixers(res):
    ys, gs = [], []
    for b in range(B):
        r = res[4 * b:4 * b + 4]
        ya = np.concatenate([r[j]["ya"] for j in range(4)], 0)
        yb = np.concatenate([r[j]["yb"] for j in range(4)], 0)
        lat = yb[:, NCTX:].reshape(MW, 64, 64).transpose(0, 2, 1).reshape(MW, S)
        yb = np.concatenate([yb[:, :NCTX], lat], 1)
        yc = np.concatenate([r[0]["yc"][0:128], r[1]["yc"][0:128], r[2]["yc"][0:128], r[3]["yc"][0:128],
                             r[0]["yc"][128:256], r[1]["yc"][128:256]], 0)
        yd = np.concatenate([r[j]["yd"] for j in range(4)], 0)
        ys.append(np.stack([ya, yb, yc, yd], 0))
        gs.append(np.stack([r[j]["gates"] for j in range(4)], 0))
    return ys, gs


def _tok_idx(q):
    return np.concatenate([np.arange(64 * q, 64 * q + 64), NCTX + np.arange(1024 * q, 1024 * q + 1024)])


def run_c(inp, mods, li, x_lat, x_ctx, res_am):
    ys, gs = assemble_mixers(res_am)
    m = mods[li]
    maps = []
    rw = np.ascontiguousarray(np.concatenate([inp["moe_group_w"][li], inp["moe_expert_w"][li]], 1))
    rb = np.ascontiguousarray(np.concatenate([inp["moe_group_b"][li], inp["moe_expert_b"][li]])[None, :])
    for core in range(NCORES):
        b, q = core // 4, core % 4
        idx = _tok_idx(q)
        xall = np.concatenate([x_ctx[b], x_lat[b]], 0)
        z = np.zeros(D, np.float32)
        modv = np.stack([m[b, 2 * D:3 * D], m[2, 2 * D:3 * D], m[b, 3 * D:4 * D], m[b, 4 * D:5 * D], m[2, 3 * D:4 * D], m[2, 4 * D:5 * D], z, z], 1)
        maps.append({"xT": np.ascontiguousarray(xall[idx].T), "yT": np.ascontiguousarray(ys[b][:, :, idx]),
                     "gT": np.ascontiguousarray(gs[b][:, :, idx]), "gluw": inp["s5_glu_w"][li], "wbr": inp["w_branch"][li],
                     "wout": inp["w_out"][li], "modv": np.ascontiguousarray(modv),
                     "ssd_nw": np.ascontiguousarray(inp["ssd_norm_w"][li].reshape(6, 128).T), "rw": rw, "rb": rb})
    res = _run(build_c(), maps)
    xmid = np.zeros((B, T, D), np.float32)
    h2T = np.zeros((D, B * T), ml_dtypes.bfloat16)
    comb = np.zeros((B * T, 16), np.float32)
    for core in range(NCORES):
        b, q = core // 4, core % 4
        idx = _tok_idx(q)
        xmid[b, idx] = res[core]["xmidT"].T
        h2T[:, b * T + idx] = res[core]["h2T"]
        comb[b * T + idx] = res[core]["comb"]
    return xmid, h2T, comb


def kernel(**inputs):
    inp = {k_: np.asarray(v) for k_, v in inputs.items()}
    mods = run_mods(inp["c"], inp["c_ctx"], inp["ada_w"], inp["ada_b"])
    x_lat, x_ctx = inp["x"], inp["ctx"]
    out = None
    for li in range(DEPTH):
        res = run_mixers(inp, mods, li, x_lat, x_ctx)
        xmid, h2T, comb = run_c(inp, mods, li, x_lat, x_ctx, res)
        del res
        parts = run_e(inp, li, h2T, comb)
        xend, out = run_r(inp, mods, li, xmid, parts)
        del parts
        x_ctx, x_lat = xend[:, :NCTX], xend[:, NCTX:]
    return np.ascontiguousarray(out[:, NCTX:])
```
